# Optimizing a Trainium2 kernel written in Bass

```python
import math
import jax, jax.numpy as jnp
from jax import lax
import numpy as np

D_MODEL = 2048
BATCH = 8
SEQ = 2048
DEPTH = 4

N_MIXERS = 2
D_FF = 5632
ATT_HEADS = 32
ATT_KV_HEADS = 4
ATT_HEAD_DIM = D_MODEL // ATT_HEADS
ATT_GROUP = ATT_HEADS // ATT_KV_HEADS
WINDOW = 128
BLOCK = WINDOW
ATT_QKV_COLS = ATT_HEADS * ATT_HEAD_DIM + 2 * ATT_KV_HEADS * ATT_HEAD_DIM
M_HEADS = 4
M_V_DIM = D_MODEL // M_HEADS
M_QK_DIM = M_V_DIM // 2
CHUNK = 128
M_PROJ_COLS = 2 * M_HEADS * M_QK_DIM + 2 * M_HEADS * M_V_DIM + 2 * M_HEADS
N_ATT_LAYERS = (DEPTH + 1) // 2
N_MLSTM_LAYERS = DEPTH // 2
DEEPNORM_ALPHA = (2.0 * DEPTH) ** 0.25
DEEPNORM_BETA = (8.0 * DEPTH) ** -0.25
LN_EPS = 1e-5

kernel_name = "hybrid_swa_sink_alibi_mlstm_macaron_deepnorm"


def layer_norm(x, g, b):
    xf = x.astype(jnp.float32)
    mu = jnp.mean(xf, axis=-1, keepdims=True)
    var = jnp.mean(jnp.square(xf - mu), axis=-1, keepdims=True)
    return ((xf - mu) * lax.rsqrt(var + LN_EPS) * g.astype(jnp.float32) + b.astype(jnp.float32)).astype(x.dtype)


def swiglu(x, w1, w3, w2):
    return (jax.nn.silu(x @ w1) * (x @ w3)) @ w2


def alibi_slopes(n_heads):
    return 2.0 ** (-8.0 * jnp.arange(1, n_heads + 1, dtype=jnp.float32) / n_heads)


def sliding_window_attention(x, w_qkv, sinks, w_o):
    B, S, _ = x.shape
    H, KV, G, Dh = ATT_HEADS, ATT_KV_HEADS, ATT_GROUP, ATT_HEAD_DIM
    nb = S // BLOCK
    qkv = x @ w_qkv
    q, k, v = jnp.split(qkv, [H * Dh, H * Dh + KV * Dh], axis=-1)
    q = q.reshape(B, nb, BLOCK, KV, G, Dh)
    k = k.reshape(B, nb, BLOCK, KV, Dh)
    v = v.reshape(B, nb, BLOCK, KV, Dh)
    pad = jnp.zeros_like(k[:, :1])
    kb = jnp.concatenate([jnp.concatenate([pad, k[:, :-1]], axis=1), k], axis=2)
    vb = jnp.concatenate([jnp.concatenate([pad, v[:, :-1]], axis=1), v], axis=2)
    s = jnp.einsum('bnqkgd,bnskd->bnkgqs', q, kb).astype(jnp.float32) * (Dh ** -0.5)
    qi = jnp.arange(BLOCK)[:, None]
    kj = jnp.arange(2 * BLOCK)[None, :]
    dist = BLOCK + qi - kj
    in_window = (dist >= 0) & (dist < WINDOW)
    has_prev = (jnp.arange(nb)[:, None, None] > 0) | (kj >= BLOCK)[None]
    valid = in_window[None] & has_prev
    slopes = alibi_slopes(H).reshape(KV, G)
    s = s - slopes[:, :, None, None] * dist.astype(jnp.float32)
    s = jnp.where(valid[None, :, None, None], s, -jnp.inf)
    sink = sinks.astype(jnp.float32).reshape(KV, G)[:, :, None, None]
    m = jnp.maximum(jnp.max(s, axis=-1, keepdims=True), sink)
    p = jnp.exp(s - m)
    p = p / (jnp.sum(p, axis=-1, keepdims=True) + jnp.exp(sink - m))
    o = jnp.einsum('bnkgqs,bnskd->bnqkgd', p.astype(x.dtype), vb).reshape(B, S, H * Dh)
    return o @ w_o


def mlstm(x, w_in, b_gates, w_o):
    B, S, _ = x.shape
    H, Dk, Dv, L = M_HEADS, M_QK_DIM, M_V_DIM, CHUNK
    nc = S // L
    f32 = jnp.float32
    proj = x @ w_in
    q, k, v, og, gates = jnp.split(
        proj, [H * Dk, 2 * H * Dk, 2 * H * Dk + H * Dv, 2 * H * Dk + 2 * H * Dv], axis=-1)
    gates = gates.astype(f32) + b_gates.astype(f32)
    i_pre = gates[..., :H]
    log_f = jax.nn.log_sigmoid(gates[..., H:])

    def to_chunks(t, d):
        return t.astype(f32).reshape(B, nc, L, H, d).transpose(1, 0, 3, 2, 4)

    qc = to_chunks(q, Dk)
    kc = to_chunks(k, Dk) * (Dk ** -0.5)
    vc = to_chunks(v, Dv)
    ic = i_pre.reshape(B, nc, L, H).transpose(1, 0, 3, 2)
    fc = log_f.reshape(B, nc, L, H).transpose(1, 0, 3, 2)
    causal = jnp.tril(jnp.ones((L, L), dtype=bool))

    def step(carry, inp):
        C, n, m = carry
        qt, kt, vt, ig, lf = inp
        b = jnp.cumsum(lf, axis=-1)
        a = b + m[..., None]
        dmat = b[..., :, None] - b[..., None, :] + ig[..., None, :]
        dmat = jnp.where(causal, dmat, -jnp.inf)
        m_rows = jnp.maximum(a, jnp.max(dmat, axis=-1))
        inter = jnp.exp(a - m_rows)
        w = jnp.einsum('bhtd,bhsd->bhts', qt, kt) * jnp.exp(dmat - m_rows[..., None])
        num = inter[..., None] * jnp.einsum('bhtd,bhde->bhte', qt, C) + jnp.einsum('bhts,bhse->bhte', w, vt)
        den = inter * jnp.einsum('bhtd,bhd->bht', qt, n) + jnp.sum(w, axis=-1)
        h = num / jnp.maximum(jnp.abs(den), jnp.exp(-m_rows))[..., None]
        m_end = m_rows[..., -1]
        decay = jnp.exp(b[..., -1] + m - m_end)
        wk = jnp.exp(b[..., -1:] - b + ig - m_end[..., None])
        C = decay[..., None, None] * C + jnp.einsum('bhs,bhsd,bhse->bhde', wk, kt, vt)
        n = decay[..., None] * n + jnp.einsum('bhs,bhsd->bhd', wk, kt)
        return (C, n, m_end), h

    init = (jnp.zeros((B, H, Dk, Dv), f32), jnp.zeros((B, H, Dk), f32), jnp.zeros((B, H), f32))
    _, hs = lax.scan(step, init, (qc, kc, vc, ic, fc))
    h = hs.transpose(1, 0, 3, 2, 4).reshape(B, S, H * Dv).astype(x.dtype)
    return (h * jax.nn.sigmoid(og)) @ w_o


def setup_inputs(seed: int = 0) -> dict:
    key = jax.random.key(seed)
    ks = jax.random.split(key, 16)
    D, F = D_MODEL, D_FF
    beta = DEEPNORM_BETA
    x = jax.random.normal(ks[0], (BATCH, SEQ, D), jnp.float32)
    ffn_w1 = jax.random.normal(ks[1], (DEPTH, 2, D, F), jnp.float32) * (D ** -0.5) * beta
    ffn_w3 = jax.random.normal(ks[2], (DEPTH, 2, D, F), jnp.float32) * (D ** -0.5) * beta
    ffn_w2 = jax.random.normal(ks[3], (DEPTH, 2, F, D), jnp.float32) * (F ** -0.5) * beta
    ln_g = 1.0 + 0.02 * jax.random.normal(ks[4], (DEPTH, 3, D), jnp.float32)
    ln_b = 0.02 * jax.random.normal(ks[5], (DEPTH, 3, D), jnp.float32)
    att_scale = jnp.concatenate([
        jnp.ones((ATT_HEADS * ATT_HEAD_DIM + ATT_KV_HEADS * ATT_HEAD_DIM,), jnp.float32),
        jnp.full((ATT_KV_HEADS * ATT_HEAD_DIM,), beta, jnp.float32)])
    att_w_qkv = jax.random.normal(ks[6], (N_ATT_LAYERS, D, ATT_QKV_COLS), jnp.float32) * (D ** -0.5) * att_scale
    att_sinks = 0.5 * jax.random.normal(ks[7], (N_ATT_LAYERS, ATT_HEADS), jnp.float32)
    att_w_o = jax.random.normal(ks[8], (N_ATT_LAYERS, ATT_HEADS * ATT_HEAD_DIM, D), jnp.float32) * (D ** -0.5) * beta
    m_scale = jnp.concatenate([
        jnp.ones((2 * M_HEADS * M_QK_DIM,), jnp.float32),
        jnp.full((M_HEADS * M_V_DIM,), beta, jnp.float32),
        jnp.ones((M_HEADS * M_V_DIM,), jnp.float32),
        jnp.full((2 * M_HEADS,), 0.1, jnp.float32)])
    mlstm_w_in = jax.random.normal(ks[9], (N_MLSTM_LAYERS, D, M_PROJ_COLS), jnp.float32) * (D ** -0.5) * m_scale
    i_bias = 0.1 * jax.random.normal(ks[10], (N_MLSTM_LAYERS, M_HEADS), jnp.float32)
    f_bias = jnp.linspace(3.0, 6.0, M_HEADS, dtype=jnp.float32)[None] + 0.1 * jax.random.normal(ks[11], (N_MLSTM_LAYERS, M_HEADS), jnp.float32)
    mlstm_b_gates = jnp.concatenate([i_bias, f_bias], axis=-1)
    mlstm_w_o = jax.random.normal(ks[12], (N_MLSTM_LAYERS, M_HEADS * M_V_DIM, D), jnp.float32) * (D ** -0.5) * beta
    return {"x": x, "ffn_w1": ffn_w1, "ffn_w3": ffn_w3, "ffn_w2": ffn_w2,
            "ln_g": ln_g, "ln_b": ln_b,
            "att_w_qkv": att_w_qkv, "att_sinks": att_sinks, "att_w_o": att_w_o,
            "mlstm_w_in": mlstm_w_in, "mlstm_b_gates": mlstm_b_gates, "mlstm_w_o": mlstm_w_o}


def reference(x, ffn_w1, ffn_w3, ffn_w2, ln_g, ln_b, att_w_qkv, att_sinks, att_w_o,
              mlstm_w_in, mlstm_b_gates, mlstm_w_o):
    alpha = DEEPNORM_ALPHA
    for layer in range(DEPTH):
        ffn_a = swiglu(x, ffn_w1[layer, 0], ffn_w3[layer, 0], ffn_w2[layer, 0])
        x = layer_norm(alpha * x + 0.5 * ffn_a, ln_g[layer, 0], ln_b[layer, 0])
        j = layer // N_MIXERS
        if layer % N_MIXERS == 0:
            y = sliding_window_attention(x, att_w_qkv[j], att_sinks[j], att_w_o[j])
        else:
            y = mlstm(x, mlstm_w_in[j], mlstm_b_gates[j], mlstm_w_o[j])
        x = layer_norm(alpha * x + y, ln_g[layer, 1], ln_b[layer, 1])
        ffn_b = swiglu(x, ffn_w1[layer, 1], ffn_w3[layer, 1], ffn_w2[layer, 1])
        x = layer_norm(alpha * x + 0.5 * ffn_b, ln_g[layer, 2], ln_b[layer, 2])
    return x
```

```python
import contextlib
import numpy as np
import concourse.bass as bass
import concourse.mybir as mybir
from concourse.bass_utils import run_bass_kernel_spmd

F32 = mybir.dt.float32
BF16 = mybir.dt.bfloat16
AF = mybir.ActivationFunctionType
ALU = mybir.AluOpType
AX = mybir.AxisListType

D = 2048
FF = 5632
KC = D // 128
FCH = FF // 128
TT = 512
FB = 256
NFB = FF // FB
DEPTH = 4
ALPHA = (2.0 * DEPTH) ** 0.25
EPS_S = 1e-5 / (ALPHA * ALPHA)
NEG = -30000.0

ENGS = ("pe", "act", "dve", "pool", "sp")


class Tok:
    __slots__ = ("w", "r", "rd")

    def __init__(self):
        self.w = None
        self.r = {}
        self.rd = []


class Op:
    __slots__ = ("eng", "fn", "deps", "dsem", "dcount", "need_sig", "sig")

    def __init__(self, eng, fn, deps, dsem):
        self.eng = eng
        self.fn = fn
        self.deps = deps
        self.dsem = dsem
        self.dcount = 0
        self.need_sig = False
        self.sig = 0


class Sched:
    def __init__(self, nc):
        self.nc = nc
        self.ops = []
        self.streams = {e: [] for e in ENGS}
        self.dsem_counts = {}
        self.last = {e: None for e in ENGS}
        self.pending_dma = []
        self.cost = {}

    def op(self, eng, meth, reads=(), writes=(), dsem=None, **kw):
        i = self.add(eng, (meth, kw), reads, writes, dsem)
        self.cost[i] = self._cost(eng, meth, kw)
        return i

    @staticmethod
    def _nfree(ap):
        n = 1
        for d in ap.shape[1:]:
            n *= d
        return n

    def _cost(self, eng, meth, kw):
        try:
            if meth == "dma_start":
                o, i_ = kw["out"], kw["in_"]
                b = max(self._nfree(o) * o.shape[0] * mybir.dt.size(o.dtype),
                        self._nfree(i_) * i_.shape[0] * mybir.dt.size(i_.dtype))
                return ("dma", b / 300.0)
            if meth == "matmul":
                n = self._nfree(kw["out"])
                f = 4.0 if kw["lhsT"].dtype == F32 else 1.0
                return ("c", 15.0 + f * n / 2.4)
            if meth == "transpose":
                return ("c", 110.0)
            src = kw.get("in_", kw.get("in0", kw.get("ap")))
            n = self._nfree(src)
            if eng == "act":
                return ("c", 220.0 + 1.2 * n + (100.0 if "accum_out" in kw else 0.0))
            per = 1.04
            for k in ("in_", "in0", "in1"):
                a = kw.get(k)
                if a is not None and hasattr(a, "tensor") and type(a.tensor).__name__ == "PSumTensorHandle":
                    per = 1.35
            if meth == "reciprocal":
                per = 6.5
            if meth == "memset":
                per = 0.5
            return ("c", 70.0 + per * n)
        except Exception:
            return ("c", 500.0)

    def reschedule(self, lo, hi):
        import heapq
        ops, cost = self.ops, self.cost
        idx = [i for i in range(lo, hi) if ops[i].fn is not None]
        if not idx:
            return
        inseg = set(idx)
        ndep = {}
        users = {}
        for i in idx:
            ds = [j for j in ops[i].deps if j in inseg]
            ndep[i] = len(ds)
            for j in ds:
                users.setdefault(j, []).append(i)
        fin = {}
        efree = {e: 0.0 for e in ENGS}
        dma_free = 0.0
        SYNC = 180.0
        ready = [i for i in idx if ndep[i] == 0]
        order = []
        while ready:
            best, bstart = None, None
            for i in ready:
                op = ops[i]
                t = efree[op.eng]
                for j in op.deps:
                    if j in fin:
                        tj = fin[j] + (0.0 if (ops[j].eng == op.eng and ops[j].dsem is None) else SYNC)
                        if tj > t:
                            t = tj
                if bstart is None or t < bstart - 1e-9 or (abs(t - bstart) <= 1e-9 and i < best):
                    best, bstart = i, t
            i = best
            ready.remove(i)
            op = ops[i]
            kind, dur = cost.get(i, ("c", 500.0))
            if kind == "dma":
                efree[op.eng] = bstart + 60.0
                st = max(bstart + 60.0, dma_free)
                dma_free = st + dur
                fin[i] = dma_free + 1800.0
            else:
                efree[op.eng] = bstart + dur
                fin[i] = bstart + dur
            order.append(i)
            for u in users.get(i, ()):
                ndep[u] -= 1
                if ndep[u] == 0:
                    ready.append(u)
        assert len(order) == len(idx)
        pos = {i: k for k, i in enumerate(order)}
        for e in ENGS:
            st = self.streams[e]
            seg = [i for i in st if i in inseg]
            if not seg:
                continue
            first = st.index(seg[0])
            seg_sorted = sorted(seg, key=lambda i: pos[i])
            assert st[first:first + len(seg)] == seg
            st[first:first + len(seg)] = seg_sorted

    def add(self, eng, fn, reads=(), writes=(), dsem=None, extra_deps=()):
        i = len(self.ops)
        deps = set(extra_deps)
        for t in reads:
            if t.w is not None:
                deps.add(t.w)
        for t in writes:
            if t.w is not None:
                deps.add(t.w)
            deps.update(t.r.values())
            deps.update(t.rd)
        for t in reads:
            if dsem is not None:
                t.rd.append(i)
            else:
                t.r[eng] = i
        for t in writes:
            t.w = i
            t.r = {}
            t.rd = []
        if dsem is not None:
            dsem = eng + "_" + dsem
        op = Op(eng, fn, deps, dsem)
        if dsem is not None:
            c = self.dsem_counts.get(dsem, 0) + 16
            self.dsem_counts[dsem] = c
            op.dcount = c
            self.pending_dma.append(i)
        self.ops.append(op)
        self.streams[eng].append(i)
        if fn is not None:
            self.last[eng] = i
        return i

    def barrier(self, dmas=True):
        deps = set(self.pending_dma) if dmas else set()
        for e in ENGS:
            if self.last[e] is not None:
                deps.add(self.last[e])
        if dmas:
            self.pending_dma = []
        for e in ENGS:
            self.add(e, None, extra_deps=deps)

    def _skip(self, d, op):
        return d.eng == op.eng and op.dsem is None and d.eng == "pe"

    def emit(self, stack):
        nc = self.nc
        ops = self.ops
        for op in ops:
            for j in op.deps:
                d = ops[j]
                if d.dsem is not None or self._skip(d, op):
                    continue
                d.need_sig = True
        for e in ENGS:
            cnt = 0
            dlast = {}
            for i in self.streams[e]:
                op = ops[i]
                if op.dsem is None:
                    if op.need_sig:
                        cnt += 1
                        op.sig = cnt
                else:
                    dlast[op.dsem] = dlast.get(op.dsem, 0) + 16
                    op.dcount = dlast[op.dsem]
        esem = {e: stack.enter_context(nc.semaphore("s_" + e)) for e in ENGS}
        dsem = {k: stack.enter_context(nc.semaphore("d_" + k)) for k in self.dsem_counts}
        engobj = {"pe": "tensor", "act": "scalar", "dve": "vector", "pool": "gpsimd", "sp": "sync"}

        def run_stream(e, eng):
            waited = {}
            for i in self.streams[e]:
                op = ops[i]
                need = {}
                for j in op.deps:
                    d = ops[j]
                    if d.dsem is not None:
                        key = ("d", d.dsem)
                        val = d.dcount
                    else:
                        if self._skip(d, op):
                            continue
                        key = ("e", d.eng)
                        val = d.sig
                    if need.get(key, 0) < val:
                        need[key] = val
                for key, val in need.items():
                    if waited.get(key, 0) >= val:
                        continue
                    waited[key] = val
                    sem = dsem[key[1]] if key[0] == "d" else esem[key[1]]
                    eng.wait_ge(sem, val)
                if op.fn is None:
                    continue
                ins = getattr(eng, op.fn[0])(**op.fn[1])
                if op.dsem is not None:
                    ins.then_inc(dsem[op.dsem], 16)
                elif op.need_sig:
                    ins.then_inc(esem[e], 1)

        with nc.Block() as block:
            for e in ENGS:
                if not self.streams[e]:
                    continue

                def body(eng, e=e):
                    run_stream(e, eng)
                getattr(block, engobj[e])(body)


class Rot:
    def __init__(self, bufs, ntok=None):
        self.bufs = bufs
        self.toks = [Tok() if ntok is None else [Tok() for _ in range(ntok)] for _ in bufs]
        self.i = 0

    def next(self):
        k = self.i % len(self.bufs)
        self.i += 1
        return self.bufs[k], self.toks[k], k


class Builder:
    def __init__(self, s_tok, plan, n_ffn, n_att, n_ml, resched=("mlstm", "att")):
        self.resched = set(resched)
        self.S_TOK = s_tok
        self.NTILE = s_tok // TT
        self.plan = plan
        nc = self.nc = bass.Bass("TRN2", target_bir_lowering=False)
        self.S = Sched(nc)
        dt = nc.dram_tensor
        self.xT = dt("xT", [D, s_tok], F32, kind="ExternalInput").ap()
        self.outT = dt("outT", [D, s_tok], F32, kind="ExternalOutput").ap()
        self.scr = [dt("scrA", [D, s_tok], F32, kind="Internal").ap(),
                    dt("scrB", [D, s_tok], F32, kind="Internal").ap()]
        nsb = len(plan)
        self.ln_g = dt("ln_g", [128, nsb * KC], F32, kind="ExternalInput").ap()
        self.ln_b = dt("ln_b", [128, nsb * KC], F32, kind="ExternalInput").ap()
        if n_ffn:
            self.w1 = dt("w1", [n_ffn * NFB, 128, KC, FB], F32, kind="ExternalInput").ap()
            self.w3 = dt("w3", [n_ffn * NFB, 128, KC, FB], F32, kind="ExternalInput").ap()
            self.w2 = dt("w2", [n_ffn * KC, 128, FCH, 128], F32, kind="ExternalInput").ap()
        if n_att:
            self.wq = dt("wq", [n_att * KC, 128, KC, 128], F32, kind="ExternalInput").ap()
            self.wk = dt("wk", [n_att * 8, 128, KC, 128], F32, kind="ExternalInput").ap()
            self.wv = dt("wv", [n_att, 128, KC, 256], F32, kind="ExternalInput").ap()
            self.wo_a = dt("wo_a", [n_att * KC, 128, KC, 128], F32, kind="ExternalInput").ap()
            self.sinks = dt("sinks", [n_att, 128, 32], F32, kind="ExternalInput").ap()
            self.alibi = dt("alibi", [128, 32 * 256], F32, kind="ExternalInput").ap()
            self.identd = dt("identd", [128, 128], F32, kind="ExternalInput").ap()
        if n_ml:
            self.m_wq = dt("m_wq", [n_ml * 8, 128, KC, 128], F32, kind="ExternalInput").ap()
            self.m_wk = dt("m_wk", [n_ml * 8, 128, KC, 128], F32, kind="ExternalInput").ap()
            self.m_wtm = dt("m_wtm", [n_ml * 6, 128, KC, 512], F32, kind="ExternalInput").ap()
            self.m_wog = dt("m_wog", [n_ml * KC, 128, KC, 128], F32, kind="ExternalInput").ap()
            self.m_wg = dt("m_wg", [n_ml, 128, KC, 8], F32, kind="ExternalInput").ap()
            self.m_bg = dt("m_bg", [n_ml, 128, 8], F32, kind="ExternalInput").ap()
            self.wo_m = dt("wo_m", [n_ml * KC, 128, KC, 128], F32, kind="ExternalInput").ap()
            self.m_const = dt("m_const", [128, 4 * 128], F32, kind="ExternalInput").ap()
            if not hasattr(self, "identd"):
                self.identd = dt("identd", [128, 128], F32, kind="ExternalInput").ap()

    def dtok(self, ap, ti, c):
        if ap is self.xT:
            return []
        tab = self.__dict__.setdefault("_dtoks", {}).setdefault(
            id(ap), [[Tok() for _ in range(KC)] for _ in range(self.NTILE)])
        return [tab[ti][c]]

    def sb(self, st, name, shape, dtype):
        self.uid = getattr(self, "uid", 0) + 1
        return st.enter_context(self.nc.sbuf_tensor(f"{name}_u{self.uid}", shape, dtype))

    def build(self):
        nc, S = self.nc, self.S
        with contextlib.ExitStack() as top:
            self.ps = [top.enter_context(nc.psum_tensor(f"ps{i}", [128, 512], F32)) for i in range(8)]
            self.pst = [Tok() for _ in range(8)]
            nsb = len(self.plan)
            self.gcol = self.sb(top, "gcol", [128, nsb * KC], F32)
            self.bcol = self.sb(top, "bcol", [128, nsb * KC], F32)
            self.ones = self.sb(top, "ones", [128, 128], F32)
            self.ctok = Tok()
            self.gtok = Tok()
            self.btok = Tok()
            S.op("sp", "dma_start", writes=[self.gtok], dsem="cg", out=self.gcol[:], in_=self.ln_g)
            S.op("sp", "dma_start", writes=[self.btok], dsem="cb", out=self.bcol[:], in_=self.ln_b)
            S.op("dve", "memset", writes=[self.ctok], ap=self.ones[:], constant=1.0)
            self.onesbf = self.sb(top, "onesbf", [128, 128], BF16)
            S.op("dve", "memset", writes=[self.ctok], ap=self.onesbf[:], constant=1.0)
            self.y = self.sb(top, "y", [128, KC, TT], F32)
            self.ytok = [Tok() for _ in range(KC)]
            self.sq = Rot([self.sb(top, f"sq{i}", [128, 2, TT], BF16) for i in range(2)])
            self.ost = Rot([self.sb(top, f"ost{i}", [128, TT], F32) for i in range(2)])
            self.stat = self.sb(top, "stat", [128, 3, TT], F32)
            self.stattok = Tok()
            self.setup_consts(top)
            S.barrier()
            nsub = len(self.plan)
            for si, sub in enumerate(self.plan):
                src = self.xT if si == 0 else self.scr[(si - 1) % 2]
                dst = self.outT if si == nsub - 1 else self.scr[si % 2]
                seg_lo = len(S.ops)
                with contextlib.ExitStack() as st:
                    if sub[0] == "ffn":
                        self.ffn(st, src, dst, sub[1], si)
                    elif sub[0] == "att":
                        self.att(st, src, dst, sub[1], si)
                    elif sub[0] == "mlstm":
                        self.mlstm(st, src, dst, sub[1], si)
                    last = (si == nsub - 1)
                    if last:
                        self.flush_norm()
                    if sub[0] in self.resched:
                        S.reschedule(seg_lo, len(S.ops))
                    if si < 3:
                        self.sbuf_left = getattr(self, "sbuf_left", {})
                        self.sbuf_left[sub[0]] = nc.sbuf_bytes_remaining
                    S.barrier(dmas=last)
            S.emit(top)
        return nc

    def tail(self, t0, src, dst, si, coef, n_fc, lhs_of, hT_of, htoks):
        S, ps, pst = self.S, self.ps, self.pst
        y, ytok = self.y, self.ytok
        srcv = src.rearrange("(c p) t -> c p t", p=128)
        dstv = dst.rearrange("(c p) t -> c p t", p=128)
        S1, S2 = 6, 7
        pend = None

        def stats(c):
            sqb, sqt, _ = self.sq.next()
            S.op("act", "copy", reads=[ytok[c]], writes=[sqt], out=sqb[:, 0, :], in_=y[:, c, :])
            S.op("act", "activation", reads=[ytok[c]], writes=[sqt], out=sqb[:, 1, :], in_=y[:, c, :], func=AF.Square)
            S.op("pe", "matmul", reads=[sqt, self.ctok], writes=[pst[S1]],
                 out=ps[S1][:], lhsT=self.onesbf[:], rhs=sqb[:, 0, :], start=(c == 0), stop=(c == KC - 1))
            S.op("pe", "matmul", reads=[sqt, self.ctok], writes=[pst[S2]],
                 out=ps[S2][:], lhsT=self.onesbf[:], rhs=sqb[:, 1, :], start=(c == 0), stop=(c == KC - 1))

        for c in range(KC):
            S.op("sp", "dma_start", reads=self.dtok(src, t0 // TT, c), writes=[ytok[c]], dsem=f"xr{c}",
                 out=y[:, c, :], in_=srcv[c, :, t0:t0 + TT])
            lhs, wtok = lhs_of(c)
            bank = 4 + (c % 2)
            for fc in range(n_fc):
                S.op("pe", "matmul", reads=[wtok, htoks[fc]], writes=[pst[bank]],
                     out=ps[bank][:], lhsT=lhs(fc), rhs=hT_of(fc), start=(fc == 0), stop=(fc == n_fc - 1))
            if pend is not None:
                stats(pend)
            S.op("dve", "scalar_tensor_tensor", reads=[pst[bank]], writes=[ytok[c]],
                 out=y[:, c, :], in0=ps[bank][:], scalar=coef, in1=y[:, c, :], op0=ALU.mult, op1=ALU.add)
            pend = c
        stats(pend)
        st_, stt = self.stat, self.stattok
        S.op("dve", "tensor_scalar_mul", reads=[pst[S1]], writes=[stt],
             out=st_[:, 0, :], in0=ps[S1][:], scalar1=1.0 / D)
        S.op("dve", "tensor_tensor", reads=[stt], writes=[stt],
             out=st_[:, 2, :], in0=st_[:, 0, :], in1=st_[:, 0, :], op=ALU.mult)
        S.op("dve", "scalar_tensor_tensor", reads=[pst[S2], stt], writes=[stt],
             out=st_[:, 1, :], in0=ps[S2][:], scalar=1.0 / D, in1=st_[:, 2, :], op0=ALU.mult, op1=ALU.subtract)
        S.op("dve", "tensor_scalar_add", reads=[stt], writes=[stt],
             out=st_[:, 1, :], in0=st_[:, 1, :], scalar1=EPS_S)
        S.op("act", "sqrt", reads=[stt], writes=[stt], out=st_[:, 1, :], in_=st_[:, 1, :])
        S.op("dve", "reciprocal", reads=[stt], writes=[stt], out=st_[:, 1, :], in_=st_[:, 1, :])
        def norm_step(c):
            S.op("dve", "tensor_tensor", reads=[stt], writes=[ytok[c]],
                 out=y[:, c, :], in0=y[:, c, :], in1=st_[:, 0, :], op=ALU.subtract)
            S.op("dve", "tensor_tensor", reads=[stt], writes=[ytok[c]],
                 out=y[:, c, :], in0=y[:, c, :], in1=st_[:, 1, :], op=ALU.mult)
            ob, ot, ok = self.ost.next()
            col = si * KC + c
            S.op("act", "activation", reads=[ytok[c], self.gtok, self.btok], writes=[ot],
                 out=ob[:], in_=y[:, c, :], func=AF.Identity,
                 bias=self.bcol[:, col:col + 1], scale=self.gcol[:, col:col + 1])
            S.op("sp", "dma_start", reads=[ot], writes=self.dtok(dst, t0 // TT, c), dsem=f"st{ok}",
                 out=dstv[c, :, t0:t0 + TT], in_=ob[:])

        self.pending_norm = [(lambda c=c: norm_step(c)) for c in range(KC)]

    def flush_norm(self, n=None):
        pn = getattr(self, "pending_norm", [])
        k = len(pn) if n is None else min(n, len(pn))
        for f in pn[:k]:
            f()
        self.pending_norm = pn[k:]

    def load_xT(self, xb, xtoks, xk, src, t0, ntok=TT, off=0):
        srcv = src.rearrange("(c p) t -> p c t", p=128)
        for q in range(4):
            rd = [t for c in range(4 * q, 4 * q + 4) for t in self.dtok(src, t0 // TT, c)]
            self.S.op("pool", "dma_start", reads=rd, writes=[xtoks[q]], dsem=f"x{xk}_{q}",
                      out=xb[:, 4 * q:4 * q + 4, off:off + ntok], in_=srcv[:, 4 * q:4 * q + 4, t0:t0 + ntok])

    def ffn(self, st, src, dst, widx, si):
        S, ps, pst = self.S, self.ps, self.pst
        xrot = Rot([self.sb(st, f"fx{i}", [128, KC, TT], BF16) for i in range(2)], ntok=4)
        hT = self.sb(st, "hT", [128, FCH, TT], BF16)
        htoks = [Tok() for _ in range(FCH)]
        w13 = Rot([self.sb(st, f"w13_{i}", [128, 2, KC, FB], BF16) for i in range(2)], ntok=2)
        w2r = Rot([self.sb(st, f"w2_{i}", [128, FCH, 128], BF16) for i in range(2)])
        sg = Rot([self.sb(st, f"sg{i}", [128, TT], F32) for i in range(2)])
        def issue_w13(fb):
            wb, wtok, wk = w13.next()
            S.op("pool", "dma_start", writes=[wtok[0]], dsem=f"w13_{wk}_0", out=wb[:, 0], in_=self.w1[widx * NFB + fb])
            S.op("pool", "dma_start", writes=[wtok[1]], dsem=f"w13_{wk}_1", out=wb[:, 1], in_=self.w3[widx * NFB + fb])
            return wb, wtok

        pre = {}
        for ti in range(self.NTILE):
            t0 = ti * TT
            if ti in pre:
                xb, xtok, wpre = pre.pop(ti)
            else:
                wpre = [issue_w13(0)]
                xb, xtok, xk = xrot.next()
                self.load_xT(xb, xtok, xk, src, t0)
                wpre.append(issue_w13(1))
            for fb in range(NFB):
                wb, wtok = wpre[fb] if fb < len(wpre) else issue_w13(fb)
                for fi in range(FB // 128):
                    f = fb * (FB // 128) + fi
                    ba = (f % 2) * 2
                    for wi in range(2):
                        for k in range(KC):
                            S.op("pe", "matmul", reads=[wtok[wi], xtok[k // 4]], writes=[pst[ba + wi]],
                                 out=ps[ba + wi][:], lhsT=wb[:, wi, k, fi * 128:(fi + 1) * 128], rhs=xb[:, k, :],
                                 start=(k == 0), stop=(k == KC - 1))
                    sgb, sgt, _ = sg.next()
                    S.op("act", "activation", reads=[pst[ba]], writes=[sgt], out=sgb[:], in_=ps[ba][:], func=AF.Silu)
                    S.op("dve", "tensor_tensor", reads=[sgt, pst[ba + 1]], writes=[htoks[f]],
                         out=hT[:, f, :], in0=sgb[:], in1=ps[ba + 1][:], op=ALU.mult)
                    if f >= 1:
                        self.flush_norm(1)
            self.flush_norm()

            def lhs_of(c, ti=ti):
                wb, wtok, wk = w2r.next()
                S.op("pool", "dma_start", writes=[wtok], dsem=f"w2_{wk}", out=wb[:], in_=self.w2[widx * KC + c])
                if ti + 1 < self.NTILE:
                    if c == 1:
                        nxb, nxtok, nxk = xrot.next()
                        self.load_xT(nxb, nxtok, nxk, src, (ti + 1) * TT)
                        pre[ti + 1] = (nxb, nxtok, [])
                    elif c in (3, 5):
                        pre[ti + 1][2].append(issue_w13(len(pre[ti + 1][2])))
                return (lambda fc: wb[:, fc, :]), wtok

            self.tail(t0, src, dst, si, 0.5 / ALPHA, FCH, lhs_of, lambda fc: hT[:, fc, :], htoks)

    def setup_consts(self, top):
        S = self.S
        if hasattr(self, "identd"):
            self.ident = self.sb(top, "ident", [128, 128], BF16)
            self.itok = Tok()
            S.op("pool", "dma_start", writes=[self.itok], dsem="ci", out=self.ident[:], in_=self.identd)

    @staticmethod
    def bc(ap2, n):
        g = ap2.shape[1]
        return ap2.rearrange("p (g o) -> p g o", o=1).broadcast_to([128, g, n])

    def att(self, st, src, dst, j, si):
        S, ps, pst = self.S, self.ps, self.pst
        xrot = Rot([self.sb(st, f"ax{i}", [128, KC, TT], BF16) for i in range(1)], ntok=4)
        qT = self.sb(st, "qT", [128, KC, TT], BF16)
        qtok = [Tok() for _ in range(KC)]
        kT2 = self.sb(st, "kT2", [128, 8, 128 + TT], BF16)
        ktok = [Tok() for _ in range(8)]
        vpad = self.sb(st, "vpad", [128, 5, 4, 2, 128], BF16)
        vtok = [Tok() for _ in range(5)]
        wsl = Rot([self.sb(st, f"awq{i}", [128, KC, 128], BF16) for i in range(3)])
        wv = self.sb(st, "awv", [128, KC, 256], BF16)
        bias = self.sb(st, "abias", [128, 32 * 256], F32)
        sinks = self.sb(st, "asink", [128, 32], F32)
        atok = Tok()
        astok = Tok()
        avtok = Tok()
        sbr = Rot([self.sb(st, f"asb{i}", [128, 8 * 256], F32) for i in range(2)])
        pnr = Rot([self.sb(st, f"apn{i}", [128, 8, 256], BF16) for i in range(2)])
        ptr = Rot([self.sb(st, f"apt{i}", [128, 16, 128], BF16) for i in range(2)])
        smr = Rot([self.sb(st, f"asm{i}", [128, 6, 8], F32) for i in range(2)])
        oT = self.sb(st, "oT", [128, KC, TT], BF16)
        otoks = [Tok() for _ in range(KC)]
        psb = [ps[4].bitcast(BF16), ps[5].bitcast(BF16)]

        S.op("sp", "dma_start", writes=[atok], dsem="ab", out=bias[:], in_=self.alibi)
        S.op("sp", "dma_start", writes=[astok], dsem="as", out=sinks[:], in_=self.sinks[j])
        S.op("pool", "dma_start", writes=[avtok], dsem="av", out=wv[:], in_=self.wv[j])
        S.op("dve", "memset", writes=vtok, ap=vpad[:], constant=0.0)
        S.op("dve", "memset", writes=ktok, ap=kT2[:], constant=0.0)

        for ti in range(self.NTILE):
            t0 = ti * TT
            if ti == 0:
                nx = xrot.next()
                self.load_xT(nx[0], nx[1], nx[2], src, t0)
            xb, xtok, xk = nx
            if ti > 0:
                for kv in range(8):
                    S.op("dve", "tensor_copy", reads=[], writes=[ktok[kv]],
                         out=kT2[:, kv, 0:128], in_=kT2[:, kv, TT:TT + 128])
                S.op("dve", "tensor_copy", reads=[vtok[4]], writes=[vtok[0]], out=vpad[:, 0], in_=vpad[:, 4])
            pbc = [0]

            def issue_qk(c):
                wb, wtok, wk = wsl.next()
                srcw = self.wq[j * KC + c] if c < KC else self.wk[j * 8 + (c - KC)]
                S.op("pool", "dma_start", writes=[wtok], dsem=f"aw{wk}", out=wb[:], in_=srcw)
                return wb, wtok

            def proj_qk(c, wbt, bank):
                wb, wtok = wbt
                for k in range(KC):
                    S.op("pe", "matmul", reads=[wtok, xtok[k // 4]], writes=[pst[bank]],
                         out=ps[bank][:], lhsT=wb[:, k, :], rhs=xb[:, k, :], start=(k == 0), stop=(k == KC - 1))
                if c < KC:
                    S.op("act", "copy", reads=[pst[bank]], writes=[qtok[c]], out=qT[:, c, :], in_=ps[bank][:])
                else:
                    kv = c - KC
                    S.op("act", "copy", reads=[pst[bank]], writes=[ktok[kv]],
                         out=kT2[:, kv, 128:128 + TT], in_=ps[bank][:])

            for c in list(range(KC, KC + 8)) + [0, 1, 2, 3]:
                proj_qk(c, issue_qk(c), pbc[0] % 2)
                pbc[0] += 1
                self.flush_norm(1)
            for blk in range(4):
                bank = 2 + blk % 2
                for k in range(KC):
                    S.op("pe", "matmul", reads=[avtok, xtok[k // 4]], writes=[pst[bank]],
                         out=ps[bank][:, 0:256], lhsT=xb[:, k, blk * 128:(blk + 1) * 128], rhs=wv[:, k, :],
                         start=(k == 0), stop=(k == KC - 1))
                pv = ps[bank][:, 0:256].rearrange("p (a b) -> p a b", b=64)
                S.op("dve", "tensor_copy", reads=[pst[bank]], writes=[vtok[1 + blk]],
                     out=vpad[:, 1 + blk, :, 0, 0:64], in_=pv)
                S.op("dve", "tensor_copy", reads=[pst[bank]], writes=[vtok[1 + blk]],
                     out=vpad[:, 1 + blk, :, 1, 64:128], in_=pv)
            self.flush_norm()
            groups = [(blk, kv) for kv in range(4) for blk in range(4)]
            gst = {}

            def stA(i):
                blk, kv = groups[i]
                for g in range(8):
                    c = kv * 4 + g // 2
                    par = g % 2
                    bank = g // 2
                    S.op("pe", "matmul", reads=[qtok[c], ktok[kv * 2 + par]], writes=[pst[bank]],
                         out=ps[bank][:, par * 256:(par + 1) * 256],
                         lhsT=qT[:, c, blk * 128:(blk + 1) * 128],
                         rhs=kT2[:, kv * 2 + par, blk * 128:blk * 128 + 256],
                         start=True, stop=True)

            def stB(i):
                blk, kv = groups[i]
                first = (ti == 0 and blk == 0)
                sbt, sbtok, _ = sbr.next()
                sb3 = sbt[:].rearrange("p (g k) -> p g k", k=256)
                for gp in range(4):
                    h0 = kv * 8 + 2 * gp
                    S.op("dve", "scalar_tensor_tensor", reads=[pst[gp], atok], writes=[sbtok],
                         out=sbt[:, gp * 512:(gp + 1) * 512], in0=ps[gp][:], scalar=0.125,
                         in1=bias[:, h0 * 256:(h0 + 2) * 256], op0=ALU.mult, op1=ALU.add)
                if first:
                    S.op("dve", "memset", writes=[sbtok], ap=sb3[:, :, 0:128], constant=NEG)
                gst[i] = (sbt, sbtok, sb3)

            def stB2(i):
                blk, kv = groups[i]
                sbt, sbtok, sb3 = gst[i]
                sm, smtok, _ = smr.next()
                snk = sinks[:, kv * 8:(kv + 1) * 8]
                S.op("dve", "tensor_reduce", reads=[sbtok], writes=[smtok],
                     out=sm[:, 0, :], in_=sb3, axis=AX.X, op=ALU.max)
                S.op("dve", "tensor_tensor", reads=[smtok, astok], writes=[smtok],
                     out=sm[:, 1, :], in0=sm[:, 0, :], in1=snk, op=ALU.max)
                S.op("dve", "tensor_tensor", reads=[smtok, astok], writes=[smtok],
                     out=sm[:, 3, :], in0=snk, in1=sm[:, 1, :], op=ALU.subtract)
                S.op("dve", "tensor_scalar_mul", reads=[smtok], writes=[smtok],
                     out=sm[:, 1, :], in0=sm[:, 1, :], scalar1=-1.0)
                for g in range(8):
                    S.op("act", "activation", reads=[sbtok, smtok], writes=([sbtok, smtok] if g == 7 else []),
                         out=sb3[:, g, :], in_=sb3[:, g, :], func=AF.Exp, bias=sm[:, 1, g:g + 1],
                         accum_out=sm[:, 2, g:g + 1])
                S.op("act", "activation", reads=[smtok], writes=[smtok], out=sm[:, 3, :], in_=sm[:, 3, :],
                     func=AF.Exp)
                gst[i] = (sbt, sbtok, sb3, sm, smtok)

            def stB3(i):
                sbt, sbtok, sb3, sm, smtok = gst[i]
                S.op("dve", "tensor_tensor", reads=[smtok], writes=[smtok],
                     out=sm[:, 4, :], in0=sm[:, 2, :], in1=sm[:, 3, :], op=ALU.add)
                S.op("dve", "reciprocal", reads=[smtok], writes=[smtok], out=sm[:, 5, :], in_=sm[:, 4, :])
                pn, pntok, _ = pnr.next()
                S.op("dve", "tensor_tensor", reads=[sbtok, smtok], writes=[pntok],
                     out=pn[:], in0=sb3, in1=self.bc(sm[:, 5, :], 256), op=ALU.mult)
                gst[i] = (pn, pntok)

            def stC(i):
                pn, pntok = gst[i]
                for g in range(8):
                    for kb in range(2):
                        ii = g * 2 + kb
                        S.op("pe", "transpose", reads=[pntok, self.itok], writes=[pst[4 + ii // 8]],
                             out=psb[ii // 8][:, (ii % 8) * 128:(ii % 8 + 1) * 128],
                             in_=pn[:, g, kb * 128:(kb + 1) * 128], identity=self.ident[:])
                pt, pttok, _ = ptr.next()
                S.op("act", "copy", reads=[pst[4]], writes=[pttok],
                     out=pt[:, 0:8, :], in_=psb[0][:].rearrange("p (a b) -> p a b", b=128))
                S.op("dve", "tensor_copy", reads=[pst[5]], writes=[pttok],
                     out=pt[:, 8:16, :], in_=psb[1][:].rearrange("p (a b) -> p a b", b=128))
                gst[i] = (pt, pttok)

            def stD(i):
                blk, kv = groups[i]
                pt, pttok = gst.pop(i)
                for jp in range(4):
                    c = kv * 4 + jp
                    bank = 6
                    n = 0
                    for par in range(2):
                        for kb in range(2):
                            S.op("pe", "matmul", reads=[pttok, vtok[blk + kb]], writes=[pst[bank]],
                                 out=ps[bank][:, 0:128], lhsT=vpad[:, blk + kb, kv, par, :],
                                 rhs=pt[:, (2 * jp + par) * 2 + kb, :], start=(n == 0), stop=(n == 3))
                            n += 1
                    S.op("act", "copy", reads=[pst[bank]], writes=[otoks[c]],
                         out=oT[:, c, blk * 128:(blk + 1) * 128], in_=ps[bank][:, 0:128])

            ng = len(groups)
            stA(0)
            nextq = issue_qk(4)
            for i in range(ng + 2):
                if i < 12:
                    curq = nextq
                    if i + 1 < 12:
                        nextq = issue_qk(4 + i + 1)
                    proj_qk(4 + i, curq, 7)
                if i < ng:
                    stB(i)
                if i + 1 < ng:
                    stA(i + 1)
                if i < ng:
                    stB2(i)
                if 1 <= i <= ng:
                    stB3(i - 1)
                    stC(i - 1)
                if 2 <= i <= ng + 1:
                    stD(i - 2)
            if ti + 1 < self.NTILE:
                nx = xrot.next()
                self.load_xT(nx[0], nx[1], nx[2], src, (ti + 1) * TT)

            def lhs_of(c):
                wb, wtok, wk = wsl.next()
                S.op("pool", "dma_start", writes=[wtok], dsem=f"aw{wk}", out=wb[:], in_=self.wo_a[j * KC + c])
                return (lambda fc: wb[:, fc, :]), wtok

            self.tail(t0, src, dst, si, 1.0 / ALPHA, KC, lhs_of, lambda fc: oT[:, fc, :], otoks)

    def mlstm(self, st, src, dst, j, si):
        S, ps, pst = self.S, self.ps, self.pst
        bc = self.bc
        xrot = Rot([self.sb(st, "mx0", [128, KC, TT], BF16)], ntok=4)
        wsl = Rot([self.sb(st, f"mwc{i}", [128, KC, 128], BF16) for i in range(3)])
        wtm = Rot([self.sb(st, f"mwt{i}", [128, KC, 512], BF16) for i in range(2)])
        wg = self.sb(st, "mwg", [128, KC, 8], BF16)
        bg = self.sb(st, "mbg", [128, 8], F32)
        cst = self.sb(st, "mcst", [128, 4, 128], F32)
        onesb = self.sb(st, "monesb", [128, 1], BF16)
        wgtok, bgtok, csttok = Tok(), Tok(), Tok()
        qT = self.sb(st, "mqT", [128, 8, TT], BF16)
        kT = self.sb(st, "mkT", [128, 8, TT], BF16)
        qtok = [Tok() for _ in range(8)]
        ktok = [Tok() for _ in range(8)]
        sog = self.sb(st, "msog", [128, KC, TT], BF16)
        sogtok = [Tok() for _ in range(KC)]
        ktm = self.sb(st, "mktm", [128, 4, 1024], BF16)
        ktmtok = [Tok() for _ in range(4)]
        vtm = self.sb(st, "mvtm", [128, 4, 2048], BF16)
        vtmtok = [Tok() for _ in range(4)]
        C = self.sb(st, "mC", [128, 8, 512], F32)
        Cb = self.sb(st, "mCb", [128, 8, 512], BF16)
        ctok = [Tok() for _ in range(8)]
        cbtok = [Tok() for _ in range(8)]
        nst = self.sb(st, "mn", [128, 8], F32)
        nb = self.sb(st, "mnb", [128, 8], BF16)
        mprev = self.sb(st, "mm", [128, 4], F32)
        sttok = Tok()
        smr = Rot([self.sb(st, f"msm{i}", [128, 20, 4], F32) for i in range(2)])
        Dg = self.sb(st, "mDg", [128, 4, 128], F32)
        dgtok = Tok()
        dmr = Rot([self.sb(st, f"mdm{i}", [128, 4, 128], F32) for i in range(1)])
        wbf = self.sb(st, "mwbf", [128, 4, 128], BF16)
        wbftok = Tok()
        wT = self.sb(st, "mwT", [128, 4, 128], BF16)
        wTtok = Tok()
        Bs = Rot([self.sb(st, f"mBs{i}", [128, 512], F32) for i in range(2)])
        hb = self.sb(st, "mhb", [128, 4, 512], BF16)
        hbtok = Tok()
        kw = self.sb(st, "mkw", [128, 4, 256], BF16)
        kwtok = Tok()
        ident, tri, sel, maskneg = (cst[:, i, :] for i in range(4))
        ps3b = ps[3].bitcast(BF16)
        ps6b, ps7b = ps[6].bitcast(BF16), ps[7].bitcast(BF16)

        S.op("pool", "dma_start", writes=[wgtok], dsem="mg", out=wg[:], in_=self.m_wg[j])
        S.op("sp", "dma_start", writes=[bgtok], dsem="mb", out=bg[:], in_=self.m_bg[j])
        S.op("sp", "dma_start", writes=[csttok], dsem="mc", out=cst[:],
             in_=self.m_const.rearrange("p (a b) -> p a b", b=128))
        S.op("dve", "memset", writes=ctok, ap=C[:], constant=0.0)
        S.op("dve", "memset", writes=cbtok, ap=Cb[:], constant=0.0)
        S.op("dve", "memset", writes=[sttok], ap=nst[:], constant=0.0)
        S.op("dve", "memset", writes=[sttok], ap=nb[:], constant=0.0)
        S.op("dve", "memset", writes=[sttok], ap=mprev[:], constant=0.0)
        S.op("dve", "memset", writes=[csttok], ap=onesb[:], constant=1.0)

        for ti in range(self.NTILE):
            t0 = ti * TT
            if ti == 0:
                nx = xrot.next()
                self.load_xT(nx[0], nx[1], nx[2], src, t0)
            xb, xtok, xk = nx
            pb = 0
            for c in range(32):
                wb, wtok, wk_ = wsl.next()
                if c < 8:
                    srcw = self.m_wq[j * 8 + c]
                elif c < 16:
                    srcw = self.m_wk[j * 8 + (c - 8)]
                else:
                    srcw = self.m_wog[j * KC + (c - 16)]
                S.op("pool", "dma_start", writes=[wtok], dsem=f"mw{wk_}", out=wb[:], in_=srcw)
                bank = pb % 2
                pb += 1
                for k in range(KC):
                    S.op("pe", "matmul", reads=[wtok, xtok[k // 4]], writes=[pst[bank]],
                         out=ps[bank][:], lhsT=wb[:, k, :], rhs=xb[:, k, :], start=(k == 0), stop=(k == KC - 1))
                if c < 8:
                    S.op("act", "copy", reads=[pst[bank]], writes=[qtok[c]], out=qT[:, c, :], in_=ps[bank][:])
                elif c < 16:
                    S.op("act", "copy", reads=[pst[bank]], writes=[ktok[c - 8]], out=kT[:, c - 8, :], in_=ps[bank][:])
                else:
                    S.op("act", "activation", reads=[pst[bank]], writes=[sogtok[c - 16]],
                         out=sog[:, c - 16, :], in_=ps[bank][:], func=AF.Sigmoid)
                self.flush_norm(1)
            self.flush_norm()
            for blk in range(6):
                wb, wtok, wk_ = wtm.next()
                S.op("pool", "dma_start", writes=[wtok], dsem=f"mt{wk_}", out=wb[:], in_=self.m_wtm[j * 6 + blk])
                for ch in range(4):
                    bank = 2 + (blk * 4 + ch) % 2
                    for k in range(KC):
                        S.op("pe", "matmul", reads=[wtok, xtok[k // 4]], writes=[pst[bank]],
                             out=ps[bank][:], lhsT=xb[:, k, ch * 128:(ch + 1) * 128], rhs=wb[:, k, :],
                             start=(k == 0), stop=(k == KC - 1))
                    if blk < 2:
                        S.op("dve", "tensor_copy", reads=[pst[bank]], writes=[ktmtok[ch]],
                             out=ktm[:, ch, blk * 512:(blk + 1) * 512], in_=ps[bank][:])
                    else:
                        S.op("act", "copy", reads=[pst[bank]], writes=[vtmtok[ch]],
                             out=vtm[:, ch, (blk - 2) * 512:(blk - 1) * 512], in_=ps[bank][:])
            for ch in range(4):
                tk = slice(ch * 128, (ch + 1) * 128)
                sm, smtok, _ = smr.next()
                G, ig, fp = sm[:, 0:2, :], sm[:, 0, :], sm[:, 1, :]
                lf, b_, btot, a_, r_ = sm[:, 2, :], sm[:, 3, :], sm[:, 4, :], sm[:, 5, :], sm[:, 6, :]
                mx, mrow, inter, rsw, qn = sm[:, 7, :], sm[:, 8, :], sm[:, 9, :], sm[:, 10, :], sm[:, 11, :]
                den, emm, rdd, mnew, dec = sm[:, 12, :], sm[:, 13, :], sm[:, 14, :], sm[:, 15, :], sm[:, 16, :]
                wk16, tmp = sm[:, 17, :], sm[:, 18, :]
                for k in range(KC):
                    S.op("pe", "matmul", reads=[wgtok, xtok[k // 4]], writes=[pst[0]],
                         out=ps[0][:, 0:8], lhsT=xb[:, k, tk], rhs=wg[:, k, :], start=(k == 0), stop=(k == KC - 1))
                S.op("dve", "tensor_tensor", reads=[pst[0], bgtok], writes=[smtok],
                     out=sm[:, 0:2, :].rearrange("p a b -> p (a b)"), in0=ps[0][:, 0:8], in1=bg[:], op=ALU.add)
                S.op("act", "activation", reads=[smtok], writes=[smtok], out=lf, in_=fp, func=AF.Exp, scale=-1.0)
                S.op("dve", "tensor_scalar_add", reads=[smtok], writes=[smtok], out=lf, in0=lf, scalar1=1.0)
                S.op("act", "activation", reads=[smtok], writes=[smtok], out=lf, in_=lf, func=AF.Ln)
                S.op("dve", "tensor_scalar_mul", reads=[smtok], writes=[smtok], out=lf, in0=lf, scalar1=-1.0)
                S.op("pe", "matmul", reads=[smtok, csttok], writes=[pst[0]],
                     out=ps[0][:, 8:12], lhsT=tri, rhs=lf, start=True, stop=True)
                S.op("pe", "matmul", reads=[smtok, self.ctok], writes=[pst[0]],
                     out=ps[0][:, 12:16], lhsT=self.ones[:], rhs=lf, start=True, stop=True)
                S.op("dve", "tensor_copy", reads=[pst[0]], writes=[smtok],
                     out=sm[:, 3:5, :].rearrange("p a b -> p (a b)"), in_=ps[0][:, 8:16])
                S.op("dve", "tensor_tensor", reads=[smtok, sttok], writes=[smtok], out=a_, in0=b_, in1=mprev[:], op=ALU.add)
                S.op("dve", "tensor_tensor", reads=[smtok], writes=[smtok], out=r_, in0=ig, in1=b_, op=ALU.subtract)
                S.op("dve", "tensor_tensor", reads=[smtok, csttok], writes=[dgtok], out=Dg[:],
                     in0=cst[:, 0:1, :].broadcast_to([128, 4, 128]), in1=bc(r_, 128), op=ALU.mult)
                S.op("pe", "matmul", reads=[dgtok, self.ctok], writes=[pst[1]],
                     out=ps[1][:], lhsT=self.ones[:], rhs=Dg[:].rearrange("p a b -> p (a b)"), start=True, stop=True)
                dm, dmtok, _ = dmr.next()
                S.op("dve", "tensor_tensor", reads=[pst[1], smtok], writes=[dmtok], out=dm[:],
                     in0=ps[1][:].rearrange("p (a b) -> p a b", b=128), in1=bc(b_, 128), op=ALU.add)
                S.op("dve", "tensor_tensor", reads=[csttok], writes=[dmtok], out=dm[:], in0=dm[:],
                     in1=cst[:, 3:4, :].broadcast_to([128, 4, 128]), op=ALU.add)
                S.op("dve", "tensor_reduce", reads=[dmtok], writes=[smtok], out=mx, in_=dm[:], axis=AX.X, op=ALU.max)
                S.op("dve", "tensor_tensor", reads=[smtok], writes=[smtok], out=mrow, in0=mx, in1=a_, op=ALU.max)
                S.op("dve", "tensor_tensor", reads=[smtok], writes=[dmtok], out=dm[:], in0=dm[:], in1=bc(mrow, 128),
                     op=ALU.subtract)
                S.op("act", "activation", reads=[dmtok], writes=[dmtok], out=dm[:], in_=dm[:], func=AF.Exp)
                S.op("dve", "tensor_tensor", reads=[smtok], writes=[smtok], out=inter, in0=a_, in1=mrow, op=ALU.subtract)
                S.op("act", "activation", reads=[smtok], writes=[smtok], out=inter, in_=inter, func=AF.Exp)
                for h in range(4):
                    for dc in range(2):
                        S.op("pe", "matmul", reads=[qtok[2 * h + dc], ktok[2 * h + dc]], writes=[pst[2]],
                             out=ps[2][:, h * 128:(h + 1) * 128], lhsT=qT[:, 2 * h + dc, tk], rhs=kT[:, 2 * h + dc, tk],
                             start=(dc == 0), stop=(dc == 1))
                S.op("dve", "scalar_tensor_tensor", reads=[pst[2]], writes=[dmtok], out=dm[:],
                     in0=ps[2][:].rearrange("p (a b) -> p a b", b=128), scalar=1.0 / 16.0, in1=dm[:],
                     op0=ALU.mult, op1=ALU.mult)
                S.op("dve", "tensor_reduce", reads=[dmtok], writes=[smtok], out=rsw, in_=dm[:], axis=AX.X, op=ALU.add)
                S.op("act", "copy", reads=[dmtok], writes=[wbftok], out=wbf[:], in_=dm[:])
                for h in range(4):
                    S.op("pe", "transpose", reads=[wbftok, self.itok], writes=[pst[3]],
                         out=ps3b[:, h * 128:(h + 1) * 128], in_=wbf[:, h, :], identity=self.ident[:])
                S.op("act", "copy", reads=[pst[3]], writes=[wTtok], out=wT[:].rearrange("p a b -> p (a b)"),
                     in_=ps3b[:, 0:512])
                for h in range(4):
                    for dc in range(2):
                        S.op("pe", "matmul", reads=[qtok[2 * h + dc], sttok], writes=[pst[0]],
                             out=ps[0][:, 16 + h:17 + h], lhsT=qT[:, 2 * h + dc, tk], rhs=nb[:, 2 * h + dc:2 * h + dc + 1],
                             start=(dc == 0), stop=(dc == 1))
                S.op("dve", "tensor_copy", reads=[pst[0]], writes=[smtok], out=qn, in_=ps[0][:, 16:20])
                S.op("dve", "tensor_tensor", reads=[smtok], writes=[smtok], out=den, in0=inter, in1=qn, op=ALU.mult)
                S.op("dve", "tensor_tensor", reads=[smtok], writes=[smtok], out=den, in0=den, in1=rsw, op=ALU.add)
                S.op("dve", "tensor_scalar_mul", reads=[smtok], writes=[smtok], out=tmp, in0=den, scalar1=-1.0)
                S.op("dve", "tensor_tensor", reads=[smtok], writes=[smtok], out=den, in0=den, in1=tmp, op=ALU.max)
                S.op("act", "activation", reads=[smtok], writes=[smtok], out=emm, in_=mrow, func=AF.Exp, scale=-1.0)
                S.op("dve", "tensor_tensor", reads=[smtok], writes=[smtok], out=den, in0=den, in1=emm, op=ALU.max)
                S.op("dve", "reciprocal", reads=[smtok], writes=[smtok], out=rdd, in_=den)
                S.op("dve", "tensor_tensor", reads=[smtok], writes=[smtok], out=sm[:, 19, :], in0=inter, in1=rdd,
                     op=ALU.mult)
                for h in range(4):
                    for dc in range(2):
                        S.op("pe", "matmul", reads=[qtok[2 * h + dc], cbtok[2 * h + dc]], writes=[pst[4]],
                             out=ps[4][:], lhsT=qT[:, 2 * h + dc, tk], rhs=Cb[:, 2 * h + dc, :],
                             start=(dc == 0), stop=(dc == 1))
                    S.op("pe", "matmul", reads=[wTtok, vtmtok[ch]], writes=[pst[5]],
                         out=ps[5][:], lhsT=wT[:, h, :], rhs=vtm[:, ch, h * 512:(h + 1) * 512], start=True, stop=True)
                    bsb, bstok, _ = Bs.next()
                    S.op("act", "activation", reads=[pst[5], smtok], writes=[bstok], out=bsb[:], in_=ps[5][:],
                         func=AF.Identity, scale=sm[:, 14, h:h + 1])
                    S.op("dve", "scalar_tensor_tensor", reads=[pst[4], bstok, smtok], writes=[hbtok],
                         out=hb[:, h, :], in0=ps[4][:], scalar=sm[:, 19, h:h + 1], in1=bsb[:],
                         op0=ALU.mult, op1=ALU.add)
                for half in range(2):
                    pbv = ps6b if half == 0 else ps7b
                    for i in range(8):
                        c = half * 8 + i
                        S.op("pe", "transpose", reads=[hbtok, self.itok], writes=[pst[6 + half]],
                             out=pbv[:, i * 128:(i + 1) * 128], in_=hb[:, c // 4, (c % 4) * 128:(c % 4 + 1) * 128],
                             identity=self.ident[:])
                    S.op("dve", "tensor_tensor", reads=[pst[6 + half]], writes=sogtok[half * 8:half * 8 + 8],
                         out=sog[:, half * 8:half * 8 + 8, tk], in0=pbv[:].rearrange("p (a b) -> p a b", b=128),
                         in1=sog[:, half * 8:half * 8 + 8, tk], op=ALU.mult)
                S.op("pe", "matmul", reads=[smtok, csttok], writes=[pst[0]],
                     out=ps[0][:, 20:24], lhsT=sel, rhs=mrow, start=True, stop=True)
                S.op("dve", "tensor_copy", reads=[pst[0]], writes=[smtok], out=mnew, in_=ps[0][:, 20:24])
                S.op("dve", "tensor_tensor", reads=[smtok], writes=[smtok], out=tmp, in0=btot, in1=mnew, op=ALU.subtract)
                S.op("dve", "tensor_tensor", reads=[smtok, sttok], writes=[smtok], out=dec, in0=tmp, in1=mprev[:], op=ALU.add)
                S.op("act", "activation", reads=[smtok], writes=[smtok], out=dec, in_=dec, func=AF.Exp)
                S.op("dve", "tensor_tensor", reads=[smtok], writes=[smtok], out=wk16, in0=tmp, in1=r_, op=ALU.add)
                S.op("act", "activation", reads=[smtok], writes=[smtok], out=wk16, in_=wk16, func=AF.Exp)
                S.op("dve", "tensor_scalar_mul", reads=[smtok], writes=[smtok], out=wk16, in0=wk16, scalar1=1.0 / 16.0)
                S.op("dve", "tensor_tensor", reads=[ktmtok[ch], smtok], writes=[kwtok], out=kw[:],
                     in0=ktm[:, ch, :].rearrange("p (a b) -> p a b", b=256), in1=bc(wk16, 256), op=ALU.mult)
                for h in range(4):
                    for dc in range(2):
                        jj = 2 * h + dc
                        bank = 1 + jj % 2
                        S.op("pe", "matmul", reads=[kwtok, vtmtok[ch]], writes=[pst[bank]],
                             out=ps[bank][:], lhsT=kw[:, h, dc * 128:(dc + 1) * 128],
                             rhs=vtm[:, ch, h * 512:(h + 1) * 512], start=True, stop=True)
                        S.op("dve", "scalar_tensor_tensor", reads=[pst[bank], smtok], writes=[ctok[jj]],
                             out=C[:, jj, :], in0=C[:, jj, :], scalar=sm[:, 16, h:h + 1], in1=ps[bank][:],
                             op0=ALU.mult, op1=ALU.add)
                        S.op("act", "copy", reads=[ctok[jj]], writes=[cbtok[jj]], out=Cb[:, jj, :], in_=C[:, jj, :])
                for h in range(4):
                    for dc in range(2):
                        jj = 2 * h + dc
                        S.op("pe", "matmul", reads=[kwtok, csttok], writes=[pst[0]],
                             out=ps[0][:, 24 + jj:25 + jj], lhsT=kw[:, h, dc * 128:(dc + 1) * 128], rhs=onesb[:],
                             start=True, stop=True)
                S.op("dve", "tensor_tensor", reads=[smtok], writes=[sttok],
                     out=nst[:].rearrange("p (a b) -> p a b", b=2), in0=nst[:].rearrange("p (a b) -> p a b", b=2),
                     in1=bc(dec, 2), op=ALU.mult)
                S.op("dve", "tensor_tensor", reads=[pst[0]], writes=[sttok], out=nst[:], in0=nst[:],
                     in1=ps[0][:, 24:32], op=ALU.add)
                S.op("act", "copy", reads=[sttok], writes=[sttok], out=nb[:], in_=nst[:])
                S.op("dve", "tensor_copy", reads=[smtok], writes=[sttok], out=mprev[:], in_=mnew)

            if ti + 1 < self.NTILE:
                nx = xrot.next()
                self.load_xT(nx[0], nx[1], nx[2], src, (ti + 1) * TT)

            def lhs_of(c):
                wb, wtok, wk_ = wsl.next()
                S.op("pool", "dma_start", writes=[wtok], dsem=f"mw{wk_}", out=wb[:], in_=self.wo_m[j * KC + c])
                return (lambda fc: wb[:, fc, :]), wtok

            self.tail(t0, src, dst, si, 1.0 / ALPHA, KC, lhs_of, lambda fc: sog[:, fc, :], sogtok)


def full_plan():
    plan = []
    for l in range(DEPTH):
        plan.append(("ffn", 2 * l))
        plan.append(("att", l // 2) if l % 2 == 0 else ("mlstm", l // 2))
        plan.append(("ffn", 2 * l + 1))
    return plan


def lay_w13(w):
    n = w.shape[0]
    return np.ascontiguousarray(
        w.reshape(n, KC, 128, NFB, FB).transpose(0, 3, 2, 1, 4)).reshape(n * NFB, 128, KC, FB)


def lay_w2(w):
    n = w.shape[0]
    return np.ascontiguousarray(
        w.reshape(n, FCH, 128, KC, 128).transpose(0, 3, 2, 1, 4)).reshape(n * KC, 128, FCH, 128)


def lay_ln(v):
    n = v.shape[0]
    return np.ascontiguousarray(v.reshape(n, KC, 128).transpose(2, 0, 1)).reshape(128, n * KC)


def lay_kchunks(w, width):
    n = w.shape[1] // width
    return np.ascontiguousarray(w.reshape(KC, 128, n, width).transpose(2, 1, 0, 3))


def att_layouts(w_qkv, sinks, w_o):
    n = w_qkv.shape[0]
    wq = np.concatenate([lay_kchunks(w_qkv[i][:, :2048], 128) for i in range(n)], axis=0)
    wk = []
    for i in range(n):
        kk = w_qkv[i][:, 2048:2304].reshape(D, 4, 1, 64)
        z = np.zeros_like(kk)
        kk2 = np.concatenate([np.concatenate([kk, z], axis=3), np.concatenate([z, kk], axis=3)], axis=2)
        wk.append(lay_kchunks(kk2.reshape(D, 8 * 128), 128))
    wk = np.concatenate(wk, axis=0)
    wv = np.concatenate([lay_kchunks(w_qkv[i][:, 2304:2560], 256) for i in range(n)], axis=0)
    wo = np.concatenate([lay_kchunks(w_o[i], 128) for i in range(n)], axis=0)
    sk = np.ascontiguousarray(np.broadcast_to(sinks[:, None, :], (n, 128, 32))).astype(np.float32)
    return {"wq": wq, "wk": wk, "wv": wv, "wo_a": wo, "sinks": sk}


def ml_layouts(w_in, b_gates, w_o):
    n = w_in.shape[0]
    cat = lambda f: np.concatenate([f(i) for i in range(n)], axis=0)
    return {
        "m_wq": cat(lambda i: lay_kchunks(w_in[i][:, 0:1024], 128)),
        "m_wk": cat(lambda i: lay_kchunks(w_in[i][:, 1024:2048], 128)),
        "m_wtm": cat(lambda i: lay_kchunks(w_in[i][:, 1024:4096], 512)),
        "m_wog": cat(lambda i: lay_kchunks(w_in[i][:, 4096:6144], 128)),
        "m_wg": cat(lambda i: lay_kchunks(w_in[i][:, 6144:6152], 8)),
        "m_bg": np.ascontiguousarray(np.broadcast_to(b_gates[:, None, :], (n, 128, 8))).astype(np.float32),
        "wo_m": cat(lambda i: lay_kchunks(w_o[i], 128)),
    }


def ml_consts():
    i = np.arange(128)
    ident = np.eye(128, dtype=np.float32)
    tri = (i[:, None] <= i[None, :]).astype(np.float32)
    sel = np.zeros((128, 128), np.float32)
    sel[127, :] = 1.0
    mask = np.where(i[None, :] <= i[:, None], 0.0, NEG).astype(np.float32)
    return np.ascontiguousarray(np.concatenate([ident, tri, sel, mask], axis=1))


def alibi_table():
    q = np.arange(128)[:, None]
    jj = np.arange(256)[None, :]
    dist = (128 + q - jj).astype(np.float32)
    valid = (dist >= 0) & (dist < 128)
    slopes = (2.0 ** (-8.0 * np.arange(1, 33, dtype=np.float32) / 32)).astype(np.float32)
    tab = np.where(valid[:, None, :], -slopes[None, :, None] * dist[:, None, :], np.float32(NEG))
    return np.ascontiguousarray(tab.astype(np.float32).reshape(128, 32 * 256))


def prepare_weights(ffn_w1, ffn_w3, ffn_w2, ln_g, ln_b, att_w_qkv, att_sinks, att_w_o,
                    mlstm_w_in, mlstm_b_gates, mlstm_w_o):
    f32 = lambda a: np.asarray(a, dtype=np.float32)
    w = {
        "w1": lay_w13(f32(ffn_w1).reshape(2 * DEPTH, D, FF)),
        "w3": lay_w13(f32(ffn_w3).reshape(2 * DEPTH, D, FF)),
        "w2": lay_w2(f32(ffn_w2).reshape(2 * DEPTH, FF, D)),
        "ln_g": lay_ln(f32(ln_g).reshape(3 * DEPTH, D)),
        "ln_b": lay_ln(f32(ln_b).reshape(3 * DEPTH, D)),
        "alibi": alibi_table(),
        "identd": np.eye(128, dtype=np.float32),
        "m_const": ml_consts(),
    }
    w.update(att_layouts(f32(att_w_qkv), f32(att_sinks), f32(att_w_o)))
    w.update(ml_layouts(f32(mlstm_w_in), f32(mlstm_b_gates), f32(mlstm_w_o)))
    return w


def run_module(xs, weights, s_tok):
    n = len(xs)
    b = Builder(s_tok, full_plan(), 2 * DEPTH, DEPTH // 2, DEPTH // 2)
    nc = b.build()
    in_maps = []
    for i in range(n):
        m = dict(weights)
        m["xT"] = np.ascontiguousarray(np.asarray(xs[i], dtype=np.float32).T)
        in_maps.append(m)
    res = run_bass_kernel_spmd(nc, in_maps, core_ids=list(range(n)))
    return [np.ascontiguousarray(r["outT"].T) for r in res.results]


def kernel(x, ffn_w1, ffn_w3, ffn_w2, ln_g, ln_b, att_w_qkv, att_sinks, att_w_o,
           mlstm_w_in, mlstm_b_gates, mlstm_w_o):
    x = np.asarray(x, dtype=np.float32)
    weights = prepare_weights(ffn_w1, ffn_w3, ffn_w2, ln_g, ln_b, att_w_qkv, att_sinks, att_w_o,
                              mlstm_w_in, mlstm_b_gates, mlstm_w_o)
    outs = run_module([x[i] for i in range(x.shape[0])], weights, x.shape[1])
    return np.stack(outs, axis=0).astype(np.float32)
```

```python
import contextlib
import numpy as np
import concourse.bass as bass
import concourse.mybir as mybir
from concourse.bass_utils import run_bass_kernel_spmd

F32 = mybir.dt.float32
BF16 = mybir.dt.bfloat16
AF = mybir.ActivationFunctionType
ALU = mybir.AluOpType
AX = mybir.AxisListType

D = 2048
FF = 5632
KC = D // 128
FCH = FF // 128
TT = 512
FB = 256
NFB = FF // FB
DEPTH = 4
ALPHA = (2.0 * DEPTH) ** 0.25
EPS_S = 1e-5 / (ALPHA * ALPHA)
NEG = -30000.0

ENGS = ("pe", "act", "dve", "pool", "sp")


class Tok:
    __slots__ = ("w", "r", "rd")

    def __init__(self):
        self.w = None
        self.r = {}
        self.rd = []


class Op:
    __slots__ = ("eng", "fn", "deps", "dsem", "dcount", "need_sig", "sig")

    def __init__(self, eng, fn, deps, dsem):
        self.eng = eng
        self.fn = fn
        self.deps = deps
        self.dsem = dsem
        self.dcount = 0
        self.need_sig = False
        self.sig = 0


class Sched:
    def __init__(self, nc):
        self.nc = nc
        self.ops = []
        self.streams = {e: [] for e in ENGS}
        self.dsem_counts = {}
        self.last = {e: None for e in ENGS}
        self.pending_dma = []
        self.cost = {}

    def op(self, eng, meth, reads=(), writes=(), dsem=None, **kw):
        i = self.add(eng, (meth, kw), reads, writes, dsem)
        self.cost[i] = self._cost(eng, meth, kw)
        return i

    @staticmethod
    def _nfree(ap):
        n = 1
        for d in ap.shape[1:]:
            n *= d
        return n

    def _cost(self, eng, meth, kw):
        try:
            if meth == "dma_start":
                o, i_ = kw["out"], kw["in_"]
                b = max(self._nfree(o) * o.shape[0] * mybir.dt.size(o.dtype),
                        self._nfree(i_) * i_.shape[0] * mybir.dt.size(i_.dtype))
                return ("dma", b / 300.0)
            if meth == "matmul":
                n = self._nfree(kw["out"])
                f = 4.0 if kw["lhsT"].dtype == F32 else 1.0
                return ("c", 15.0 + f * n / 2.4)
            if meth == "transpose":
                return ("c", 110.0)
            src = kw.get("in_", kw.get("in0", kw.get("ap")))
            n = self._nfree(src)
            if eng == "act":
                return ("c", 220.0 + 1.2 * n + (100.0 if "accum_out" in kw else 0.0))
            per = 1.04
            for k in ("in_", "in0", "in1"):
                a = kw.get(k)
                if a is not None and hasattr(a, "tensor") and type(a.tensor).__name__ == "PSumTensorHandle":
                    per = 1.35
            if meth == "reciprocal":
                per = 6.5
            if meth == "memset":
                per = 0.5
            return ("c", 70.0 + per * n)
        except Exception:
            return ("c", 500.0)

    def reschedule(self, lo, hi):
        import heapq
        ops, cost = self.ops, self.cost
        idx = [i for i in range(lo, hi) if ops[i].fn is not None]
        if not idx:
            return
        inseg = set(idx)
        ndep = {}
        users = {}
        for i in idx:
            ds = [j for j in ops[i].deps if j in inseg]
            ndep[i] = len(ds)
            for j in ds:
                users.setdefault(j, []).append(i)
        fin = {}
        efree = {e: 0.0 for e in ENGS}
        dma_free = 0.0
        SYNC = 180.0
        ready = [i for i in idx if ndep[i] == 0]
        order = []
        while ready:
            best, bstart = None, None
            for i in ready:
                op = ops[i]
                t = efree[op.eng]
                for j in op.deps:
                    if j in fin:
                        tj = fin[j] + (0.0 if (ops[j].eng == op.eng and ops[j].dsem is None) else SYNC)
                        if tj > t:
                            t = tj
                if bstart is None or t < bstart - 1e-9 or (abs(t - bstart) <= 1e-9 and i < best):
                    best, bstart = i, t
            i = best
            ready.remove(i)
            op = ops[i]
            kind, dur = cost.get(i, ("c", 500.0))
            if kind == "dma":
                efree[op.eng] = bstart + 60.0
                st = max(bstart + 60.0, dma_free)
                dma_free = st + dur
                fin[i] = dma_free + 1800.0
            else:
                efree[op.eng] = bstart + dur
                fin[i] = bstart + dur
            order.append(i)
            for u in users.get(i, ()):
                ndep[u] -= 1
                if ndep[u] == 0:
                    ready.append(u)
        assert len(order) == len(idx)
        pos = {i: k for k, i in enumerate(order)}
        for e in ENGS:
            st = self.streams[e]
            seg = [i for i in st if i in inseg]
            if not seg:
                continue
            first = st.index(seg[0])
            seg_sorted = sorted(seg, key=lambda i: pos[i])
            assert st[first:first + len(seg)] == seg
            st[first:first + len(seg)] = seg_sorted

    def add(self, eng, fn, reads=(), writes=(), dsem=None, extra_deps=()):
        i = len(self.ops)
        deps = set(extra_deps)
        for t in reads:
            if t.w is not None:
                deps.add(t.w)
        for t in writes:
            if t.w is not None:
                deps.add(t.w)
            deps.update(t.r.values())
            deps.update(t.rd)
        for t in reads:
            if dsem is not None:
                t.rd.append(i)
            else:
                t.r[eng] = i
        for t in writes:
            t.w = i
            t.r = {}
            t.rd = []
        if dsem is not None:
            dsem = eng + "_" + dsem
        op = Op(eng, fn, deps, dsem)
        if dsem is not None:
            c = self.dsem_counts.get(dsem, 0) + 16
            self.dsem_counts[dsem] = c
            op.dcount = c
            self.pending_dma.append(i)
        self.ops.append(op)
        self.streams[eng].append(i)
        if fn is not None:
            self.last[eng] = i
        return i

    def barrier(self, dmas=True):
        deps = set(self.pending_dma) if dmas else set()
        for e in ENGS:
            if self.last[e] is not None:
                deps.add(self.last[e])
        if dmas:
            self.pending_dma = []
        for e in ENGS:
            self.add(e, None, extra_deps=deps)

    def _skip(self, d, op):
        return d.eng == op.eng and op.dsem is None and d.eng == "pe"

    def emit(self, stack):
        nc = self.nc
        ops = self.ops
        for op in ops:
            for j in op.deps:
                d = ops[j]
                if d.dsem is not None or self._skip(d, op):
                    continue
                d.need_sig = True
        for e in ENGS:
            cnt = 0
            dlast = {}
            for i in self.streams[e]:
                op = ops[i]
                if op.dsem is None:
                    if op.need_sig:
                        cnt += 1
                        op.sig = cnt
                else:
                    dlast[op.dsem] = dlast.get(op.dsem, 0) + 16
                    op.dcount = dlast[op.dsem]
        esem = {e: stack.enter_context(nc.semaphore("s_" + e)) for e in ENGS}
        dsem = {k: stack.enter_context(nc.semaphore("d_" + k)) for k in self.dsem_counts}
        engobj = {"pe": "tensor", "act": "scalar", "dve": "vector", "pool": "gpsimd", "sp": "sync"}

        def run_stream(e, eng):
            waited = {}
            for i in self.streams[e]:
                op = ops[i]
                need = {}
                for j in op.deps:
                    d = ops[j]
                    if d.dsem is not None:
                        key = ("d", d.dsem)
                        val = d.dcount
                    else:
                        if self._skip(d, op):
                            continue
                        key = ("e", d.eng)
                        val = d.sig
                    if need.get(key, 0) < val:
                        need[key] = val
                for key, val in need.items():
                    if waited.get(key, 0) >= val:
                        continue
                    waited[key] = val
                    sem = dsem[key[1]] if key[0] == "d" else esem[key[1]]
                    eng.wait_ge(sem, val)
                if op.fn is None:
                    continue
                ins = getattr(eng, op.fn[0])(**op.fn[1])
                if op.dsem is not None:
                    ins.then_inc(dsem[op.dsem], 16)
                elif op.need_sig:
                    ins.then_inc(esem[e], 1)

        with nc.Block() as block:
            for e in ENGS:
                if not self.streams[e]:
                    continue

                def body(eng, e=e):
                    run_stream(e, eng)
                getattr(block, engobj[e])(body)


class Rot:
    def __init__(self, bufs, ntok=None):
        self.bufs = bufs
        self.toks = [Tok() if ntok is None else [Tok() for _ in range(ntok)] for _ in bufs]
        self.i = 0

    def next(self):
        k = self.i % len(self.bufs)
        self.i += 1
        return self.bufs[k], self.toks[k], k


class Builder:
    def __init__(self, s_tok, plan, n_ffn, n_att, n_ml, resched=("mlstm", "att")):
        self.resched = set(resched)
        self.S_TOK = s_tok
        self.NTILE = s_tok // TT
        self.plan = plan
        nc = self.nc = bass.Bass("TRN2", target_bir_lowering=False)
        self.S = Sched(nc)
        dt = nc.dram_tensor
        self.xT = dt("xT", [D, s_tok], F32, kind="ExternalInput").ap()
        self.outT = dt("outT", [D, s_tok], F32, kind="ExternalOutput").ap()
        self.scr = [dt("scrA", [D, s_tok], F32, kind="Internal").ap(),
                    dt("scrB", [D, s_tok], F32, kind="Internal").ap()]
        nsb = len(plan)
        self.ln_g = dt("ln_g", [128, nsb * KC], F32, kind="ExternalInput").ap()
        self.ln_b = dt("ln_b", [128, nsb * KC], F32, kind="ExternalInput").ap()
        if n_ffn:
            self.w1 = dt("w1", [n_ffn * NFB, 128, KC, FB], F32, kind="ExternalInput").ap()
            self.w3 = dt("w3", [n_ffn * NFB, 128, KC, FB], F32, kind="ExternalInput").ap()
            self.w2 = dt("w2", [n_ffn * KC, 128, FCH, 128], F32, kind="ExternalInput").ap()
        if n_att:
            self.wq = dt("wq", [n_att * KC, 128, KC, 128], F32, kind="ExternalInput").ap()
            self.wk = dt("wk", [n_att * 8, 128, KC, 128], F32, kind="ExternalInput").ap()
            self.wv = dt("wv", [n_att, 128, KC, 256], F32, kind="ExternalInput").ap()
            self.wo_a = dt("wo_a", [n_att * KC, 128, KC, 128], F32, kind="ExternalInput").ap()
            self.sinks = dt("sinks", [n_att, 128, 32], F32, kind="ExternalInput").ap()
            self.alibi = dt("alibi", [128, 32 * 256], F32, kind="ExternalInput").ap()
            self.identd = dt("identd", [128, 128], F32, kind="ExternalInput").ap()
        if n_ml:
            self.m_wq = dt("m_wq", [n_ml * 8, 128, KC, 128], F32, kind="ExternalInput").ap()
            self.m_wk = dt("m_wk", [n_ml * 8, 128, KC, 128], F32, kind="ExternalInput").ap()
            self.m_wtm = dt("m_wtm", [n_ml * 6, 128, KC, 512], F32, kind="ExternalInput").ap()
            self.m_wog = dt("m_wog", [n_ml * KC, 128, KC, 128], F32, kind="ExternalInput").ap()
            self.m_wg = dt("m_wg", [n_ml, 128, KC, 8], F32, kind="ExternalInput").ap()
            self.m_bg = dt("m_bg", [n_ml, 128, 8], F32, kind="ExternalInput").ap()
            self.wo_m = dt("wo_m", [n_ml * KC, 128, KC, 128], F32, kind="ExternalInput").ap()
            self.m_const = dt("m_const", [128, 4 * 128], F32, kind="ExternalInput").ap()
            if not hasattr(self, "identd"):
                self.identd = dt("identd", [128, 128], F32, kind="ExternalInput").ap()

    def dtok(self, ap, ti, c):
        if ap is self.xT:
            return []
        tab = self.__dict__.setdefault("_dtoks", {}).setdefault(
            id(ap), [[Tok() for _ in range(KC)] for _ in range(self.NTILE)])
        return [tab[ti][c]]

    def sb(self, st, name, shape, dtype):
        self.uid = getattr(self, "uid", 0) + 1
        return st.enter_context(self.nc.sbuf_tensor(f"{name}_u{self.uid}", shape, dtype))

    def build(self):
        nc, S = self.nc, self.S
        with contextlib.ExitStack() as top:
            self.ps = [top.enter_context(nc.psum_tensor(f"ps{i}", [128, 512], F32)) for i in range(8)]
            self.pst = [Tok() for _ in range(8)]
            nsb = len(self.plan)
            self.gcol = self.sb(top, "gcol", [128, nsb * KC], F32)
            self.bcol = self.sb(top, "bcol", [128, nsb * KC], F32)
            self.ones = self.sb(top, "ones", [128, 128], F32)
            self.ctok = Tok()
            self.gtok = Tok()
            self.btok = Tok()
            S.op("sp", "dma_start", writes=[self.gtok], dsem="cg", out=self.gcol[:], in_=self.ln_g)
            S.op("sp", "dma_start", writes=[self.btok], dsem="cb", out=self.bcol[:], in_=self.ln_b)
            S.op("dve", "memset", writes=[self.ctok], ap=self.ones[:], constant=1.0)
            self.onesbf = self.sb(top, "onesbf", [128, 128], BF16)
            S.op("dve", "memset", writes=[self.ctok], ap=self.onesbf[:], constant=1.0)
            self.y = self.sb(top, "y", [128, KC, TT], F32)
            self.ytok = [Tok() for _ in range(KC)]
            self.sq = Rot([self.sb(top, f"sq{i}", [128, 2, TT], BF16) for i in range(2)])
            self.ost = Rot([self.sb(top, f"ost{i}", [128, TT], F32) for i in range(2)])
            self.stat = self.sb(top, "stat", [128, 3, TT], F32)
            self.stattok = Tok()
            self.setup_consts(top)
            S.barrier()
            nsub = len(self.plan)
            for si, sub in enumerate(self.plan):
                src = self.xT if si == 0 else self.scr[(si - 1) % 2]
                dst = self.outT if si == nsub - 1 else self.scr[si % 2]
                seg_lo = len(S.ops)
                with contextlib.ExitStack() as st:
                    if sub[0] == "ffn":
                        self.ffn(st, src, dst, sub[1], si)
                    elif sub[0] == "att":
                        self.att(st, src, dst, sub[1], si)
                    elif sub[0] == "mlstm":
                        self.mlstm(st, src, dst, sub[1], si)
                    last = (si == nsub - 1)
                    if last:
                        self.flush_norm()
                    if sub[0] in self.resched:
                        S.reschedule(seg_lo, len(S.ops))
                    if si < 3:
                        self.sbuf_left = getattr(self, "sbuf_left", {})
                        self.sbuf_left[sub[0]] = nc.sbuf_bytes_remaining
                    S.barrier(dmas=last)
            S.emit(top)
        return nc

    def tail(self, t0, src, dst, si, coef, n_fc, lhs_of, hT_of, htoks):
        S, ps, pst = self.S, self.ps, self.pst
        y, ytok = self.y, self.ytok
        srcv = src.rearrange("(c p) t -> c p t", p=128)
        dstv = dst.rearrange("(c p) t -> c p t", p=128)
        S1, S2 = 6, 7
        pend = None

        def stats(c):
            sqb, sqt, _ = self.sq.next()
            S.op("act", "copy", reads=[ytok[c]], writes=[sqt], out=sqb[:, 0, :], in_=y[:, c, :])
            S.op("act", "activation", reads=[ytok[c]], writes=[sqt], out=sqb[:, 1, :], in_=y[:, c, :], func=AF.Square)
            S.op("pe", "matmul", reads=[sqt, self.ctok], writes=[pst[S1]],
                 out=ps[S1][:], lhsT=self.onesbf[:], rhs=sqb[:, 0, :], start=(c == 0), stop=(c == KC - 1))
            S.op("pe", "matmul", reads=[sqt, self.ctok], writes=[pst[S2]],
                 out=ps[S2][:], lhsT=self.onesbf[:], rhs=sqb[:, 1, :], start=(c == 0), stop=(c == KC - 1))

        for c in range(KC):
            S.op("sp", "dma_start", reads=self.dtok(src, t0 // TT, c), writes=[ytok[c]], dsem=f"xr{c}",
                 out=y[:, c, :], in_=srcv[c, :, t0:t0 + TT])
            lhs, wtok = lhs_of(c)
            bank = 4 + (c % 2)
            for fc in range(n_fc):
                S.op("pe", "matmul", reads=[wtok, htoks[fc]], writes=[pst[bank]],
                     out=ps[bank][:], lhsT=lhs(fc), rhs=hT_of(fc), start=(fc == 0), stop=(fc == n_fc - 1))
            if pend is not None:
                stats(pend)
            S.op("dve", "scalar_tensor_tensor", reads=[pst[bank]], writes=[ytok[c]],
                 out=y[:, c, :], in0=ps[bank][:], scalar=coef, in1=y[:, c, :], op0=ALU.mult, op1=ALU.add)
            pend = c
        stats(pend)
        st_, stt = self.stat, self.stattok
        S.op("dve", "tensor_scalar_mul", reads=[pst[S1]], writes=[stt],
             out=st_[:, 0, :], in0=ps[S1][:], scalar1=1.0 / D)
        S.op("dve", "tensor_tensor", reads=[stt], writes=[stt],
             out=st_[:, 2, :], in0=st_[:, 0, :], in1=st_[:, 0, :], op=ALU.mult)
        S.op("dve", "scalar_tensor_tensor", reads=[pst[S2], stt], writes=[stt],
             out=st_[:, 1, :], in0=ps[S2][:], scalar=1.0 / D, in1=st_[:, 2, :], op0=ALU.mult, op1=ALU.subtract)
        S.op("dve", "tensor_scalar_add", reads=[stt], writes=[stt],
             out=st_[:, 1, :], in0=st_[:, 1, :], scalar1=EPS_S)
        S.op("act", "sqrt", reads=[stt], writes=[stt], out=st_[:, 1, :], in_=st_[:, 1, :])
        S.op("dve", "reciprocal", reads=[stt], writes=[stt], out=st_[:, 1, :], in_=st_[:, 1, :])
        def norm_step(c):
            S.op("dve", "tensor_tensor", reads=[stt], writes=[ytok[c]],
                 out=y[:, c, :], in0=y[:, c, :], in1=st_[:, 0, :], op=ALU.subtract)
            S.op("dve", "tensor_tensor", reads=[stt], writes=[ytok[c]],
                 out=y[:, c, :], in0=y[:, c, :], in1=st_[:, 1, :], op=ALU.mult)
            ob, ot, ok = self.ost.next()
            col = si * KC + c
            S.op("act", "activation", reads=[ytok[c], self.gtok, self.btok], writes=[ot],
                 out=ob[:], in_=y[:, c, :], func=AF.Identity,
                 bias=self.bcol[:, col:col + 1], scale=self.gcol[:, col:col + 1])
            S.op("sp", "dma_start", reads=[ot], writes=self.dtok(dst, t0 // TT, c), dsem=f"st{ok}",
                 out=dstv[c, :, t0:t0 + TT], in_=ob[:])

        self.pending_norm = [(lambda c=c: norm_step(c)) for c in range(KC)]

    def flush_norm(self, n=None):
        pn = getattr(self, "pending_norm", [])
        k = len(pn) if n is None else min(n, len(pn))
        for f in pn[:k]:
            f()
        self.pending_norm = pn[k:]

    def load_xT(self, xb, xtoks, xk, src, t0, ntok=TT, off=0):
        srcv = src.rearrange("(c p) t -> p c t", p=128)
        for q in range(4):
            rd = [t for c in range(4 * q, 4 * q + 4) for t in self.dtok(src, t0 // TT, c)]
            self.S.op("pool", "dma_start", reads=rd, writes=[xtoks[q]], dsem=f"x{xk}_{q}",
                      out=xb[:, 4 * q:4 * q + 4, off:off + ntok], in_=srcv[:, 4 * q:4 * q + 4, t0:t0 + ntok])

    def ffn(self, st, src, dst, widx, si):
        S, ps, pst = self.S, self.ps, self.pst
        xrot = Rot([self.sb(st, f"fx{i}", [128, KC, TT], BF16) for i in range(2)], ntok=4)
        hT = self.sb(st, "hT", [128, FCH, TT], BF16)
        htoks = [Tok() for _ in range(FCH)]
        w13 = Rot([self.sb(st, f"w13_{i}", [128, 2, KC, FB], BF16) for i in range(2)], ntok=2)
        w2r = Rot([self.sb(st, f"w2_{i}", [128, FCH, 128], BF16) for i in range(3)])
        sg = Rot([self.sb(st, f"sg{i}", [128, TT], F32) for i in range(2)])
        def issue_w13(fb):
            wb, wtok, wk = w13.next()
            S.op("pool", "dma_start", writes=[wtok[0]], dsem=f"w13_{wk}_0", out=wb[:, 0], in_=self.w1[widx * NFB + fb])
            S.op("pool", "dma_start", writes=[wtok[1]], dsem=f"w13_{wk}_1", out=wb[:, 1], in_=self.w3[widx * NFB + fb])
            return wb, wtok

        pre = {}
        for ti in range(self.NTILE):
            t0 = ti * TT
            if ti in pre:
                xb, xtok, wpre = pre.pop(ti)
            else:
                wpre = [issue_w13(0)]
                xb, xtok, xk = xrot.next()
                self.load_xT(xb, xtok, xk, src, t0)
                wpre.append(issue_w13(1))
            for fb in range(NFB):
                wb, wtok = wpre[fb] if fb < len(wpre) else issue_w13(fb)
                for fi in range(FB // 128):
                    f = fb * (FB // 128) + fi
                    ba = (f % 2) * 2
                    for wi in range(2):
                        for k in range(KC):
                            S.op("pe", "matmul", reads=[wtok[wi], xtok[k // 4]], writes=[pst[ba + wi]],
                                 out=ps[ba + wi][:], lhsT=wb[:, wi, k, fi * 128:(fi + 1) * 128], rhs=xb[:, k, :],
                                 start=(k == 0), stop=(k == KC - 1))
                    sgb, sgt, _ = sg.next()
                    S.op("act", "activation", reads=[pst[ba]], writes=[sgt], out=sgb[:], in_=ps[ba][:], func=AF.Silu)
                    S.op("dve", "tensor_tensor", reads=[sgt, pst[ba + 1]], writes=[htoks[f]],
                         out=hT[:, f, :], in0=sgb[:], in1=ps[ba + 1][:], op=ALU.mult)
                    if f >= 1:
                        self.flush_norm(1)
            self.flush_norm()

            def lhs_of(c, ti=ti):
                wb, wtok, wk = w2r.next()
                S.op("pool", "dma_start", writes=[wtok], dsem=f"w2_{wk}", out=wb[:], in_=self.w2[widx * KC + c])
                if ti + 1 < self.NTILE:
                    if c == 1:
                        nxb, nxtok, nxk = xrot.next()
                        self.load_xT(nxb, nxtok, nxk, src, (ti + 1) * TT)
                        pre[ti + 1] = (nxb, nxtok, [])
                    elif c in (3, 5):
                        pre[ti + 1][2].append(issue_w13(len(pre[ti + 1][2])))
                return (lambda fc: wb[:, fc, :]), wtok

            self.tail(t0, src, dst, si, 0.5 / ALPHA, FCH, lhs_of, lambda fc: hT[:, fc, :], htoks)

    def setup_consts(self, top):
        S = self.S
        if hasattr(self, "identd"):
            self.ident = self.sb(top, "ident", [128, 128], BF16)
            self.itok = Tok()
            S.op("pool", "dma_start", writes=[self.itok], dsem="ci", out=self.ident[:], in_=self.identd)

    @staticmethod
    def bc(ap2, n):
        g = ap2.shape[1]
        return ap2.rearrange("p (g o) -> p g o", o=1).broadcast_to([128, g, n])

    def att(self, st, src, dst, j, si):
        S, ps, pst = self.S, self.ps, self.pst
        xrot = Rot([self.sb(st, f"ax{i}", [128, KC, TT], BF16) for i in range(1)], ntok=4)
        qT = self.sb(st, "qT", [128, KC, TT], BF16)
        qtok = [Tok() for _ in range(KC)]
        kT2 = self.sb(st, "kT2", [128, 8, 128 + TT], BF16)
        ktok = [Tok() for _ in range(8)]
        vpad = self.sb(st, "vpad", [128, 5, 4, 2, 128], BF16)
        vtok = [Tok() for _ in range(5)]
        wsl = Rot([self.sb(st, f"awq{i}", [128, KC, 128], BF16) for i in range(3)])
        wv = self.sb(st, "awv", [128, KC, 256], BF16)
        bias = self.sb(st, "abias", [128, 32 * 256], F32)
        sinks = self.sb(st, "asink", [128, 32], F32)
        atok = Tok()
        astok = Tok()
        avtok = Tok()
        sbr = Rot([self.sb(st, f"asb{i}", [128, 8 * 256], F32) for i in range(2)])
        pnr = Rot([self.sb(st, f"apn{i}", [128, 8, 256], BF16) for i in range(2)])
        ptr = Rot([self.sb(st, f"apt{i}", [128, 16, 128], BF16) for i in range(2)])
        smr = Rot([self.sb(st, f"asm{i}", [128, 6, 8], F32) for i in range(2)])
        oT = self.sb(st, "oT", [128, KC, TT], BF16)
        otoks = [Tok() for _ in range(KC)]
        psb = [ps[4].bitcast(BF16), ps[5].bitcast(BF16)]

        S.op("sp", "dma_start", writes=[atok], dsem="ab", out=bias[:], in_=self.alibi)
        S.op("sp", "dma_start", writes=[astok], dsem="as", out=sinks[:], in_=self.sinks[j])
        S.op("pool", "dma_start", writes=[avtok], dsem="av", out=wv[:], in_=self.wv[j])
        S.op("dve", "memset", writes=vtok, ap=vpad[:], constant=0.0)
        S.op("dve", "memset", writes=ktok, ap=kT2[:], constant=0.0)

        for ti in range(self.NTILE):
            t0 = ti * TT
            if ti == 0:
                nx = xrot.next()
                self.load_xT(nx[0], nx[1], nx[2], src, t0)
            xb, xtok, xk = nx
            if ti > 0:
                for kv in range(8):
                    S.op("dve", "tensor_copy", reads=[], writes=[ktok[kv]],
                         out=kT2[:, kv, 0:128], in_=kT2[:, kv, TT:TT + 128])
                S.op("dve", "tensor_copy", reads=[vtok[4]], writes=[vtok[0]], out=vpad[:, 0], in_=vpad[:, 4])
            pbc = [0]

            def issue_qk(c):
                wb, wtok, wk = wsl.next()
                srcw = self.wq[j * KC + c] if c < KC else self.wk[j * 8 + (c - KC)]
                S.op("pool", "dma_start", writes=[wtok], dsem=f"aw{wk}", out=wb[:], in_=srcw)
                return wb, wtok

            def proj_qk(c, wbt, bank):
                wb, wtok = wbt
                for k in range(KC):
                    S.op("pe", "matmul", reads=[wtok, xtok[k // 4]], writes=[pst[bank]],
                         out=ps[bank][:], lhsT=wb[:, k, :], rhs=xb[:, k, :], start=(k == 0), stop=(k == KC - 1))
                if c < KC:
                    S.op("act", "copy", reads=[pst[bank]], writes=[qtok[c]], out=qT[:, c, :], in_=ps[bank][:])
                else:
                    kv = c - KC
                    S.op("act", "copy", reads=[pst[bank]], writes=[ktok[kv]],
                         out=kT2[:, kv, 128:128 + TT], in_=ps[bank][:])

            for c in list(range(KC, KC + 8)) + [0, 1, 2, 3]:
                proj_qk(c, issue_qk(c), pbc[0] % 2)
                pbc[0] += 1
                self.flush_norm(1)
            for blk in range(4):
                bank = 2 + blk % 2
                for k in range(KC):
                    S.op("pe", "matmul", reads=[avtok, xtok[k // 4]], writes=[pst[bank]],
                         out=ps[bank][:, 0:256], lhsT=xb[:, k, blk * 128:(blk + 1) * 128], rhs=wv[:, k, :],
                         start=(k == 0), stop=(k == KC - 1))
                pv = ps[bank][:, 0:256].rearrange("p (a b) -> p a b", b=64)
                S.op("dve", "tensor_copy", reads=[pst[bank]], writes=[vtok[1 + blk]],
                     out=vpad[:, 1 + blk, :, 0, 0:64], in_=pv)
                S.op("dve", "tensor_copy", reads=[pst[bank]], writes=[vtok[1 + blk]],
                     out=vpad[:, 1 + blk, :, 1, 64:128], in_=pv)
            self.flush_norm()
            groups = [(blk, kv) for kv in range(4) for blk in range(4)]
            gst = {}

            def stA(i):
                blk, kv = groups[i]
                for g in range(8):
                    c = kv * 4 + g // 2
                    par = g % 2
                    bank = g // 2
                    S.op("pe", "matmul", reads=[qtok[c], ktok[kv * 2 + par]], writes=[pst[bank]],
                         out=ps[bank][:, par * 256:(par + 1) * 256],
                         lhsT=qT[:, c, blk * 128:(blk + 1) * 128],
                         rhs=kT2[:, kv * 2 + par, blk * 128:blk * 128 + 256],
                         start=True, stop=True)

            def stB(i):
                blk, kv = groups[i]
                first = (ti == 0 and blk == 0)
                sbt, sbtok, _ = sbr.next()
                sb3 = sbt[:].rearrange("p (g k) -> p g k", k=256)
                for gp in range(4):
                    h0 = kv * 8 + 2 * gp
                    S.op("dve", "scalar_tensor_tensor", reads=[pst[gp], atok], writes=[sbtok],
                         out=sbt[:, gp * 512:(gp + 1) * 512], in0=ps[gp][:], scalar=0.125,
                         in1=bias[:, h0 * 256:(h0 + 2) * 256], op0=ALU.mult, op1=ALU.add)
                if first:
                    S.op("dve", "memset", writes=[sbtok], ap=sb3[:, :, 0:128], constant=NEG)
                gst[i] = (sbt, sbtok, sb3)

            def stB2(i):
                blk, kv = groups[i]
                sbt, sbtok, sb3 = gst[i]
                sm, smtok, _ = smr.next()
                snk = sinks[:, kv * 8:(kv + 1) * 8]
                S.op("dve", "tensor_reduce", reads=[sbtok], writes=[smtok],
                     out=sm[:, 0, :], in_=sb3, axis=AX.X, op=ALU.max)
                S.op("dve", "tensor_tensor", reads=[smtok, astok], writes=[smtok],
                     out=sm[:, 1, :], in0=sm[:, 0, :], in1=snk, op=ALU.max)
                S.op("dve", "tensor_tensor", reads=[smtok, astok], writes=[smtok],
                     out=sm[:, 3, :], in0=snk, in1=sm[:, 1, :], op=ALU.subtract)
                S.op("dve", "tensor_scalar_mul", reads=[smtok], writes=[smtok],
                     out=sm[:, 1, :], in0=sm[:, 1, :], scalar1=-1.0)
                for g in range(8):
                    S.op("act", "activation", reads=[sbtok, smtok], writes=([sbtok, smtok] if g == 7 else []),
                         out=sb3[:, g, :], in_=sb3[:, g, :], func=AF.Exp, bias=sm[:, 1, g:g + 1],
                         accum_out=sm[:, 2, g:g + 1])
                S.op("act", "activation", reads=[smtok], writes=[smtok], out=sm[:, 3, :], in_=sm[:, 3, :],
                     func=AF.Exp)
                gst[i] = (sbt, sbtok, sb3, sm, smtok)

            def stB3(i):
                sbt, sbtok, sb3, sm, smtok = gst[i]
                S.op("dve", "tensor_tensor", reads=[smtok], writes=[smtok],
                     out=sm[:, 4, :], in0=sm[:, 2, :], in1=sm[:, 3, :], op=ALU.add)
                S.op("dve", "reciprocal", reads=[smtok], writes=[smtok], out=sm[:, 5, :], in_=sm[:, 4, :])
                pn, pntok, _ = pnr.next()
                S.op("dve", "tensor_tensor", reads=[sbtok, smtok], writes=[pntok],
                     out=pn[:], in0=sb3, in1=self.bc(sm[:, 5, :], 256), op=ALU.mult)
                gst[i] = (pn, pntok)

            def stC(i):
                pn, pntok = gst[i]
                for g in range(8):
                    for kb in range(2):
                        ii = g * 2 + kb
                        S.op("pe", "transpose", reads=[pntok, self.itok], writes=[pst[4 + ii // 8]],
                             out=psb[ii // 8][:, (ii % 8) * 128:(ii % 8 + 1) * 128],
                             in_=pn[:, g, kb * 128:(kb + 1) * 128], identity=self.ident[:])
                pt, pttok, _ = ptr.next()
                S.op("act", "copy", reads=[pst[4]], writes=[pttok],
                     out=pt[:, 0:8, :], in_=psb[0][:].rearrange("p (a b) -> p a b", b=128))
                S.op("dve", "tensor_copy", reads=[pst[5]], writes=[pttok],
                     out=pt[:, 8:16, :], in_=psb[1][:].rearrange("p (a b) -> p a b", b=128))
                gst[i] = (pt, pttok)

            def stD(i):
                blk, kv = groups[i]
                pt, pttok = gst.pop(i)
                for jp in range(4):
                    c = kv * 4 + jp
                    bank = 6
                    n = 0
                    for par in range(2):
                        for kb in range(2):
                            S.op("pe", "matmul", reads=[pttok, vtok[blk + kb]], writes=[pst[bank]],
                                 out=ps[bank][:, 0:128], lhsT=vpad[:, blk + kb, kv, par, :],
                                 rhs=pt[:, (2 * jp + par) * 2 + kb, :], start=(n == 0), stop=(n == 3))
                            n += 1
                    S.op("act", "copy", reads=[pst[bank]], writes=[otoks[c]],
                         out=oT[:, c, blk * 128:(blk + 1) * 128], in_=ps[bank][:, 0:128])

            ng = len(groups)
            stA(0)
            nextq = issue_qk(4)
            for i in range(ng + 2):
                if i < 12:
                    curq = nextq
                    if i + 1 < 12:
                        nextq = issue_qk(4 + i + 1)
                    proj_qk(4 + i, curq, 7)
                if i < ng:
                    stB(i)
                if i + 1 < ng:
                    stA(i + 1)
                if i < ng:
                    stB2(i)
                if 1 <= i <= ng:
                    stB3(i - 1)
                    stC(i - 1)
                if 2 <= i <= ng + 1:
                    stD(i - 2)
            if ti + 1 < self.NTILE:
                nx = xrot.next()
                self.load_xT(nx[0], nx[1], nx[2], src, (ti + 1) * TT)

            def lhs_of(c):
                wb, wtok, wk = wsl.next()
                S.op("pool", "dma_start", writes=[wtok], dsem=f"aw{wk}", out=wb[:], in_=self.wo_a[j * KC + c])
                return (lambda fc: wb[:, fc, :]), wtok

            self.tail(t0, src, dst, si, 1.0 / ALPHA, KC, lhs_of, lambda fc: oT[:, fc, :], otoks)

    def mlstm(self, st, src, dst, j, si):
        S, ps, pst = self.S, self.ps, self.pst
        bc = self.bc
        xrot = Rot([self.sb(st, "mx0", [128, KC, TT], BF16)], ntok=4)
        wsl = Rot([self.sb(st, f"mwc{i}", [128, KC, 128], BF16) for i in range(3)])
        wtm = Rot([self.sb(st, f"mwt{i}", [128, KC, 512], BF16) for i in range(2)])
        wg = self.sb(st, "mwg", [128, KC, 8], BF16)
        bg = self.sb(st, "mbg", [128, 8], F32)
        cst = self.sb(st, "mcst", [128, 4, 128], F32)
        onesb = self.sb(st, "monesb", [128, 1], BF16)
        wgtok, bgtok, csttok = Tok(), Tok(), Tok()
        qT = self.sb(st, "mqT", [128, 8, TT], BF16)
        kT = self.sb(st, "mkT", [128, 8, TT], BF16)
        qtok = [Tok() for _ in range(8)]
        ktok = [Tok() for _ in range(8)]
        sog = self.sb(st, "msog", [128, KC, TT], BF16)
        sogtok = [Tok() for _ in range(KC)]
        ktm = self.sb(st, "mktm", [128, 4, 1024], BF16)
        ktmtok = [Tok() for _ in range(4)]
        vtm = self.sb(st, "mvtm", [128, 4, 2048], BF16)
        vtmtok = [Tok() for _ in range(4)]
        C = self.sb(st, "mC", [128, 8, 512], F32)
        Cb = self.sb(st, "mCb", [128, 8, 512], BF16)
        ctok = [Tok() for _ in range(8)]
        cbtok = [Tok() for _ in range(8)]
        nst = self.sb(st, "mn", [128, 8], F32)
        nb = self.sb(st, "mnb", [128, 8], BF16)
        mprev = self.sb(st, "mm", [128, 4], F32)
        sttok = Tok()
        smr = Rot([self.sb(st, f"msm{i}", [128, 20, 4], F32) for i in range(2)])
        Dg = self.sb(st, "mDg", [128, 4, 128], F32)
        dgtok = Tok()
        dmr = Rot([self.sb(st, f"mdm{i}", [128, 4, 128], F32) for i in range(1)])
        wbf = self.sb(st, "mwbf", [128, 4, 128], BF16)
        wbftok = Tok()
        wT = self.sb(st, "mwT", [128, 4, 128], BF16)
        wTtok = Tok()
        Bs = Rot([self.sb(st, f"mBs{i}", [128, 512], F32) for i in range(2)])
        hb = self.sb(st, "mhb", [128, 4, 512], BF16)
        hbtok = Tok()
        kw = self.sb(st, "mkw", [128, 4, 256], BF16)
        kwtok = Tok()
        ident, tri, sel, maskneg = (cst[:, i, :] for i in range(4))
        ps3b = ps[3].bitcast(BF16)
        ps6b, ps7b = ps[6].bitcast(BF16), ps[7].bitcast(BF16)

        S.op("pool", "dma_start", writes=[wgtok], dsem="mg", out=wg[:], in_=self.m_wg[j])
        S.op("sp", "dma_start", writes=[bgtok], dsem="mb", out=bg[:], in_=self.m_bg[j])
        S.op("sp", "dma_start", writes=[csttok], dsem="mc", out=cst[:],
             in_=self.m_const.rearrange("p (a b) -> p a b", b=128))
        S.op("dve", "memset", writes=ctok, ap=C[:], constant=0.0)
        S.op("dve", "memset", writes=cbtok, ap=Cb[:], constant=0.0)
        S.op("dve", "memset", writes=[sttok], ap=nst[:], constant=0.0)
        S.op("dve", "memset", writes=[sttok], ap=nb[:], constant=0.0)
        S.op("dve", "memset", writes=[sttok], ap=mprev[:], constant=0.0)
        S.op("dve", "memset", writes=[csttok], ap=onesb[:], constant=1.0)

        for ti in range(self.NTILE):
            t0 = ti * TT
            if ti == 0:
                nx = xrot.next()
                self.load_xT(nx[0], nx[1], nx[2], src, t0)
            xb, xtok, xk = nx
            pb = 0
            for c in range(32):
                wb, wtok, wk_ = wsl.next()
                if c < 8:
                    srcw = self.m_wq[j * 8 + c]
                elif c < 16:
                    srcw = self.m_wk[j * 8 + (c - 8)]
                else:
                    srcw = self.m_wog[j * KC + (c - 16)]
                S.op("pool", "dma_start", writes=[wtok], dsem=f"mw{wk_}", out=wb[:], in_=srcw)
                bank = pb % 2
                pb += 1
                for k in range(KC):
                    S.op("pe", "matmul", reads=[wtok, xtok[k // 4]], writes=[pst[bank]],
                         out=ps[bank][:], lhsT=wb[:, k, :], rhs=xb[:, k, :], start=(k == 0), stop=(k == KC - 1))
                if c < 8:
                    S.op("act", "copy", reads=[pst[bank]], writes=[qtok[c]], out=qT[:, c, :], in_=ps[bank][:])
                elif c < 16:
                    S.op("act", "copy", reads=[pst[bank]], writes=[ktok[c - 8]], out=kT[:, c - 8, :], in_=ps[bank][:])
                else:
                    S.op("act", "activation", reads=[pst[bank]], writes=[sogtok[c - 16]],
                         out=sog[:, c - 16, :], in_=ps[bank][:], func=AF.Sigmoid)
                self.flush_norm(1)
            self.flush_norm()
            for blk in range(6):
                wb, wtok, wk_ = wtm.next()
                S.op("pool", "dma_start", writes=[wtok], dsem=f"mt{wk_}", out=wb[:], in_=self.m_wtm[j * 6 + blk])
                for ch in range(4):
                    bank = 2 + (blk * 4 + ch) % 2
                    for k in range(KC):
                        S.op("pe", "matmul", reads=[wtok, xtok[k // 4]], writes=[pst[bank]],
                             out=ps[bank][:], lhsT=xb[:, k, ch * 128:(ch + 1) * 128], rhs=wb[:, k, :],
                             start=(k == 0), stop=(k == KC - 1))
                    if blk < 2:
                        S.op("dve", "tensor_copy", reads=[pst[bank]], writes=[ktmtok[ch]],
                             out=ktm[:, ch, blk * 512:(blk + 1) * 512], in_=ps[bank][:])
                    else:
                        S.op("act", "copy", reads=[pst[bank]], writes=[vtmtok[ch]],
                             out=vtm[:, ch, (blk - 2) * 512:(blk - 1) * 512], in_=ps[bank][:])
            for ch in range(4):
                tk = slice(ch * 128, (ch + 1) * 128)
                sm, smtok, _ = smr.next()
                G, ig, fp = sm[:, 0:2, :], sm[:, 0, :], sm[:, 1, :]
                lf, b_, btot, a_, r_ = sm[:, 2, :], sm[:, 3, :], sm[:, 4, :], sm[:, 5, :], sm[:, 6, :]
                mx, mrow, inter, rsw, qn = sm[:, 7, :], sm[:, 8, :], sm[:, 9, :], sm[:, 10, :], sm[:, 11, :]
                den, emm, rdd, mnew, dec = sm[:, 12, :], sm[:, 13, :], sm[:, 14, :], sm[:, 15, :], sm[:, 16, :]
                wk16, tmp = sm[:, 17, :], sm[:, 18, :]
                for k in range(KC):
                    S.op("pe", "matmul", reads=[wgtok, xtok[k // 4]], writes=[pst[0]],
                         out=ps[0][:, 0:8], lhsT=xb[:, k, tk], rhs=wg[:, k, :], start=(k == 0), stop=(k == KC - 1))
                S.op("dve", "tensor_tensor", reads=[pst[0], bgtok], writes=[smtok],
                     out=sm[:, 0:2, :].rearrange("p a b -> p (a b)"), in0=ps[0][:, 0:8], in1=bg[:], op=ALU.add)
                S.op("act", "activation", reads=[smtok], writes=[smtok], out=lf, in_=fp, func=AF.Exp, scale=-1.0)
                S.op("dve", "tensor_scalar_add", reads=[smtok], writes=[smtok], out=lf, in0=lf, scalar1=1.0)
                S.op("act", "activation", reads=[smtok], writes=[smtok], out=lf, in_=lf, func=AF.Ln)
                S.op("dve", "tensor_scalar_mul", reads=[smtok], writes=[smtok], out=lf, in0=lf, scalar1=-1.0)
                S.op("pe", "matmul", reads=[smtok, csttok], writes=[pst[0]],
                     out=ps[0][:, 8:12], lhsT=tri, rhs=lf, start=True, stop=True)
                S.op("pe", "matmul", reads=[smtok, self.ctok], writes=[pst[0]],
                     out=ps[0][:, 12:16], lhsT=self.ones[:], rhs=lf, start=True, stop=True)
                S.op("dve", "tensor_copy", reads=[pst[0]], writes=[smtok],
                     out=sm[:, 3:5, :].rearrange("p a b -> p (a b)"), in_=ps[0][:, 8:16])
                S.op("dve", "tensor_tensor", reads=[smtok, sttok], writes=[smtok], out=a_, in0=b_, in1=mprev[:], op=ALU.add)
                S.op("dve", "tensor_tensor", reads=[smtok], writes=[smtok], out=r_, in0=ig, in1=b_, op=ALU.subtract)
                S.op("dve", "tensor_tensor", reads=[smtok, csttok], writes=[dgtok], out=Dg[:],
                     in0=cst[:, 0:1, :].broadcast_to([128, 4, 128]), in1=bc(r_, 128), op=ALU.mult)
                S.op("pe", "matmul", reads=[dgtok, self.ctok], writes=[pst[1]],
                     out=ps[1][:], lhsT=self.ones[:], rhs=Dg[:].rearrange("p a b -> p (a b)"), start=True, stop=True)
                dm, dmtok, _ = dmr.next()
                S.op("dve", "tensor_tensor", reads=[pst[1], smtok], writes=[dmtok], out=dm[:],
                     in0=ps[1][:].rearrange("p (a b) -> p a b", b=128), in1=bc(b_, 128), op=ALU.add)
                S.op("dve", "tensor_tensor", reads=[csttok], writes=[dmtok], out=dm[:], in0=dm[:],
                     in1=cst[:, 3:4, :].broadcast_to([128, 4, 128]), op=ALU.add)
                S.op("dve", "tensor_reduce", reads=[dmtok], writes=[smtok], out=mx, in_=dm[:], axis=AX.X, op=ALU.max)
                S.op("dve", "tensor_tensor", reads=[smtok], writes=[smtok], out=mrow, in0=mx, in1=a_, op=ALU.max)
                S.op("dve", "tensor_tensor", reads=[smtok], writes=[dmtok], out=dm[:], in0=dm[:], in1=bc(mrow, 128),
                     op=ALU.subtract)
                S.op("act", "activation", reads=[dmtok], writes=[dmtok], out=dm[:], in_=dm[:], func=AF.Exp)
                S.op("dve", "tensor_tensor", reads=[smtok], writes=[smtok], out=inter, in0=a_, in1=mrow, op=ALU.subtract)
                S.op("act", "activation", reads=[smtok], writes=[smtok], out=inter, in_=inter, func=AF.Exp)
                for h in range(4):
                    for dc in range(2):
                        S.op("pe", "matmul", reads=[qtok[2 * h + dc], ktok[2 * h + dc]], writes=[pst[2]],
                             out=ps[2][:, h * 128:(h + 1) * 128], lhsT=qT[:, 2 * h + dc, tk], rhs=kT[:, 2 * h + dc, tk],
                             start=(dc == 0), stop=(dc == 1))
                S.op("dve", "scalar_tensor_tensor", reads=[pst[2]], writes=[dmtok], out=dm[:],
                     in0=ps[2][:].rearrange("p (a b) -> p a b", b=128), scalar=1.0 / 16.0, in1=dm[:],
                     op0=ALU.mult, op1=ALU.mult)
                S.op("dve", "tensor_reduce", reads=[dmtok], writes=[smtok], out=rsw, in_=dm[:], axis=AX.X, op=ALU.add)
                S.op("act", "copy", reads=[dmtok], writes=[wbftok], out=wbf[:], in_=dm[:])
                for h in range(4):
                    S.op("pe", "transpose", reads=[wbftok, self.itok], writes=[pst[3]],
                         out=ps3b[:, h * 128:(h + 1) * 128], in_=wbf[:, h, :], identity=self.ident[:])
                S.op("act", "copy", reads=[pst[3]], writes=[wTtok], out=wT[:].rearrange("p a b -> p (a b)"),
                     in_=ps3b[:, 0:512])
                for h in range(4):
                    for dc in range(2):
                        S.op("pe", "matmul", reads=[qtok[2 * h + dc], sttok], writes=[pst[0]],
                             out=ps[0][:, 16 + h:17 + h], lhsT=qT[:, 2 * h + dc, tk], rhs=nb[:, 2 * h + dc:2 * h + dc + 1],
                             start=(dc == 0), stop=(dc == 1))
                S.op("dve", "tensor_copy", reads=[pst[0]], writes=[smtok], out=qn, in_=ps[0][:, 16:20])
                S.op("dve", "tensor_tensor", reads=[smtok], writes=[smtok], out=den, in0=inter, in1=qn, op=ALU.mult)
                S.op("dve", "tensor_tensor", reads=[smtok], writes=[smtok], out=den, in0=den, in1=rsw, op=ALU.add)
                S.op("dve", "tensor_scalar_mul", reads=[smtok], writes=[smtok], out=tmp, in0=den, scalar1=-1.0)
                S.op("dve", "tensor_tensor", reads=[smtok], writes=[smtok], out=den, in0=den, in1=tmp, op=ALU.max)
                S.op("act", "activation", reads=[smtok], writes=[smtok], out=emm, in_=mrow, func=AF.Exp, scale=-1.0)
                S.op("dve", "tensor_tensor", reads=[smtok], writes=[smtok], out=den, in0=den, in1=emm, op=ALU.max)
                S.op("dve", "reciprocal", reads=[smtok], writes=[smtok], out=rdd, in_=den)
                S.op("dve", "tensor_tensor", reads=[smtok], writes=[smtok], out=sm[:, 19, :], in0=inter, in1=rdd,
                     op=ALU.mult)
                for h in range(4):
                    for dc in range(2):
                        S.op("pe", "matmul", reads=[qtok[2 * h + dc], cbtok[2 * h + dc]], writes=[pst[4]],
                             out=ps[4][:], lhsT=qT[:, 2 * h + dc, tk], rhs=Cb[:, 2 * h + dc, :],
                             start=(dc == 0), stop=(dc == 1))
                    S.op("pe", "matmul", reads=[wTtok, vtmtok[ch]], writes=[pst[5]],
                         out=ps[5][:], lhsT=wT[:, h, :], rhs=vtm[:, ch, h * 512:(h + 1) * 512], start=True, stop=True)
                    bsb, bstok, _ = Bs.next()
                    S.op("act", "activation", reads=[pst[5], smtok], writes=[bstok], out=bsb[:], in_=ps[5][:],
                         func=AF.Identity, scale=sm[:, 14, h:h + 1])
                    S.op("dve", "scalar_tensor_tensor", reads=[pst[4], bstok, smtok], writes=[hbtok],
                         out=hb[:, h, :], in0=ps[4][:], scalar=sm[:, 19, h:h + 1], in1=bsb[:],
                         op0=ALU.mult, op1=ALU.add)
                for half in range(2):
                    pbv = ps6b if half == 0 else ps7b
                    for i in range(8):
                        c = half * 8 + i
                        S.op("pe", "transpose", reads=[hbtok, self.itok], writes=[pst[6 + half]],
                             out=pbv[:, i * 128:(i + 1) * 128], in_=hb[:, c // 4, (c % 4) * 128:(c % 4 + 1) * 128],
                             identity=self.ident[:])
                    S.op("dve", "tensor_tensor", reads=[pst[6 + half]], writes=sogtok[half * 8:half * 8 + 8],
                         out=sog[:, half * 8:half * 8 + 8, tk], in0=pbv[:].rearrange("p (a b) -> p a b", b=128),
                         in1=sog[:, half * 8:half * 8 + 8, tk], op=ALU.mult)
                S.op("pe", "matmul", reads=[smtok, csttok], writes=[pst[0]],
                     out=ps[0][:, 20:24], lhsT=sel, rhs=mrow, start=True, stop=True)
                S.op("dve", "tensor_copy", reads=[pst[0]], writes=[smtok], out=mnew, in_=ps[0][:, 20:24])
                S.op("dve", "tensor_tensor", reads=[smtok], writes=[smtok], out=tmp, in0=btot, in1=mnew, op=ALU.subtract)
                S.op("dve", "tensor_tensor", reads=[smtok, sttok], writes=[smtok], out=dec, in0=tmp, in1=mprev[:], op=ALU.add)
                S.op("act", "activation", reads=[smtok], writes=[smtok], out=dec, in_=dec, func=AF.Exp)
                S.op("dve", "tensor_tensor", reads=[smtok], writes=[smtok], out=wk16, in0=tmp, in1=r_, op=ALU.add)
                S.op("act", "activation", reads=[smtok], writes=[smtok], out=wk16, in_=wk16, func=AF.Exp)
                S.op("dve", "tensor_scalar_mul", reads=[smtok], writes=[smtok], out=wk16, in0=wk16, scalar1=1.0 / 16.0)
                S.op("dve", "tensor_tensor", reads=[ktmtok[ch], smtok], writes=[kwtok], out=kw[:],
                     in0=ktm[:, ch, :].rearrange("p (a b) -> p a b", b=256), in1=bc(wk16, 256), op=ALU.mult)
                for h in range(4):
                    for dc in range(2):
                        jj = 2 * h + dc
                        bank = 1 + jj % 2
                        S.op("pe", "matmul", reads=[kwtok, vtmtok[ch]], writes=[pst[bank]],
                             out=ps[bank][:], lhsT=kw[:, h, dc * 128:(dc + 1) * 128],
                             rhs=vtm[:, ch, h * 512:(h + 1) * 512], start=True, stop=True)
                        S.op("dve", "scalar_tensor_tensor", reads=[pst[bank], smtok], writes=[ctok[jj]],
                             out=C[:, jj, :], in0=C[:, jj, :], scalar=sm[:, 16, h:h + 1], in1=ps[bank][:],
                             op0=ALU.mult, op1=ALU.add)
                        S.op("act", "copy", reads=[ctok[jj]], writes=[cbtok[jj]], out=Cb[:, jj, :], in_=C[:, jj, :])
                for h in range(4):
                    for dc in range(2):
                        jj = 2 * h + dc
                        S.op("pe", "matmul", reads=[kwtok, csttok], writes=[pst[0]],
                             out=ps[0][:, 24 + jj:25 + jj], lhsT=kw[:, h, dc * 128:(dc + 1) * 128], rhs=onesb[:],
                             start=True, stop=True)
                S.op("dve", "tensor_tensor", reads=[smtok], writes=[sttok],
                     out=nst[:].rearrange("p (a b) -> p a b", b=2), in0=nst[:].rearrange("p (a b) -> p a b", b=2),
                     in1=bc(dec, 2), op=ALU.mult)
                S.op("dve", "tensor_tensor", reads=[pst[0]], writes=[sttok], out=nst[:], in0=nst[:],
                     in1=ps[0][:, 24:32], op=ALU.add)
                S.op("act", "copy", reads=[sttok], writes=[sttok], out=nb[:], in_=nst[:])
                S.op("dve", "tensor_copy", reads=[smtok], writes=[sttok], out=mprev[:], in_=mnew)

            if ti + 1 < self.NTILE:
                nx = xrot.next()
                self.load_xT(nx[0], nx[1], nx[2], src, (ti + 1) * TT)

            def lhs_of(c):
                wb, wtok, wk_ = wsl.next()
                S.op("pool", "dma_start", writes=[wtok], dsem=f"mw{wk_}", out=wb[:], in_=self.wo_m[j * KC + c])
                return (lambda fc: wb[:, fc, :]), wtok

            self.tail(t0, src, dst, si, 1.0 / ALPHA, KC, lhs_of, lambda fc: sog[:, fc, :], sogtok)


def full_plan():
    plan = []
    for l in range(DEPTH):
        plan.append(("ffn", 2 * l))
        plan.append(("att", l // 2) if l % 2 == 0 else ("mlstm", l // 2))
        plan.append(("ffn", 2 * l + 1))
    return plan


def lay_w13(w):
    n = w.shape[0]
    return np.ascontiguousarray(
        w.reshape(n, KC, 128, NFB, FB).transpose(0, 3, 2, 1, 4)).reshape(n * NFB, 128, KC, FB)


def lay_w2(w):
    n = w.shape[0]
    return np.ascontiguousarray(
        w.reshape(n, FCH, 128, KC, 128).transpose(0, 3, 2, 1, 4)).reshape(n * KC, 128, FCH, 128)


def lay_ln(v):
    n = v.shape[0]
    return np.ascontiguousarray(v.reshape(n, KC, 128).transpose(2, 0, 1)).reshape(128, n * KC)


def lay_kchunks(w, width):
    n = w.shape[1] // width
    return np.ascontiguousarray(w.reshape(KC, 128, n, width).transpose(2, 1, 0, 3))


def att_layouts(w_qkv, sinks, w_o):
    n = w_qkv.shape[0]
    wq = np.concatenate([lay_kchunks(w_qkv[i][:, :2048], 128) for i in range(n)], axis=0)
    wk = []
    for i in range(n):
        kk = w_qkv[i][:, 2048:2304].reshape(D, 4, 1, 64)
        z = np.zeros_like(kk)
        kk2 = np.concatenate([np.concatenate([kk, z], axis=3), np.concatenate([z, kk], axis=3)], axis=2)
        wk.append(lay_kchunks(kk2.reshape(D, 8 * 128), 128))
    wk = np.concatenate(wk, axis=0)
    wv = np.concatenate([lay_kchunks(w_qkv[i][:, 2304:2560], 256) for i in range(n)], axis=0)
    wo = np.concatenate([lay_kchunks(w_o[i], 128) for i in range(n)], axis=0)
    sk = np.ascontiguousarray(np.broadcast_to(sinks[:, None, :], (n, 128, 32))).astype(np.float32)
    return {"wq": wq, "wk": wk, "wv": wv, "wo_a": wo, "sinks": sk}


def ml_layouts(w_in, b_gates, w_o):
    n = w_in.shape[0]
    cat = lambda f: np.concatenate([f(i) for i in range(n)], axis=0)
    return {
        "m_wq": cat(lambda i: lay_kchunks(w_in[i][:, 0:1024], 128)),
        "m_wk": cat(lambda i: lay_kchunks(w_in[i][:, 1024:2048], 128)),
        "m_wtm": cat(lambda i: lay_kchunks(w_in[i][:, 1024:4096], 512)),
        "m_wog": cat(lambda i: lay_kchunks(w_in[i][:, 4096:6144], 128)),
        "m_wg": cat(lambda i: lay_kchunks(w_in[i][:, 6144:6152], 8)),
        "m_bg": np.ascontiguousarray(np.broadcast_to(b_gates[:, None, :], (n, 128, 8))).astype(np.float32),
        "wo_m": cat(lambda i: lay_kchunks(w_o[i], 128)),
    }


def ml_consts():
    i = np.arange(128)
    ident = np.eye(128, dtype=np.float32)
    tri = (i[:, None] <= i[None, :]).astype(np.float32)
    sel = np.zeros((128, 128), np.float32)
    sel[127, :] = 1.0
    mask = np.where(i[None, :] <= i[:, None], 0.0, NEG).astype(np.float32)
    return np.ascontiguousarray(np.concatenate([ident, tri, sel, mask], axis=1))


def alibi_table():
    q = np.arange(128)[:, None]
    jj = np.arange(256)[None, :]
    dist = (128 + q - jj).astype(np.float32)
    valid = (dist >= 0) & (dist < 128)
    slopes = (2.0 ** (-8.0 * np.arange(1, 33, dtype=np.float32) / 32)).astype(np.float32)
    tab = np.where(valid[:, None, :], -slopes[None, :, None] * dist[:, None, :], np.float32(NEG))
    return np.ascontiguousarray(tab.astype(np.float32).reshape(128, 32 * 256))


def prepare_weights(ffn_w1, ffn_w3, ffn_w2, ln_g, ln_b, att_w_qkv, att_sinks, att_w_o,
                    mlstm_w_in, mlstm_b_gates, mlstm_w_o):
    f32 = lambda a: np.asarray(a, dtype=np.float32)
    w = {
        "w1": lay_w13(f32(ffn_w1).reshape(2 * DEPTH, D, FF)),
        "w3": lay_w13(f32(ffn_w3).reshape(2 * DEPTH, D, FF)),
        "w2": lay_w2(f32(ffn_w2).reshape(2 * DEPTH, FF, D)),
        "ln_g": lay_ln(f32(ln_g).reshape(3 * DEPTH, D)),
        "ln_b": lay_ln(f32(ln_b).reshape(3 * DEPTH, D)),
        "alibi": alibi_table(),
        "identd": np.eye(128, dtype=np.float32),
        "m_const": ml_consts(),
    }
    w.update(att_layouts(f32(att_w_qkv), f32(att_sinks), f32(att_w_o)))
    w.update(ml_layouts(f32(mlstm_w_in), f32(mlstm_b_gates), f32(mlstm_w_o)))
    return w


def run_module(xs, weights, s_tok):
    n = len(xs)
    b = Builder(s_tok, full_plan(), 2 * DEPTH, DEPTH // 2, DEPTH // 2)
    nc = b.build()
    in_maps = []
    for i in range(n):
        m = dict(weights)
        m["xT"] = np.ascontiguousarray(np.asarray(xs[i], dtype=np.float32).T)
        in_maps.append(m)
    res = run_bass_kernel_spmd(nc, in_maps, core_ids=list(range(n)))
    return [np.ascontiguousarray(r["outT"].T) for r in res.results]


def kernel(x, ffn_w1, ffn_w3, ffn_w2, ln_g, ln_b, att_w_qkv, att_sinks, att_w_o,
           mlstm_w_in, mlstm_b_gates, mlstm_w_o):
    x = np.asarray(x, dtype=np.float32)
    weights = prepare_weights(ffn_w1, ffn_w3, ffn_w2, ln_g, ln_b, att_w_qkv, att_sinks, att_w_o,
                              mlstm_w_in, mlstm_b_gates, mlstm_w_o)
    outs = run_module([x[i] for i in range(x.shape[0])], weights, x.shape[1])
    return np.stack(outs, axis=0).astype(np.float32)
```

```python
import contextlib
import numpy as np
import concourse.bass as bass
import concourse.mybir as mybir
from concourse.bass_utils import run_bass_kernel_spmd

F32 = mybir.dt.float32
BF16 = mybir.dt.bfloat16
AF = mybir.ActivationFunctionType
ALU = mybir.AluOpType
AX = mybir.AxisListType

D = 2048
FF = 5632
KC = D // 128
FCH = FF // 128
TT = 512
FB = 256
NFB = FF // FB
DEPTH = 4
ALPHA = (2.0 * DEPTH) ** 0.25
EPS_S = 1e-5 / (ALPHA * ALPHA)
NEG = -30000.0

ENGS = ("pe", "act", "dve", "pool", "sp")


class Tok:
    __slots__ = ("w", "r", "rd")

    def __init__(self):
        self.w = None
        self.r = {}
        self.rd = []


class Op:
    __slots__ = ("eng", "fn", "deps", "dsem", "dcount", "need_sig", "sig")

    def __init__(self, eng, fn, deps, dsem):
        self.eng = eng
        self.fn = fn
        self.deps = deps
        self.dsem = dsem
        self.dcount = 0
        self.need_sig = False
        self.sig = 0


class Sched:
    def __init__(self, nc):
        self.nc = nc
        self.ops = []
        self.streams = {e: [] for e in ENGS}
        self.dsem_counts = {}
        self.last = {e: None for e in ENGS}
        self.pending_dma = []
        self.cost = {}

    def op(self, eng, meth, reads=(), writes=(), dsem=None, **kw):
        i = self.add(eng, (meth, kw), reads, writes, dsem)
        self.cost[i] = self._cost(eng, meth, kw)
        return i

    @staticmethod
    def _nfree(ap):
        n = 1
        for d in ap.shape[1:]:
            n *= d
        return n

    def _cost(self, eng, meth, kw):
        try:
            if meth == "dma_start":
                o, i_ = kw["out"], kw["in_"]
                b = max(self._nfree(o) * o.shape[0] * mybir.dt.size(o.dtype),
                        self._nfree(i_) * i_.shape[0] * mybir.dt.size(i_.dtype))
                return ("dma", b / 300.0)
            if meth == "matmul":
                n = self._nfree(kw["out"])
                f = 4.0 if kw["lhsT"].dtype == F32 else 1.0
                return ("c", 15.0 + f * n / 2.4)
            if meth == "transpose":
                return ("c", 110.0)
            src = kw.get("in_", kw.get("in0", kw.get("ap")))
            n = self._nfree(src)
            if eng == "act":
                return ("c", 220.0 + 1.2 * n + (100.0 if "accum_out" in kw else 0.0))
            per = 1.04
            for k in ("in_", "in0", "in1"):
                a = kw.get(k)
                if a is not None and hasattr(a, "tensor") and type(a.tensor).__name__ == "PSumTensorHandle":
                    per = 1.35
            if meth == "reciprocal":
                per = 6.5
            if meth == "memset":
                per = 0.5
            return ("c", 70.0 + per * n)
        except Exception:
            return ("c", 500.0)

    def reschedule(self, lo, hi):
        import heapq
        ops, cost = self.ops, self.cost
        idx = [i for i in range(lo, hi) if ops[i].fn is not None]
        if not idx:
            return
        inseg = set(idx)
        ndep = {}
        users = {}
        for i in idx:
            ds = [j for j in ops[i].deps if j in inseg]
            ndep[i] = len(ds)
            for j in ds:
                users.setdefault(j, []).append(i)
        fin = {}
        efree = {e: 0.0 for e in ENGS}
        dma_free = 0.0
        SYNC = 180.0
        ready = [i for i in idx if ndep[i] == 0]
        order = []
        while ready:
            best, bstart = None, None
            for i in ready:
                op = ops[i]
                t = efree[op.eng]
                for j in op.deps:
                    if j in fin:
                        tj = fin[j] + (0.0 if (ops[j].eng == op.eng and ops[j].dsem is None) else SYNC)
                        if tj > t:
                            t = tj
                if bstart is None or t < bstart - 1e-9 or (abs(t - bstart) <= 1e-9 and i < best):
                    best, bstart = i, t
            i = best
            ready.remove(i)
            op = ops[i]
            kind, dur = cost.get(i, ("c", 500.0))
            if kind == "dma":
                efree[op.eng] = bstart + 60.0
                st = max(bstart + 60.0, dma_free)
                dma_free = st + dur
                fin[i] = dma_free + 1800.0
            else:
                efree[op.eng] = bstart + dur
                fin[i] = bstart + dur
            order.append(i)
            for u in users.get(i, ()):
                ndep[u] -= 1
                if ndep[u] == 0:
                    ready.append(u)
        assert len(order) == len(idx)
        pos = {i: k for k, i in enumerate(order)}
        for e in ENGS:
            st = self.streams[e]
            seg = [i for i in st if i in inseg]
            if not seg:
                continue
            first = st.index(seg[0])
            seg_sorted = sorted(seg, key=lambda i: pos[i])
            assert st[first:first + len(seg)] == seg
            st[first:first + len(seg)] = seg_sorted

    def add(self, eng, fn, reads=(), writes=(), dsem=None, extra_deps=()):
        i = len(self.ops)
        deps = set(extra_deps)
        for t in reads:
            if t.w is not None:
                deps.add(t.w)
        for t in writes:
            if t.w is not None:
                deps.add(t.w)
            deps.update(t.r.values())
            deps.update(t.rd)
        for t in reads:
            if dsem is not None:
                t.rd.append(i)
            else:
                t.r[eng] = i
        for t in writes:
            t.w = i
            t.r = {}
            t.rd = []
        if dsem is not None:
            dsem = eng + "_" + dsem
        op = Op(eng, fn, deps, dsem)
        if dsem is not None:
            c = self.dsem_counts.get(dsem, 0) + 16
            self.dsem_counts[dsem] = c
            op.dcount = c
            self.pending_dma.append(i)
        self.ops.append(op)
        self.streams[eng].append(i)
        if fn is not None:
            self.last[eng] = i
        return i

    def barrier(self, dmas=True):
        deps = set(self.pending_dma) if dmas else set()
        for e in ENGS:
            if self.last[e] is not None:
                deps.add(self.last[e])
        if dmas:
            self.pending_dma = []
        for e in ENGS:
            self.add(e, None, extra_deps=deps)

    def _skip(self, d, op):
        return d.eng == op.eng and op.dsem is None and d.eng == "pe"

    def emit(self, stack):
        nc = self.nc
        ops = self.ops
        for op in ops:
            for j in op.deps:
                d = ops[j]
                if d.dsem is not None or self._skip(d, op):
                    continue
                d.need_sig = True
        for e in ENGS:
            cnt = 0
            dlast = {}
            for i in self.streams[e]:
                op = ops[i]
                if op.dsem is None:
                    if op.need_sig:
                        cnt += 1
                        op.sig = cnt
                else:
                    dlast[op.dsem] = dlast.get(op.dsem, 0) + 16
                    op.dcount = dlast[op.dsem]
        esem = {e: stack.enter_context(nc.semaphore("s_" + e)) for e in ENGS}
        dsem = {k: stack.enter_context(nc.semaphore("d_" + k)) for k in self.dsem_counts}
        engobj = {"pe": "tensor", "act": "scalar", "dve": "vector", "pool": "gpsimd", "sp": "sync"}

        def run_stream(e, eng):
            waited = {}
            for i in self.streams[e]:
                op = ops[i]
                need = {}
                for j in op.deps:
                    d = ops[j]
                    if d.dsem is not None:
                        key = ("d", d.dsem)
                        val = d.dcount
                    else:
                        if self._skip(d, op):
                            continue
                        key = ("e", d.eng)
                        val = d.sig
                    if need.get(key, 0) < val:
                        need[key] = val
                for key, val in need.items():
                    if waited.get(key, 0) >= val:
                        continue
                    waited[key] = val
                    sem = dsem[key[1]] if key[0] == "d" else esem[key[1]]
                    eng.wait_ge(sem, val)
                if op.fn is None:
                    continue
                ins = getattr(eng, op.fn[0])(**op.fn[1])
                if op.dsem is not None:
                    ins.then_inc(dsem[op.dsem], 16)
                elif op.need_sig:
                    ins.then_inc(esem[e], 1)

        with nc.Block() as block:
            for e in ENGS:
                if not self.streams[e]:
                    continue

                def body(eng, e=e):
                    run_stream(e, eng)
                getattr(block, engobj[e])(body)


class Rot:
    def __init__(self, bufs, ntok=None):
        self.bufs = bufs
        self.toks = [Tok() if ntok is None else [Tok() for _ in range(ntok)] for _ in bufs]
        self.i = 0

    def next(self):
        k = self.i % len(self.bufs)
        self.i += 1
        return self.bufs[k], self.toks[k], k


class Builder:
    def __init__(self, s_tok, plan, n_ffn, n_att, n_ml, resched=("mlstm", "att")):
        self.resched = set(resched)
        self.S_TOK = s_tok
        self.NTILE = s_tok // TT
        self.plan = plan
        nc = self.nc = bass.Bass("TRN2", target_bir_lowering=False)
        self.S = Sched(nc)
        dt = nc.dram_tensor
        self.xT = dt("xT", [D, s_tok], F32, kind="ExternalInput").ap()
        self.outT = dt("outT", [D, s_tok], F32, kind="ExternalOutput").ap()
        self.scr = [dt("scrA", [D, s_tok], F32, kind="Internal").ap(),
                    dt("scrB", [D, s_tok], F32, kind="Internal").ap()]
        nsb = len(plan)
        self.ln_g = dt("ln_g", [128, nsb * KC], F32, kind="ExternalInput").ap()
        self.ln_b = dt("ln_b", [128, nsb * KC], F32, kind="ExternalInput").ap()
        if n_ffn:
            self.w1 = dt("w1", [n_ffn * NFB, 128, KC, FB], F32, kind="ExternalInput").ap()
            self.w3 = dt("w3", [n_ffn * NFB, 128, KC, FB], F32, kind="ExternalInput").ap()
            self.w2 = dt("w2", [n_ffn * KC, 128, FCH, 128], F32, kind="ExternalInput").ap()
        if n_att:
            self.wq = dt("wq", [n_att * KC, 128, KC, 128], F32, kind="ExternalInput").ap()
            self.wk = dt("wk", [n_att * 8, 128, KC, 128], F32, kind="ExternalInput").ap()
            self.wv = dt("wv", [n_att, 128, KC, 256], F32, kind="ExternalInput").ap()
            self.wo_a = dt("wo_a", [n_att * KC, 128, KC, 128], F32, kind="ExternalInput").ap()
            self.sinks = dt("sinks", [n_att, 128, 32], F32, kind="ExternalInput").ap()
            self.alibi = dt("alibi", [128, 32 * 256], F32, kind="ExternalInput").ap()
            self.identd = dt("identd", [128, 128], F32, kind="ExternalInput").ap()
        if n_ml:
            self.m_wq = dt("m_wq", [n_ml * 8, 128, KC, 128], F32, kind="ExternalInput").ap()
            self.m_wk = dt("m_wk", [n_ml * 8, 128, KC, 128], F32, kind="ExternalInput").ap()
            self.m_wtm = dt("m_wtm", [n_ml * 6, 128, KC, 512], F32, kind="ExternalInput").ap()
            self.m_wog = dt("m_wog", [n_ml * KC, 128, KC, 128], F32, kind="ExternalInput").ap()
            self.m_wg = dt("m_wg", [n_ml, 128, KC, 8], F32, kind="ExternalInput").ap()
            self.m_bg = dt("m_bg", [n_ml, 128, 8], F32, kind="ExternalInput").ap()
            self.wo_m = dt("wo_m", [n_ml * KC, 128, KC, 128], F32, kind="ExternalInput").ap()
            self.m_const = dt("m_const", [128, 4 * 128], F32, kind="ExternalInput").ap()
            if not hasattr(self, "identd"):
                self.identd = dt("identd", [128, 128], F32, kind="ExternalInput").ap()

    def dtok(self, ap, ti, c):
        if ap is self.xT:
            return []
        tab = self.__dict__.setdefault("_dtoks", {}).setdefault(
            id(ap), [[Tok() for _ in range(KC)] for _ in range(self.NTILE)])
        return [tab[ti][c]]

    def sb(self, st, name, shape, dtype):
        self.uid = getattr(self, "uid", 0) + 1
        return st.enter_context(self.nc.sbuf_tensor(f"{name}_u{self.uid}", shape, dtype))

    def build(self):
        nc, S = self.nc, self.S
        with contextlib.ExitStack() as top:
            self.ps = [top.enter_context(nc.psum_tensor(f"ps{i}", [128, 512], F32)) for i in range(8)]
            self.pst = [Tok() for _ in range(8)]
            nsb = len(self.plan)
            self.gcol = self.sb(top, "gcol", [128, nsb * KC], F32)
            self.bcol = self.sb(top, "bcol", [128, nsb * KC], F32)
            self.ones = self.sb(top, "ones", [128, 128], F32)
            self.ctok = Tok()
            self.gtok = Tok()
            self.btok = Tok()
            S.op("sp", "dma_start", writes=[self.gtok], dsem="cg", out=self.gcol[:], in_=self.ln_g)
            S.op("sp", "dma_start", writes=[self.btok], dsem="cb", out=self.bcol[:], in_=self.ln_b)
            S.op("dve", "memset", writes=[self.ctok], ap=self.ones[:], constant=1.0)
            self.onesbf = self.sb(top, "onesbf", [128, 128], BF16)
            S.op("dve", "memset", writes=[self.ctok], ap=self.onesbf[:], constant=1.0)
            self.y = self.sb(top, "y", [128, KC, TT], F32)
            self.ytok = [Tok() for _ in range(KC)]
            self.sq = Rot([self.sb(top, f"sq{i}", [128, 2, TT], BF16) for i in range(2)])
            self.ost = Rot([self.sb(top, f"ost{i}", [128, TT], F32) for i in range(2)])
            self.stat = self.sb(top, "stat", [128, 3, TT], F32)
            self.stattok = Tok()
            self.setup_consts(top)
            S.barrier()
            nsub = len(self.plan)
            for si, sub in enumerate(self.plan):
                src = self.xT if si == 0 else self.scr[(si - 1) % 2]
                dst = self.outT if si == nsub - 1 else self.scr[si % 2]
                seg_lo = len(S.ops)
                with contextlib.ExitStack() as st:
                    if sub[0] == "ffn":
                        self.ffn(st, src, dst, sub[1], si)
                    elif sub[0] == "att":
                        self.att(st, src, dst, sub[1], si)
                    elif sub[0] == "mlstm":
                        self.mlstm(st, src, dst, sub[1], si)
                    last = (si == nsub - 1)
                    if last:
                        self.flush_norm()
                    if sub[0] in self.resched:
                        S.reschedule(seg_lo, len(S.ops))
                    if si < 3:
                        self.sbuf_left = getattr(self, "sbuf_left", {})
                        self.sbuf_left[sub[0]] = nc.sbuf_bytes_remaining
                    S.barrier(dmas=last)
            S.emit(top)
        return nc

    def tail(self, t0, src, dst, si, coef, n_fc, lhs_of, hT_of, htoks):
        S, ps, pst = self.S, self.ps, self.pst
        y, ytok = self.y, self.ytok
        srcv = src.rearrange("(c p) t -> c p t", p=128)
        dstv = dst.rearrange("(c p) t -> c p t", p=128)
        S1, S2 = 6, 7
        pend = None

        def stats(c):
            sqb, sqt, _ = self.sq.next()
            S.op("act", "copy", reads=[ytok[c]], writes=[sqt], out=sqb[:, 0, :], in_=y[:, c, :])
            S.op("act", "activation", reads=[ytok[c]], writes=[sqt], out=sqb[:, 1, :], in_=y[:, c, :], func=AF.Square)
            S.op("pe", "matmul", reads=[sqt, self.ctok], writes=[pst[S1]],
                 out=ps[S1][:], lhsT=self.onesbf[:], rhs=sqb[:, 0, :], start=(c == 0), stop=(c == KC - 1))
            S.op("pe", "matmul", reads=[sqt, self.ctok], writes=[pst[S2]],
                 out=ps[S2][:], lhsT=self.onesbf[:], rhs=sqb[:, 1, :], start=(c == 0), stop=(c == KC - 1))

        for c in range(KC):
            S.op("sp", "dma_start", reads=self.dtok(src, t0 // TT, c), writes=[ytok[c]], dsem=f"xr{c}",
                 out=y[:, c, :], in_=srcv[c, :, t0:t0 + TT])
            lhs, wtok = lhs_of(c)
            bank = 4 + (c % 2)
            for fc in range(n_fc):
                S.op("pe", "matmul", reads=[wtok, htoks[fc]], writes=[pst[bank]],
                     out=ps[bank][:], lhsT=lhs(fc), rhs=hT_of(fc), start=(fc == 0), stop=(fc == n_fc - 1))
            if pend is not None:
                stats(pend)
            S.op("dve", "scalar_tensor_tensor", reads=[pst[bank]], writes=[ytok[c]],
                 out=y[:, c, :], in0=ps[bank][:], scalar=coef, in1=y[:, c, :], op0=ALU.mult, op1=ALU.add)
            pend = c
        stats(pend)
        st_, stt = self.stat, self.stattok
        S.op("dve", "tensor_scalar_mul", reads=[pst[S1]], writes=[stt],
             out=st_[:, 0, :], in0=ps[S1][:], scalar1=1.0 / D)
        S.op("dve", "tensor_tensor", reads=[stt], writes=[stt],
             out=st_[:, 2, :], in0=st_[:, 0, :], in1=st_[:, 0, :], op=ALU.mult)
        S.op("dve", "scalar_tensor_tensor", reads=[pst[S2], stt], writes=[stt],
             out=st_[:, 1, :], in0=ps[S2][:], scalar=1.0 / D, in1=st_[:, 2, :], op0=ALU.mult, op1=ALU.subtract)
        S.op("dve", "tensor_scalar_add", reads=[stt], writes=[stt],
             out=st_[:, 1, :], in0=st_[:, 1, :], scalar1=EPS_S)
        S.op("act", "sqrt", reads=[stt], writes=[stt], out=st_[:, 1, :], in_=st_[:, 1, :])
        S.op("dve", "reciprocal", reads=[stt], writes=[stt], out=st_[:, 1, :], in_=st_[:, 1, :])
        def norm_step(c):
            S.op("dve", "tensor_tensor", reads=[stt], writes=[ytok[c]],
                 out=y[:, c, :], in0=y[:, c, :], in1=st_[:, 0, :], op=ALU.subtract)
            S.op("dve", "tensor_tensor", reads=[stt], writes=[ytok[c]],
                 out=y[:, c, :], in0=y[:, c, :], in1=st_[:, 1, :], op=ALU.mult)
            ob, ot, ok = self.ost.next()
            col = si * KC + c
            S.op("act", "activation", reads=[ytok[c], self.gtok, self.btok], writes=[ot],
                 out=ob[:], in_=y[:, c, :], func=AF.Identity,
                 bias=self.bcol[:, col:col + 1], scale=self.gcol[:, col:col + 1])
            S.op("sp", "dma_start", reads=[ot], writes=self.dtok(dst, t0 // TT, c), dsem=f"st{ok}",
                 out=dstv[c, :, t0:t0 + TT], in_=ob[:])

        self.pending_norm = [(lambda c=c: norm_step(c)) for c in range(KC)]

    def flush_norm(self, n=None):
        pn = getattr(self, "pending_norm", [])
        k = len(pn) if n is None else min(n, len(pn))
        for f in pn[:k]:
            f()
        self.pending_norm = pn[k:]

    def load_xT(self, xb, xtoks, xk, src, t0, ntok=TT, off=0):
        srcv = src.rearrange("(c p) t -> p c t", p=128)
        for q in range(4):
            rd = [t for c in range(4 * q, 4 * q + 4) for t in self.dtok(src, t0 // TT, c)]
            self.S.op("pool", "dma_start", reads=rd, writes=[xtoks[q]], dsem=f"x{xk}_{q}",
                      out=xb[:, 4 * q:4 * q + 4, off:off + ntok], in_=srcv[:, 4 * q:4 * q + 4, t0:t0 + ntok])

    def ffn(self, st, src, dst, widx, si):
        S, ps, pst = self.S, self.ps, self.pst
        xrot = Rot([self.sb(st, f"fx{i}", [128, KC, TT], BF16) for i in range(2)], ntok=4)
        hT = self.sb(st, "hT", [128, FCH, TT], BF16)
        htoks = [Tok() for _ in range(FCH)]
        w13 = Rot([self.sb(st, f"w13_{i}", [128, 2, KC, FB], BF16) for i in range(2)], ntok=2)
        w2r = Rot([self.sb(st, f"w2_{i}", [128, FCH, 128], BF16) for i in range(3)])
        sg = Rot([self.sb(st, f"sg{i}", [128, TT], F32) for i in range(2)])
        def issue_w13(fb):
            wb, wtok, wk = w13.next()
            S.op("pool", "dma_start", writes=[wtok[0]], dsem=f"w13_{wk}_0", out=wb[:, 0], in_=self.w1[widx * NFB + fb])
            S.op("pool", "dma_start", writes=[wtok[1]], dsem=f"w13_{wk}_1", out=wb[:, 1], in_=self.w3[widx * NFB + fb])
            return wb, wtok

        pre = {}
        for ti in range(self.NTILE):
            t0 = ti * TT
            if ti in pre:
                xb, xtok, wpre = pre.pop(ti)
            else:
                wpre = [issue_w13(0)]
                xb, xtok, xk = xrot.next()
                self.load_xT(xb, xtok, xk, src, t0)
                wpre.append(issue_w13(1))
            for fb in range(NFB):
                wb, wtok = wpre[fb] if fb < len(wpre) else issue_w13(fb)
                for fi in range(FB // 128):
                    f = fb * (FB // 128) + fi
                    ba = (f % 2) * 2
                    for wi in range(2):
                        for k in range(KC):
                            S.op("pe", "matmul", reads=[wtok[wi], xtok[k // 4]], writes=[pst[ba + wi]],
                                 out=ps[ba + wi][:], lhsT=wb[:, wi, k, fi * 128:(fi + 1) * 128], rhs=xb[:, k, :],
                                 start=(k == 0), stop=(k == KC - 1))
                    sgb, sgt, _ = sg.next()
                    S.op("act", "activation", reads=[pst[ba]], writes=[sgt], out=sgb[:], in_=ps[ba][:], func=AF.Silu)
                    S.op("dve", "tensor_tensor", reads=[sgt, pst[ba + 1]], writes=[htoks[f]],
                         out=hT[:, f, :], in0=sgb[:], in1=ps[ba + 1][:], op=ALU.mult)
                    if f >= 1:
                        self.flush_norm(1)
            self.flush_norm()

            def lhs_of(c, ti=ti):
                wb, wtok, wk = w2r.next()
                S.op("pool", "dma_start", writes=[wtok], dsem=f"w2_{wk}", out=wb[:], in_=self.w2[widx * KC + c])
                if ti + 1 < self.NTILE:
                    if c == 1:
                        nxb, nxtok, nxk = xrot.next()
                        self.load_xT(nxb, nxtok, nxk, src, (ti + 1) * TT)
                        pre[ti + 1] = (nxb, nxtok, [])
                    elif c in (3, 5):
                        pre[ti + 1][2].append(issue_w13(len(pre[ti + 1][2])))
                return (lambda fc: wb[:, fc, :]), wtok

            self.tail(t0, src, dst, si, 0.5 / ALPHA, FCH, lhs_of, lambda fc: hT[:, fc, :], htoks)

    def setup_consts(self, top):
        S = self.S
        if hasattr(self, "identd"):
            self.ident = self.sb(top, "ident", [128, 128], BF16)
            self.itok = Tok()
            S.op("pool", "dma_start", writes=[self.itok], dsem="ci", out=self.ident[:], in_=self.identd)

    @staticmethod
    def bc(ap2, n):
        g = ap2.shape[1]
        return ap2.rearrange("p (g o) -> p g o", o=1).broadcast_to([128, g, n])

    def att(self, st, src, dst, j, si):
        S, ps, pst = self.S, self.ps, self.pst
        xrot = Rot([self.sb(st, f"ax{i}", [128, KC, TT], BF16) for i in range(1)], ntok=4)
        qT = self.sb(st, "qT", [128, KC, TT], BF16)
        qtok = [Tok() for _ in range(KC)]
        kT2 = self.sb(st, "kT2", [128, 8, 128 + TT], BF16)
        ktok = [Tok() for _ in range(8)]
        vpad = self.sb(st, "vpad", [128, 5, 4, 2, 128], BF16)
        vtok = [Tok() for _ in range(5)]
        wsl = Rot([self.sb(st, f"awq{i}", [128, KC, 128], BF16) for i in range(3)])
        wv = self.sb(st, "awv", [128, KC, 256], BF16)
        bias = self.sb(st, "abias", [128, 32 * 256], F32)
        sinks = self.sb(st, "asink", [128, 32], F32)
        atok = Tok()
        astok = Tok()
        avtok = Tok()
        sbr = Rot([self.sb(st, f"asb{i}", [128, 8 * 256], F32) for i in range(2)])
        pnr = Rot([self.sb(st, f"apn{i}", [128, 8, 256], BF16) for i in range(2)])
        ptr = Rot([self.sb(st, f"apt{i}", [128, 16, 128], BF16) for i in range(2)])
        smr = Rot([self.sb(st, f"asm{i}", [128, 6, 8], F32) for i in range(2)])
        oT = self.sb(st, "oT", [128, KC, TT], BF16)
        otoks = [Tok() for _ in range(KC)]
        psb = [ps[4].bitcast(BF16), ps[5].bitcast(BF16)]

        S.op("sp", "dma_start", writes=[atok], dsem="ab", out=bias[:], in_=self.alibi)
        S.op("sp", "dma_start", writes=[astok], dsem="as", out=sinks[:], in_=self.sinks[j])
        S.op("pool", "dma_start", writes=[avtok], dsem="av", out=wv[:], in_=self.wv[j])
        S.op("dve", "memset", writes=vtok, ap=vpad[:], constant=0.0)
        S.op("dve", "memset", writes=ktok, ap=kT2[:], constant=0.0)

        for ti in range(self.NTILE):
            t0 = ti * TT
            if ti == 0:
                nx = xrot.next()
                self.load_xT(nx[0], nx[1], nx[2], src, t0)
            xb, xtok, xk = nx
            if ti > 0:
                for kv in range(8):
                    S.op("dve", "tensor_copy", reads=[], writes=[ktok[kv]],
                         out=kT2[:, kv, 0:128], in_=kT2[:, kv, TT:TT + 128])
                S.op("dve", "tensor_copy", reads=[vtok[4]], writes=[vtok[0]], out=vpad[:, 0], in_=vpad[:, 4])
            pbc = [0]

            def issue_qk(c):
                wb, wtok, wk = wsl.next()
                srcw = self.wq[j * KC + c] if c < KC else self.wk[j * 8 + (c - KC)]
                S.op("pool", "dma_start", writes=[wtok], dsem=f"aw{wk}", out=wb[:], in_=srcw)
                return wb, wtok

            def proj_qk(c, wbt, bank):
                wb, wtok = wbt
                for k in range(KC):
                    S.op("pe", "matmul", reads=[wtok, xtok[k // 4]], writes=[pst[bank]],
                         out=ps[bank][:], lhsT=wb[:, k, :], rhs=xb[:, k, :], start=(k == 0), stop=(k == KC - 1))
                if c < KC:
                    S.op("act", "copy", reads=[pst[bank]], writes=[qtok[c]], out=qT[:, c, :], in_=ps[bank][:])
                else:
                    kv = c - KC
                    S.op("act", "copy", reads=[pst[bank]], writes=[ktok[kv]],
                         out=kT2[:, kv, 128:128 + TT], in_=ps[bank][:])

            for c in list(range(KC, KC + 8)) + [0, 1, 2, 3]:
                proj_qk(c, issue_qk(c), pbc[0] % 2)
                pbc[0] += 1
                self.flush_norm(1)
            for blk in range(4):
                bank = 2 + blk % 2
                for k in range(KC):
                    S.op("pe", "matmul", reads=[avtok, xtok[k // 4]], writes=[pst[bank]],
                         out=ps[bank][:, 0:256], lhsT=xb[:, k, blk * 128:(blk + 1) * 128], rhs=wv[:, k, :],
                         start=(k == 0), stop=(k == KC - 1))
                pv = ps[bank][:, 0:256].rearrange("p (a b) -> p a b", b=64)
                S.op("dve", "tensor_copy", reads=[pst[bank]], writes=[vtok[1 + blk]],
                     out=vpad[:, 1 + blk, :, 0, 0:64], in_=pv)
                S.op("dve", "tensor_copy", reads=[pst[bank]], writes=[vtok[1 + blk]],
                     out=vpad[:, 1 + blk, :, 1, 64:128], in_=pv)
            self.flush_norm()
            groups = [(blk, kv) for kv in range(4) for blk in range(4)]
            gst = {}

            def stA(i):
                blk, kv = groups[i]
                for gp in range(4):
                    c = kv * 4 + gp
                    S.op("pe", "matmul", reads=[qtok[c], ktok[kv * 2], ktok[kv * 2 + 1]], writes=[pst[gp]],
                         out=ps[gp][:].rearrange("p (a b) -> p a b", a=2),
                         lhsT=qT[:, c, blk * 128:(blk + 1) * 128],
                         rhs=kT2[:, kv * 2:kv * 2 + 2, blk * 128:blk * 128 + 256],
                         start=True, stop=True)

            def stB(i):
                blk, kv = groups[i]
                first = (ti == 0 and blk == 0)
                sbt, sbtok, _ = sbr.next()
                sb3 = sbt[:].rearrange("p (g k) -> p g k", k=256)
                for gp in range(4):
                    h0 = kv * 8 + 2 * gp
                    S.op("dve", "scalar_tensor_tensor", reads=[pst[gp], atok], writes=[sbtok],
                         out=sbt[:, gp * 512:(gp + 1) * 512], in0=ps[gp][:], scalar=0.125,
                         in1=bias[:, h0 * 256:(h0 + 2) * 256], op0=ALU.mult, op1=ALU.add)
                if first:
                    S.op("dve", "memset", writes=[sbtok], ap=sb3[:, :, 0:128], constant=NEG)
                gst[i] = (sbt, sbtok, sb3)

            def stB2(i):
                blk, kv = groups[i]
                sbt, sbtok, sb3 = gst[i]
                sm, smtok, _ = smr.next()
                snk = sinks[:, kv * 8:(kv + 1) * 8]
                S.op("dve", "tensor_reduce", reads=[sbtok], writes=[smtok],
                     out=sm[:, 0, :], in_=sb3, axis=AX.X, op=ALU.max)
                S.op("dve", "tensor_tensor", reads=[smtok, astok], writes=[smtok],
                     out=sm[:, 1, :], in0=sm[:, 0, :], in1=snk, op=ALU.max)
                S.op("dve", "tensor_tensor", reads=[smtok, astok], writes=[smtok],
                     out=sm[:, 3, :], in0=snk, in1=sm[:, 1, :], op=ALU.subtract)
                S.op("dve", "tensor_scalar_mul", reads=[smtok], writes=[smtok],
                     out=sm[:, 1, :], in0=sm[:, 1, :], scalar1=-1.0)
                for g in range(8):
                    S.op("act", "activation", reads=[sbtok, smtok], writes=([sbtok, smtok] if g == 7 else []),
                         out=sb3[:, g, :], in_=sb3[:, g, :], func=AF.Exp, bias=sm[:, 1, g:g + 1],
                         accum_out=sm[:, 2, g:g + 1])
                S.op("act", "activation", reads=[smtok], writes=[smtok], out=sm[:, 3, :], in_=sm[:, 3, :],
                     func=AF.Exp)
                gst[i] = (sbt, sbtok, sb3, sm, smtok)

            def stB3(i):
                sbt, sbtok, sb3, sm, smtok = gst[i]
                S.op("dve", "tensor_tensor", reads=[smtok], writes=[smtok],
                     out=sm[:, 4, :], in0=sm[:, 2, :], in1=sm[:, 3, :], op=ALU.add)
                S.op("dve", "reciprocal", reads=[smtok], writes=[smtok], out=sm[:, 5, :], in_=sm[:, 4, :])
                pn, pntok, _ = pnr.next()
                S.op("dve", "tensor_tensor", reads=[sbtok, smtok], writes=[pntok],
                     out=pn[:], in0=sb3, in1=self.bc(sm[:, 5, :], 256), op=ALU.mult)
                gst[i] = (pn, pntok)

            def stC(i):
                pn, pntok = gst[i]
                for g in range(8):
                    for kb in range(2):
                        ii = g * 2 + kb
                        S.op("pe", "transpose", reads=[pntok, self.itok], writes=[pst[4 + ii // 8]],
                             out=psb[ii // 8][:, (ii % 8) * 128:(ii % 8 + 1) * 128],
                             in_=pn[:, g, kb * 128:(kb + 1) * 128], identity=self.ident[:])
                pt, pttok, _ = ptr.next()
                S.op("act", "copy", reads=[pst[4]], writes=[pttok],
                     out=pt[:, 0:8, :], in_=psb[0][:].rearrange("p (a b) -> p a b", b=128))
                S.op("dve", "tensor_copy", reads=[pst[5]], writes=[pttok],
                     out=pt[:, 8:16, :], in_=psb[1][:].rearrange("p (a b) -> p a b", b=128))
                gst[i] = (pt, pttok)

            def stD(i):
                blk, kv = groups[i]
                pt, pttok = gst.pop(i)
                for jp in range(4):
                    c = kv * 4 + jp
                    bank = 6
                    n = 0
                    for par in range(2):
                        for kb in range(2):
                            S.op("pe", "matmul", reads=[pttok, vtok[blk + kb]], writes=[pst[bank]],
                                 out=ps[bank][:, 0:128], lhsT=vpad[:, blk + kb, kv, par, :],
                                 rhs=pt[:, (2 * jp + par) * 2 + kb, :], start=(n == 0), stop=(n == 3))
                            n += 1
                    S.op("act", "copy", reads=[pst[bank]], writes=[otoks[c]],
                         out=oT[:, c, blk * 128:(blk + 1) * 128], in_=ps[bank][:, 0:128])

            ng = len(groups)
            stA(0)
            nextq = issue_qk(4)
            for i in range(ng + 2):
                if i < 12:
                    curq = nextq
                    if i + 1 < 12:
                        nextq = issue_qk(4 + i + 1)
                    proj_qk(4 + i, curq, 7)
                if i < ng:
                    stB(i)
                if i + 1 < ng:
                    stA(i + 1)
                if i < ng:
                    stB2(i)
                if 1 <= i <= ng:
                    stB3(i - 1)
                    stC(i - 1)
                if 2 <= i <= ng + 1:
                    stD(i - 2)
            if ti + 1 < self.NTILE:
                nx = xrot.next()
                self.load_xT(nx[0], nx[1], nx[2], src, (ti + 1) * TT)

            def lhs_of(c):
                wb, wtok, wk = wsl.next()
                S.op("pool", "dma_start", writes=[wtok], dsem=f"aw{wk}", out=wb[:], in_=self.wo_a[j * KC + c])
                return (lambda fc: wb[:, fc, :]), wtok

            self.tail(t0, src, dst, si, 1.0 / ALPHA, KC, lhs_of, lambda fc: oT[:, fc, :], otoks)

    def mlstm(self, st, src, dst, j, si):
        S, ps, pst = self.S, self.ps, self.pst
        bc = self.bc
        xrot = Rot([self.sb(st, "mx0", [128, KC, TT], BF16)], ntok=4)
        wsl = Rot([self.sb(st, f"mwc{i}", [128, KC, 128], BF16) for i in range(3)])
        wtm = Rot([self.sb(st, f"mwt{i}", [128, KC, 512], BF16) for i in range(2)])
        wg = self.sb(st, "mwg", [128, KC, 8], BF16)
        bg = self.sb(st, "mbg", [128, 8], F32)
        cst = self.sb(st, "mcst", [128, 4, 128], F32)
        onesb = self.sb(st, "monesb", [128, 1], BF16)
        wgtok, bgtok, csttok = Tok(), Tok(), Tok()
        qT = self.sb(st, "mqT", [128, 8, TT], BF16)
        kT = self.sb(st, "mkT", [128, 8, TT], BF16)
        qtok = [Tok() for _ in range(8)]
        ktok = [Tok() for _ in range(8)]
        sog = self.sb(st, "msog", [128, KC, TT], BF16)
        sogtok = [Tok() for _ in range(KC)]
        ktm = self.sb(st, "mktm", [128, 4, 1024], BF16)
        ktmtok = [Tok() for _ in range(4)]
        vtm = self.sb(st, "mvtm", [128, 4, 2048], BF16)
        vtmtok = [Tok() for _ in range(4)]
        C = self.sb(st, "mC", [128, 8, 512], F32)
        Cb = self.sb(st, "mCb", [128, 8, 512], BF16)
        ctok = [Tok() for _ in range(8)]
        cbtok = [Tok() for _ in range(8)]
        nst = self.sb(st, "mn", [128, 8], F32)
        nb = self.sb(st, "mnb", [128, 8], BF16)
        mprev = self.sb(st, "mm", [128, 4], F32)
        sttok = Tok()
        smr = Rot([self.sb(st, f"msm{i}", [128, 20, 4], F32) for i in range(2)])
        Dg = self.sb(st, "mDg", [128, 4, 128], F32)
        dgtok = Tok()
        dmr = Rot([self.sb(st, f"mdm{i}", [128, 4, 128], F32) for i in range(1)])
        wbf = self.sb(st, "mwbf", [128, 4, 128], BF16)
        wbftok = Tok()
        wT = self.sb(st, "mwT", [128, 4, 128], BF16)
        wTtok = Tok()
        Bs = Rot([self.sb(st, f"mBs{i}", [128, 512], F32) for i in range(2)])
        hb = self.sb(st, "mhb", [128, 4, 512], BF16)
        hbtok = Tok()
        kw = self.sb(st, "mkw", [128, 4, 256], BF16)
        kwtok = Tok()
        ident, tri, sel, maskneg = (cst[:, i, :] for i in range(4))
        ps3b = ps[3].bitcast(BF16)
        ps6b, ps7b = ps[6].bitcast(BF16), ps[7].bitcast(BF16)

        S.op("pool", "dma_start", writes=[wgtok], dsem="mg", out=wg[:], in_=self.m_wg[j])
        S.op("sp", "dma_start", writes=[bgtok], dsem="mb", out=bg[:], in_=self.m_bg[j])
        S.op("sp", "dma_start", writes=[csttok], dsem="mc", out=cst[:],
             in_=self.m_const.rearrange("p (a b) -> p a b", b=128))
        S.op("dve", "memset", writes=ctok, ap=C[:], constant=0.0)
        S.op("dve", "memset", writes=cbtok, ap=Cb[:], constant=0.0)
        S.op("dve", "memset", writes=[sttok], ap=nst[:], constant=0.0)
        S.op("dve", "memset", writes=[sttok], ap=nb[:], constant=0.0)
        S.op("dve", "memset", writes=[sttok], ap=mprev[:], constant=0.0)
        S.op("dve", "memset", writes=[csttok], ap=onesb[:], constant=1.0)

        for ti in range(self.NTILE):
            t0 = ti * TT
            if ti == 0:
                nx = xrot.next()
                self.load_xT(nx[0], nx[1], nx[2], src, t0)
            xb, xtok, xk = nx
            pb = 0
            for c in range(32):
                wb, wtok, wk_ = wsl.next()
                if c < 8:
                    srcw = self.m_wq[j * 8 + c]
                elif c < 16:
                    srcw = self.m_wk[j * 8 + (c - 8)]
                else:
                    srcw = self.m_wog[j * KC + (c - 16)]
                S.op("pool", "dma_start", writes=[wtok], dsem=f"mw{wk_}", out=wb[:], in_=srcw)
                bank = pb % 2
                pb += 1
                for k in range(KC):
                    S.op("pe", "matmul", reads=[wtok, xtok[k // 4]], writes=[pst[bank]],
                         out=ps[bank][:], lhsT=wb[:, k, :], rhs=xb[:, k, :], start=(k == 0), stop=(k == KC - 1))
                if c < 8:
                    S.op("act", "copy", reads=[pst[bank]], writes=[qtok[c]], out=qT[:, c, :], in_=ps[bank][:])
                elif c < 16:
                    S.op("act", "copy", reads=[pst[bank]], writes=[ktok[c - 8]], out=kT[:, c - 8, :], in_=ps[bank][:])
                else:
                    S.op("act", "activation", reads=[pst[bank]], writes=[sogtok[c - 16]],
                         out=sog[:, c - 16, :], in_=ps[bank][:], func=AF.Sigmoid)
                self.flush_norm(1)
            self.flush_norm()
            for blk in range(6):
                wb, wtok, wk_ = wtm.next()
                S.op("pool", "dma_start", writes=[wtok], dsem=f"mt{wk_}", out=wb[:], in_=self.m_wtm[j * 6 + blk])
                for ch in range(4):
                    bank = 2 + (blk * 4 + ch) % 2
                    for k in range(KC):
                        S.op("pe", "matmul", reads=[wtok, xtok[k // 4]], writes=[pst[bank]],
                             out=ps[bank][:], lhsT=xb[:, k, ch * 128:(ch + 1) * 128], rhs=wb[:, k, :],
                             start=(k == 0), stop=(k == KC - 1))
                    if blk < 2:
                        S.op("dve", "tensor_copy", reads=[pst[bank]], writes=[ktmtok[ch]],
                             out=ktm[:, ch, blk * 512:(blk + 1) * 512], in_=ps[bank][:])
                    else:
                        S.op("act", "copy", reads=[pst[bank]], writes=[vtmtok[ch]],
                             out=vtm[:, ch, (blk - 2) * 512:(blk - 1) * 512], in_=ps[bank][:])
            for ch in range(4):
                tk = slice(ch * 128, (ch + 1) * 128)
                sm, smtok, _ = smr.next()
                G, ig, fp = sm[:, 0:2, :], sm[:, 0, :], sm[:, 1, :]
                lf, b_, btot, a_, r_ = sm[:, 2, :], sm[:, 3, :], sm[:, 4, :], sm[:, 5, :], sm[:, 6, :]
                mx, mrow, inter, rsw, qn = sm[:, 7, :], sm[:, 8, :], sm[:, 9, :], sm[:, 10, :], sm[:, 11, :]
                den, emm, rdd, mnew, dec = sm[:, 12, :], sm[:, 13, :], sm[:, 14, :], sm[:, 15, :], sm[:, 16, :]
                wk16, tmp = sm[:, 17, :], sm[:, 18, :]
                for k in range(KC):
                    S.op("pe", "matmul", reads=[wgtok, xtok[k // 4]], writes=[pst[0]],
                         out=ps[0][:, 0:8], lhsT=xb[:, k, tk], rhs=wg[:, k, :], start=(k == 0), stop=(k == KC - 1))
                S.op("dve", "tensor_tensor", reads=[pst[0], bgtok], writes=[smtok],
                     out=sm[:, 0:2, :].rearrange("p a b -> p (a b)"), in0=ps[0][:, 0:8], in1=bg[:], op=ALU.add)
                S.op("act", "activation", reads=[smtok], writes=[smtok], out=lf, in_=fp, func=AF.Exp, scale=-1.0)
                S.op("dve", "tensor_scalar_add", reads=[smtok], writes=[smtok], out=lf, in0=lf, scalar1=1.0)
                S.op("act", "activation", reads=[smtok], writes=[smtok], out=lf, in_=lf, func=AF.Ln)
                S.op("dve", "tensor_scalar_mul", reads=[smtok], writes=[smtok], out=lf, in0=lf, scalar1=-1.0)
                S.op("pe", "matmul", reads=[smtok, csttok], writes=[pst[0]],
                     out=ps[0][:, 8:12], lhsT=tri, rhs=lf, start=True, stop=True)
                S.op("pe", "matmul", reads=[smtok, self.ctok], writes=[pst[0]],
                     out=ps[0][:, 12:16], lhsT=self.ones[:], rhs=lf, start=True, stop=True)
                S.op("dve", "tensor_copy", reads=[pst[0]], writes=[smtok],
                     out=sm[:, 3:5, :].rearrange("p a b -> p (a b)"), in_=ps[0][:, 8:16])
                S.op("dve", "tensor_tensor", reads=[smtok, sttok], writes=[smtok], out=a_, in0=b_, in1=mprev[:], op=ALU.add)
                S.op("dve", "tensor_tensor", reads=[smtok], writes=[smtok], out=r_, in0=ig, in1=b_, op=ALU.subtract)
                S.op("dve", "tensor_tensor", reads=[smtok, csttok], writes=[dgtok], out=Dg[:],
                     in0=cst[:, 0:1, :].broadcast_to([128, 4, 128]), in1=bc(r_, 128), op=ALU.mult)
                S.op("pe", "matmul", reads=[dgtok, self.ctok], writes=[pst[1]],
                     out=ps[1][:], lhsT=self.ones[:], rhs=Dg[:].rearrange("p a b -> p (a b)"), start=True, stop=True)
                dm, dmtok, _ = dmr.next()
                S.op("dve", "tensor_tensor", reads=[pst[1], smtok], writes=[dmtok], out=dm[:],
                     in0=ps[1][:].rearrange("p (a b) -> p a b", b=128), in1=bc(b_, 128), op=ALU.add)
                S.op("dve", "tensor_tensor", reads=[csttok], writes=[dmtok], out=dm[:], in0=dm[:],
                     in1=cst[:, 3:4, :].broadcast_to([128, 4, 128]), op=ALU.add)
                S.op("dve", "tensor_reduce", reads=[dmtok], writes=[smtok], out=mx, in_=dm[:], axis=AX.X, op=ALU.max)
                S.op("dve", "tensor_tensor", reads=[smtok], writes=[smtok], out=mrow, in0=mx, in1=a_, op=ALU.max)
                S.op("dve", "tensor_tensor", reads=[smtok], writes=[dmtok], out=dm[:], in0=dm[:], in1=bc(mrow, 128),
                     op=ALU.subtract)
                S.op("act", "activation", reads=[dmtok], writes=[dmtok], out=dm[:], in_=dm[:], func=AF.Exp)
                S.op("dve", "tensor_tensor", reads=[smtok], writes=[smtok], out=inter, in0=a_, in1=mrow, op=ALU.subtract)
                S.op("act", "activation", reads=[smtok], writes=[smtok], out=inter, in_=inter, func=AF.Exp)
                for h in range(4):
                    for dc in range(2):
                        S.op("pe", "matmul", reads=[qtok[2 * h + dc], ktok[2 * h + dc]], writes=[pst[2]],
                             out=ps[2][:, h * 128:(h + 1) * 128], lhsT=qT[:, 2 * h + dc, tk], rhs=kT[:, 2 * h + dc, tk],
                             start=(dc == 0), stop=(dc == 1))
                S.op("dve", "scalar_tensor_tensor", reads=[pst[2]], writes=[dmtok], out=dm[:],
                     in0=ps[2][:].rearrange("p (a b) -> p a b", b=128), scalar=1.0 / 16.0, in1=dm[:],
                     op0=ALU.mult, op1=ALU.mult)
                S.op("dve", "tensor_reduce", reads=[dmtok], writes=[smtok], out=rsw, in_=dm[:], axis=AX.X, op=ALU.add)
                S.op("act", "copy", reads=[dmtok], writes=[wbftok], out=wbf[:], in_=dm[:])
                for h in range(4):
                    S.op("pe", "transpose", reads=[wbftok, self.itok], writes=[pst[3]],
                         out=ps3b[:, h * 128:(h + 1) * 128], in_=wbf[:, h, :], identity=self.ident[:])
                S.op("act", "copy", reads=[pst[3]], writes=[wTtok], out=wT[:].rearrange("p a b -> p (a b)"),
                     in_=ps3b[:, 0:512])
                for h in range(4):
                    for dc in range(2):
                        S.op("pe", "matmul", reads=[qtok[2 * h + dc], sttok], writes=[pst[0]],
                             out=ps[0][:, 16 + h:17 + h], lhsT=qT[:, 2 * h + dc, tk], rhs=nb[:, 2 * h + dc:2 * h + dc + 1],
                             start=(dc == 0), stop=(dc == 1))
                S.op("dve", "tensor_copy", reads=[pst[0]], writes=[smtok], out=qn, in_=ps[0][:, 16:20])
                S.op("dve", "tensor_tensor", reads=[smtok], writes=[smtok], out=den, in0=inter, in1=qn, op=ALU.mult)
                S.op("dve", "tensor_tensor", reads=[smtok], writes=[smtok], out=den, in0=den, in1=rsw, op=ALU.add)
                S.op("dve", "tensor_scalar_mul", reads=[smtok], writes=[smtok], out=tmp, in0=den, scalar1=-1.0)
                S.op("dve", "tensor_tensor", reads=[smtok], writes=[smtok], out=den, in0=den, in1=tmp, op=ALU.max)
                S.op("act", "activation", reads=[smtok], writes=[smtok], out=emm, in_=mrow, func=AF.Exp, scale=-1.0)
                S.op("dve", "tensor_tensor", reads=[smtok], writes=[smtok], out=den, in0=den, in1=emm, op=ALU.max)
                S.op("dve", "reciprocal", reads=[smtok], writes=[smtok], out=rdd, in_=den)
                S.op("dve", "tensor_tensor", reads=[smtok], writes=[smtok], out=sm[:, 19, :], in0=inter, in1=rdd,
                     op=ALU.mult)
                for h in range(4):
                    for dc in range(2):
                        S.op("pe", "matmul", reads=[qtok[2 * h + dc], cbtok[2 * h + dc]], writes=[pst[4]],
                             out=ps[4][:], lhsT=qT[:, 2 * h + dc, tk], rhs=Cb[:, 2 * h + dc, :],
                             start=(dc == 0), stop=(dc == 1))
                    S.op("pe", "matmul", reads=[wTtok, vtmtok[ch]], writes=[pst[5]],
                         out=ps[5][:], lhsT=wT[:, h, :], rhs=vtm[:, ch, h * 512:(h + 1) * 512], start=True, stop=True)
                    bsb, bstok, _ = Bs.next()
                    S.op("act", "activation", reads=[pst[5], smtok], writes=[bstok], out=bsb[:], in_=ps[5][:],
                         func=AF.Identity, scale=sm[:, 14, h:h + 1])
                    S.op("dve", "scalar_tensor_tensor", reads=[pst[4], bstok, smtok], writes=[hbtok],
                         out=hb[:, h, :], in0=ps[4][:], scalar=sm[:, 19, h:h + 1], in1=bsb[:],
                         op0=ALU.mult, op1=ALU.add)
                for half in range(2):
                    pbv = ps6b if half == 0 else ps7b
                    for i in range(8):
                        c = half * 8 + i
                        S.op("pe", "transpose", reads=[hbtok, self.itok], writes=[pst[6 + half]],
                             out=pbv[:, i * 128:(i + 1) * 128], in_=hb[:, c // 4, (c % 4) * 128:(c % 4 + 1) * 128],
                             identity=self.ident[:])
                    S.op("dve", "tensor_tensor", reads=[pst[6 + half]], writes=sogtok[half * 8:half * 8 + 8],
                         out=sog[:, half * 8:half * 8 + 8, tk], in0=pbv[:].rearrange("p (a b) -> p a b", b=128),
                         in1=sog[:, half * 8:half * 8 + 8, tk], op=ALU.mult)
                S.op("pe", "matmul", reads=[smtok, csttok], writes=[pst[0]],
                     out=ps[0][:, 20:24], lhsT=sel, rhs=mrow, start=True, stop=True)
                S.op("dve", "tensor_copy", reads=[pst[0]], writes=[smtok], out=mnew, in_=ps[0][:, 20:24])
                S.op("dve", "tensor_tensor", reads=[smtok], writes=[smtok], out=tmp, in0=btot, in1=mnew, op=ALU.subtract)
                S.op("dve", "tensor_tensor", reads=[smtok, sttok], writes=[smtok], out=dec, in0=tmp, in1=mprev[:], op=ALU.add)
                S.op("act", "activation", reads=[smtok], writes=[smtok], out=dec, in_=dec, func=AF.Exp)
                S.op("dve", "tensor_tensor", reads=[smtok], writes=[smtok], out=wk16, in0=tmp, in1=r_, op=ALU.add)
                S.op("act", "activation", reads=[smtok], writes=[smtok], out=wk16, in_=wk16, func=AF.Exp)
                S.op("dve", "tensor_scalar_mul", reads=[smtok], writes=[smtok], out=wk16, in0=wk16, scalar1=1.0 / 16.0)
                S.op("dve", "tensor_tensor", reads=[ktmtok[ch], smtok], writes=[kwtok], out=kw[:],
                     in0=ktm[:, ch, :].rearrange("p (a b) -> p a b", b=256), in1=bc(wk16, 256), op=ALU.mult)
                for h in range(4):
                    for dc in range(2):
                        jj = 2 * h + dc
                        bank = 1 + jj % 2
                        S.op("pe", "matmul", reads=[kwtok, vtmtok[ch]], writes=[pst[bank]],
                             out=ps[bank][:], lhsT=kw[:, h, dc * 128:(dc + 1) * 128],
                             rhs=vtm[:, ch, h * 512:(h + 1) * 512], start=True, stop=True)
                        S.op("dve", "scalar_tensor_tensor", reads=[pst[bank], smtok], writes=[ctok[jj]],
                             out=C[:, jj, :], in0=C[:, jj, :], scalar=sm[:, 16, h:h + 1], in1=ps[bank][:],
                             op0=ALU.mult, op1=ALU.add)
                        S.op("act", "copy", reads=[ctok[jj]], writes=[cbtok[jj]], out=Cb[:, jj, :], in_=C[:, jj, :])
                for h in range(4):
                    for dc in range(2):
                        jj = 2 * h + dc
                        S.op("pe", "matmul", reads=[kwtok, csttok], writes=[pst[0]],
                             out=ps[0][:, 24 + jj:25 + jj], lhsT=kw[:, h, dc * 128:(dc + 1) * 128], rhs=onesb[:],
                             start=True, stop=True)
                S.op("dve", "tensor_tensor", reads=[smtok], writes=[sttok],
                     out=nst[:].rearrange("p (a b) -> p a b", b=2), in0=nst[:].rearrange("p (a b) -> p a b", b=2),
                     in1=bc(dec, 2), op=ALU.mult)
                S.op("dve", "tensor_tensor", reads=[pst[0]], writes=[sttok], out=nst[:], in0=nst[:],
                     in1=ps[0][:, 24:32], op=ALU.add)
                S.op("act", "copy", reads=[sttok], writes=[sttok], out=nb[:], in_=nst[:])
                S.op("dve", "tensor_copy", reads=[smtok], writes=[sttok], out=mprev[:], in_=mnew)

            if ti + 1 < self.NTILE:
                nx = xrot.next()
                self.load_xT(nx[0], nx[1], nx[2], src, (ti + 1) * TT)

            def lhs_of(c):
                wb, wtok, wk_ = wsl.next()
                S.op("pool", "dma_start", writes=[wtok], dsem=f"mw{wk_}", out=wb[:], in_=self.wo_m[j * KC + c])
                return (lambda fc: wb[:, fc, :]), wtok

            self.tail(t0, src, dst, si, 1.0 / ALPHA, KC, lhs_of, lambda fc: sog[:, fc, :], sogtok)


def full_plan():
    plan = []
    for l in range(DEPTH):
        plan.append(("ffn", 2 * l))
        plan.append(("att", l // 2) if l % 2 == 0 else ("mlstm", l // 2))
        plan.append(("ffn", 2 * l + 1))
    return plan


def lay_w13(w):
    n = w.shape[0]
    return np.ascontiguousarray(
        w.reshape(n, KC, 128, NFB, FB).transpose(0, 3, 2, 1, 4)).reshape(n * NFB, 128, KC, FB)


def lay_w2(w):
    n = w.shape[0]
    return np.ascontiguousarray(
        w.reshape(n, FCH, 128, KC, 128).transpose(0, 3, 2, 1, 4)).reshape(n * KC, 128, FCH, 128)


def lay_ln(v):
    n = v.shape[0]
    return np.ascontiguousarray(v.reshape(n, KC, 128).transpose(2, 0, 1)).reshape(128, n * KC)


def lay_kchunks(w, width):
    n = w.shape[1] // width
    return np.ascontiguousarray(w.reshape(KC, 128, n, width).transpose(2, 1, 0, 3))


def att_layouts(w_qkv, sinks, w_o):
    n = w_qkv.shape[0]
    wq = np.concatenate([lay_kchunks(w_qkv[i][:, :2048], 128) for i in range(n)], axis=0)
    wk = []
    for i in range(n):
        kk = w_qkv[i][:, 2048:2304].reshape(D, 4, 1, 64)
        z = np.zeros_like(kk)
        kk2 = np.concatenate([np.concatenate([kk, z], axis=3), np.concatenate([z, kk], axis=3)], axis=2)
        wk.append(lay_kchunks(kk2.reshape(D, 8 * 128), 128))
    wk = np.concatenate(wk, axis=0)
    wv = np.concatenate([lay_kchunks(w_qkv[i][:, 2304:2560], 256) for i in range(n)], axis=0)
    wo = np.concatenate([lay_kchunks(w_o[i], 128) for i in range(n)], axis=0)
    sk = np.ascontiguousarray(np.broadcast_to(sinks[:, None, :], (n, 128, 32))).astype(np.float32)
    return {"wq": wq, "wk": wk, "wv": wv, "wo_a": wo, "sinks": sk}


def ml_layouts(w_in, b_gates, w_o):
    n = w_in.shape[0]
    cat = lambda f: np.concatenate([f(i) for i in range(n)], axis=0)
    return {
        "m_wq": cat(lambda i: lay_kchunks(w_in[i][:, 0:1024], 128)),
        "m_wk": cat(lambda i: lay_kchunks(w_in[i][:, 1024:2048], 128)),
        "m_wtm": cat(lambda i: lay_kchunks(w_in[i][:, 1024:4096], 512)),
        "m_wog": cat(lambda i: lay_kchunks(w_in[i][:, 4096:6144], 128)),
        "m_wg": cat(lambda i: lay_kchunks(w_in[i][:, 6144:6152], 8)),
        "m_bg": np.ascontiguousarray(np.broadcast_to(b_gates[:, None, :], (n, 128, 8))).astype(np.float32),
        "wo_m": cat(lambda i: lay_kchunks(w_o[i], 128)),
    }


def ml_consts():
    i = np.arange(128)
    ident = np.eye(128, dtype=np.float32)
    tri = (i[:, None] <= i[None, :]).astype(np.float32)
    sel = np.zeros((128, 128), np.float32)
    sel[127, :] = 1.0
    mask = np.where(i[None, :] <= i[:, None], 0.0, NEG).astype(np.float32)
    return np.ascontiguousarray(np.concatenate([ident, tri, sel, mask], axis=1))


def alibi_table():
    q = np.arange(128)[:, None]
    jj = np.arange(256)[None, :]
    dist = (128 + q - jj).astype(np.float32)
    valid = (dist >= 0) & (dist < 128)
    slopes = (2.0 ** (-8.0 * np.arange(1, 33, dtype=np.float32) / 32)).astype(np.float32)
    tab = np.where(valid[:, None, :], -slopes[None, :, None] * dist[:, None, :], np.float32(NEG))
    return np.ascontiguousarray(tab.astype(np.float32).reshape(128, 32 * 256))


def prepare_weights(ffn_w1, ffn_w3, ffn_w2, ln_g, ln_b, att_w_qkv, att_sinks, att_w_o,
                    mlstm_w_in, mlstm_b_gates, mlstm_w_o):
    f32 = lambda a: np.asarray(a, dtype=np.float32)
    w = {
        "w1": lay_w13(f32(ffn_w1).reshape(2 * DEPTH, D, FF)),
        "w3": lay_w13(f32(ffn_w3).reshape(2 * DEPTH, D, FF)),
        "w2": lay_w2(f32(ffn_w2).reshape(2 * DEPTH, FF, D)),
        "ln_g": lay_ln(f32(ln_g).reshape(3 * DEPTH, D)),
        "ln_b": lay_ln(f32(ln_b).reshape(3 * DEPTH, D)),
        "alibi": alibi_table(),
        "identd": np.eye(128, dtype=np.float32),
        "m_const": ml_consts(),
    }
    w.update(att_layouts(f32(att_w_qkv), f32(att_sinks), f32(att_w_o)))
    w.update(ml_layouts(f32(mlstm_w_in), f32(mlstm_b_gates), f32(mlstm_w_o)))
    return w


def run_module(xs, weights, s_tok):
    n = len(xs)
    b = Builder(s_tok, full_plan(), 2 * DEPTH, DEPTH // 2, DEPTH // 2)
    nc = b.build()
    in_maps = []
    for i in range(n):
        m = dict(weights)
        m["xT"] = np.ascontiguousarray(np.asarray(xs[i], dtype=np.float32).T)
        in_maps.append(m)
    res = run_bass_kernel_spmd(nc, in_maps, core_ids=list(range(n)))
    return [np.ascontiguousarray(r["outT"].T) for r in res.results]


def kernel(x, ffn_w1, ffn_w3, ffn_w2, ln_g, ln_b, att_w_qkv, att_sinks, att_w_o,
           mlstm_w_in, mlstm_b_gates, mlstm_w_o):
    x = np.asarray(x, dtype=np.float32)
    weights = prepare_weights(ffn_w1, ffn_w3, ffn_w2, ln_g, ln_b, att_w_qkv, att_sinks, att_w_o,
                              mlstm_w_in, mlstm_b_gates, mlstm_w_o)
    outs = run_module([x[i] for i in range(x.shape[0])], weights, x.shape[1])
    return np.stack(outs, axis=0).astype(np.float32)
```

```python
import contextlib
import numpy as np
import concourse.bass as bass
import concourse.mybir as mybir
from concourse.bass_utils import run_bass_kernel_spmd

F32 = mybir.dt.float32
BF16 = mybir.dt.bfloat16
AF = mybir.ActivationFunctionType
ALU = mybir.AluOpType
AX = mybir.AxisListType

D = 2048
FF = 5632
KC = D // 128
FCH = FF // 128
TT = 512
FB = 256
NFB = FF // FB
DEPTH = 4
ALPHA = (2.0 * DEPTH) ** 0.25
EPS_S = 1e-5 / (ALPHA * ALPHA)
NEG = -30000.0

ENGS = ("pe", "act", "dve", "pool", "sp")


class Tok:
    __slots__ = ("w", "r", "rd")

    def __init__(self):
        self.w = None
        self.r = {}
        self.rd = []


class Op:
    __slots__ = ("eng", "fn", "deps", "dsem", "dcount", "need_sig", "sig")

    def __init__(self, eng, fn, deps, dsem):
        self.eng = eng
        self.fn = fn
        self.deps = deps
        self.dsem = dsem
        self.dcount = 0
        self.need_sig = False
        self.sig = 0


class Sched:
    def __init__(self, nc):
        self.nc = nc
        self.ops = []
        self.streams = {e: [] for e in ENGS}
        self.dsem_counts = {}
        self.last = {e: None for e in ENGS}
        self.pending_dma = []
        self.cost = {}

    def op(self, eng, meth, reads=(), writes=(), dsem=None, **kw):
        i = self.add(eng, (meth, kw), reads, writes, dsem)
        self.cost[i] = self._cost(eng, meth, kw)
        return i

    @staticmethod
    def _nfree(ap):
        n = 1
        for d in ap.shape[1:]:
            n *= d
        return n

    def _cost(self, eng, meth, kw):
        try:
            if meth == "dma_start":
                o, i_ = kw["out"], kw["in_"]
                b = max(self._nfree(o) * o.shape[0] * mybir.dt.size(o.dtype),
                        self._nfree(i_) * i_.shape[0] * mybir.dt.size(i_.dtype))
                return ("dma", b / 300.0)
            if meth == "matmul":
                n = self._nfree(kw["out"])
                f = 4.0 if kw["lhsT"].dtype == F32 else 1.0
                return ("c", 15.0 + f * n / 2.4)
            if meth == "transpose":
                return ("c", 110.0)
            src = kw.get("in_", kw.get("in0", kw.get("ap")))
            n = self._nfree(src)
            if eng == "act":
                return ("c", 220.0 + 1.2 * n + (100.0 if "accum_out" in kw else 0.0))
            per = 1.04
            for k in ("in_", "in0", "in1"):
                a = kw.get(k)
                if a is not None and hasattr(a, "tensor") and type(a.tensor).__name__ == "PSumTensorHandle":
                    per = 1.35
            if meth == "reciprocal":
                per = 6.5
            if meth == "memset":
                per = 0.5
            return ("c", 70.0 + per * n)
        except Exception:
            return ("c", 500.0)

    def reschedule(self, lo, hi):
        import heapq
        ops, cost = self.ops, self.cost
        idx = [i for i in range(lo, hi) if ops[i].fn is not None]
        if not idx:
            return
        inseg = set(idx)
        ndep = {}
        users = {}
        for i in idx:
            ds = [j for j in ops[i].deps if j in inseg]
            ndep[i] = len(ds)
            for j in ds:
                users.setdefault(j, []).append(i)
        fin = {}
        efree = {e: 0.0 for e in ENGS}
        dma_free = 0.0
        SYNC = 180.0
        ready = [i for i in idx if ndep[i] == 0]
        order = []
        while ready:
            best, bstart = None, None
            for i in ready:
                op = ops[i]
                t = efree[op.eng]
                for j in op.deps:
                    if j in fin:
                        tj = fin[j] + (0.0 if (ops[j].eng == op.eng and ops[j].dsem is None) else SYNC)
                        if tj > t:
                            t = tj
                if bstart is None or t < bstart - 1e-9 or (abs(t - bstart) <= 1e-9 and i < best):
                    best, bstart = i, t
            i = best
            ready.remove(i)
            op = ops[i]
            kind, dur = cost.get(i, ("c", 500.0))
            if kind == "dma":
                efree[op.eng] = bstart + 60.0
                st = max(bstart + 60.0, dma_free)
                dma_free = st + dur
                fin[i] = dma_free + 1800.0
            else:
                efree[op.eng] = bstart + dur
                fin[i] = bstart + dur
            order.append(i)
            for u in users.get(i, ()):
                ndep[u] -= 1
                if ndep[u] == 0:
                    ready.append(u)
        assert len(order) == len(idx)
        pos = {i: k for k, i in enumerate(order)}
        for e in ENGS:
            st = self.streams[e]
            seg = [i for i in st if i in inseg]
            if not seg:
                continue
            first = st.index(seg[0])
            seg_sorted = sorted(seg, key=lambda i: pos[i])
            assert st[first:first + len(seg)] == seg
            st[first:first + len(seg)] = seg_sorted

    def add(self, eng, fn, reads=(), writes=(), dsem=None, extra_deps=()):
        i = len(self.ops)
        deps = set(extra_deps)
        for t in reads:
            if t.w is not None:
                deps.add(t.w)
        for t in writes:
            if t.w is not None:
                deps.add(t.w)
            deps.update(t.r.values())
            deps.update(t.rd)
        for t in reads:
            if dsem is not None:
                t.rd.append(i)
            else:
                t.r[eng] = i
        for t in writes:
            t.w = i
            t.r = {}
            t.rd = []
        if dsem is not None:
            dsem = eng + "_" + dsem
        op = Op(eng, fn, deps, dsem)
        if dsem is not None:
            c = self.dsem_counts.get(dsem, 0) + 16
            self.dsem_counts[dsem] = c
            op.dcount = c
            self.pending_dma.append(i)
        self.ops.append(op)
        self.streams[eng].append(i)
        if fn is not None:
            self.last[eng] = i
        return i

    def barrier(self, dmas=True):
        deps = set(self.pending_dma) if dmas else set()
        for e in ENGS:
            if self.last[e] is not None:
                deps.add(self.last[e])
        if dmas:
            self.pending_dma = []
        for e in ENGS:
            self.add(e, None, extra_deps=deps)

    def _skip(self, d, op):
        return d.eng == op.eng and op.dsem is None and d.eng == "pe"

    def emit(self, stack):
        nc = self.nc
        ops = self.ops
        for op in ops:
            for j in op.deps:
                d = ops[j]
                if d.dsem is not None or self._skip(d, op):
                    continue
                d.need_sig = True
        for e in ENGS:
            cnt = 0
            dlast = {}
            for i in self.streams[e]:
                op = ops[i]
                if op.dsem is None:
                    if op.need_sig:
                        cnt += 1
                        op.sig = cnt
                else:
                    dlast[op.dsem] = dlast.get(op.dsem, 0) + 16
                    op.dcount = dlast[op.dsem]
        esem = {e: stack.enter_context(nc.semaphore("s_" + e)) for e in ENGS}
        dsem = {k: stack.enter_context(nc.semaphore("d_" + k)) for k in self.dsem_counts}
        engobj = {"pe": "tensor", "act": "scalar", "dve": "vector", "pool": "gpsimd", "sp": "sync"}

        def run_stream(e, eng):
            waited = {}
            for i in self.streams[e]:
                op = ops[i]
                need = {}
                for j in op.deps:
                    d = ops[j]
                    if d.dsem is not None:
                        key = ("d", d.dsem)
                        val = d.dcount
                    else:
                        if self._skip(d, op):
                            continue
                        key = ("e", d.eng)
                        val = d.sig
                    if need.get(key, 0) < val:
                        need[key] = val
                for key, val in need.items():
                    if waited.get(key, 0) >= val:
                        continue
                    waited[key] = val
                    sem = dsem[key[1]] if key[0] == "d" else esem[key[1]]
                    eng.wait_ge(sem, val)
                if op.fn is None:
                    continue
                ins = getattr(eng, op.fn[0])(**op.fn[1])
                if op.dsem is not None:
                    ins.then_inc(dsem[op.dsem], 16)
                elif op.need_sig:
                    ins.then_inc(esem[e], 1)

        with nc.Block() as block:
            for e in ENGS:
                if not self.streams[e]:
                    continue

                def body(eng, e=e):
                    run_stream(e, eng)
                getattr(block, engobj[e])(body)


class Rot:
    def __init__(self, bufs, ntok=None):
        self.bufs = bufs
        self.toks = [Tok() if ntok is None else [Tok() for _ in range(ntok)] for _ in bufs]
        self.i = 0

    def next(self):
        k = self.i % len(self.bufs)
        self.i += 1
        return self.bufs[k], self.toks[k], k


class Builder:
    def __init__(self, s_tok, plan, n_ffn, n_att, n_ml, resched=("mlstm", "att")):
        self.resched = set(resched)
        self.S_TOK = s_tok
        self.NTILE = s_tok // TT
        self.plan = plan
        nc = self.nc = bass.Bass("TRN2", target_bir_lowering=False)
        self.S = Sched(nc)
        dt = nc.dram_tensor
        self.xT = dt("xT", [D, s_tok], F32, kind="ExternalInput").ap()
        self.outT = dt("outT", [D, s_tok], F32, kind="ExternalOutput").ap()
        self.scr = [dt("scrA", [D, s_tok], F32, kind="Internal").ap(),
                    dt("scrB", [D, s_tok], F32, kind="Internal").ap()]
        nsb = len(plan)
        self.ln_g = dt("ln_g", [128, nsb * KC], F32, kind="ExternalInput").ap()
        self.ln_b = dt("ln_b", [128, nsb * KC], F32, kind="ExternalInput").ap()
        if n_ffn:
            self.w1 = dt("w1", [n_ffn * NFB, 128, KC, FB], F32, kind="ExternalInput").ap()
            self.w3 = dt("w3", [n_ffn * NFB, 128, KC, FB], F32, kind="ExternalInput").ap()
            self.w2 = dt("w2", [n_ffn * KC, 128, FCH, 128], F32, kind="ExternalInput").ap()
        if n_att:
            self.wq = dt("wq", [n_att * KC, 128, KC, 128], F32, kind="ExternalInput").ap()
            self.wk = dt("wk", [n_att * 8, 128, KC, 128], F32, kind="ExternalInput").ap()
            self.wv = dt("wv", [n_att, 128, KC, 256], F32, kind="ExternalInput").ap()
            self.wo_a = dt("wo_a", [n_att * KC, 128, KC, 128], F32, kind="ExternalInput").ap()
            self.sinks = dt("sinks", [n_att, 128, 32], F32, kind="ExternalInput").ap()
            self.alibi = dt("alibi", [128, 32 * 256], F32, kind="ExternalInput").ap()
            self.identd = dt("identd", [128, 128], F32, kind="ExternalInput").ap()
        if n_ml:
            self.m_wq = dt("m_wq", [n_ml * 8, 128, KC, 128], F32, kind="ExternalInput").ap()
            self.m_wk = dt("m_wk", [n_ml * 8, 128, KC, 128], F32, kind="ExternalInput").ap()
            self.m_wtm = dt("m_wtm", [n_ml * 6, 128, KC, 512], F32, kind="ExternalInput").ap()
            self.m_wog = dt("m_wog", [n_ml * KC, 128, KC, 128], F32, kind="ExternalInput").ap()
            self.m_wg = dt("m_wg", [n_ml, 128, KC, 8], F32, kind="ExternalInput").ap()
            self.m_bg = dt("m_bg", [n_ml, 128, 8], F32, kind="ExternalInput").ap()
            self.wo_m = dt("wo_m", [n_ml * KC, 128, KC, 128], F32, kind="ExternalInput").ap()
            self.m_const = dt("m_const", [128, 4 * 128], F32, kind="ExternalInput").ap()
            if not hasattr(self, "identd"):
                self.identd = dt("identd", [128, 128], F32, kind="ExternalInput").ap()

    def dtok(self, ap, ti, c):
        if ap is self.xT:
            return []
        tab = self.__dict__.setdefault("_dtoks", {}).setdefault(
            id(ap), [[Tok() for _ in range(KC)] for _ in range(self.NTILE)])
        return [tab[ti][c]]

    def sb(self, st, name, shape, dtype):
        self.uid = getattr(self, "uid", 0) + 1
        return st.enter_context(self.nc.sbuf_tensor(f"{name}_u{self.uid}", shape, dtype))

    def build(self):
        nc, S = self.nc, self.S
        with contextlib.ExitStack() as top:
            self.ps = [top.enter_context(nc.psum_tensor(f"ps{i}", [128, 512], F32)) for i in range(8)]
            self.pst = [Tok() for _ in range(8)]
            nsb = len(self.plan)
            self.gcol = self.sb(top, "gcol", [128, nsb * KC], F32)
            self.bcol = self.sb(top, "bcol", [128, nsb * KC], F32)
            self.ones = self.sb(top, "ones", [128, 128], F32)
            self.ctok = Tok()
            self.gtok = Tok()
            self.btok = Tok()
            S.op("sp", "dma_start", writes=[self.gtok], dsem="cg", out=self.gcol[:], in_=self.ln_g)
            S.op("sp", "dma_start", writes=[self.btok], dsem="cb", out=self.bcol[:], in_=self.ln_b)
            S.op("dve", "memset", writes=[self.ctok], ap=self.ones[:], constant=1.0)
            self.onesbf = self.sb(top, "onesbf", [128, 128], BF16)
            S.op("dve", "memset", writes=[self.ctok], ap=self.onesbf[:], constant=1.0)
            self.y = self.sb(top, "y", [128, KC, TT], F32)
            self.ytok = [Tok() for _ in range(KC)]
            self.sq = Rot([self.sb(top, f"sq{i}", [128, 2, TT], BF16) for i in range(2)])
            self.ost = Rot([self.sb(top, f"ost{i}", [128, TT], F32) for i in range(2)])
            self.stat = self.sb(top, "stat", [128, 3, TT], F32)
            self.stattok = Tok()
            self.setup_consts(top)
            S.barrier()
            nsub = len(self.plan)
            for si, sub in enumerate(self.plan):
                src = self.xT if si == 0 else self.scr[(si - 1) % 2]
                dst = self.outT if si == nsub - 1 else self.scr[si % 2]
                seg_lo = len(S.ops)
                with contextlib.ExitStack() as st:
                    if sub[0] == "ffn":
                        self.ffn(st, src, dst, sub[1], si)
                    elif sub[0] == "att":
                        self.att(st, src, dst, sub[1], si)
                    elif sub[0] == "mlstm":
                        self.mlstm(st, src, dst, sub[1], si)
                    last = (si == nsub - 1)
                    if last:
                        self.flush_norm()
                    if sub[0] in self.resched:
                        S.reschedule(seg_lo, len(S.ops))
                    if si < 3:
                        self.sbuf_left = getattr(self, "sbuf_left", {})
                        self.sbuf_left[sub[0]] = nc.sbuf_bytes_remaining
                    S.barrier(dmas=last)
            S.emit(top)
        return nc

    def tail(self, t0, src, dst, si, coef, n_fc, lhs_of, hT_of, htoks):
        S, ps, pst = self.S, self.ps, self.pst
        y, ytok = self.y, self.ytok
        srcv = src.rearrange("(c p) t -> c p t", p=128)
        dstv = dst.rearrange("(c p) t -> c p t", p=128)
        S1, S2 = 6, 7
        pend = None

        def stats(c):
            sqb, sqt, _ = self.sq.next()
            S.op("act", "copy", reads=[ytok[c]], writes=[sqt], out=sqb[:, 0, :], in_=y[:, c, :])
            S.op("act", "activation", reads=[ytok[c]], writes=[sqt], out=sqb[:, 1, :], in_=y[:, c, :], func=AF.Square)
            S.op("pe", "matmul", reads=[sqt, self.ctok], writes=[pst[S1]],
                 out=ps[S1][:], lhsT=self.onesbf[:], rhs=sqb[:, 0, :], start=(c == 0), stop=(c == KC - 1))
            S.op("pe", "matmul", reads=[sqt, self.ctok], writes=[pst[S2]],
                 out=ps[S2][:], lhsT=self.onesbf[:], rhs=sqb[:, 1, :], start=(c == 0), stop=(c == KC - 1))

        for c in range(KC):
            S.op("sp", "dma_start", reads=self.dtok(src, t0 // TT, c), writes=[ytok[c]], dsem=f"xr{c}",
                 out=y[:, c, :], in_=srcv[c, :, t0:t0 + TT])
            lhs, wtok = lhs_of(c)
            bank = 4 + (c % 2)
            for fc in range(n_fc):
                S.op("pe", "matmul", reads=[wtok, htoks[fc]], writes=[pst[bank]],
                     out=ps[bank][:], lhsT=lhs(fc), rhs=hT_of(fc), start=(fc == 0), stop=(fc == n_fc - 1))
            if pend is not None:
                stats(pend)
            S.op("dve", "scalar_tensor_tensor", reads=[pst[bank]], writes=[ytok[c]],
                 out=y[:, c, :], in0=ps[bank][:], scalar=coef, in1=y[:, c, :], op0=ALU.mult, op1=ALU.add)
            pend = c
        stats(pend)
        st_, stt = self.stat, self.stattok
        S.op("dve", "tensor_scalar_mul", reads=[pst[S1]], writes=[stt],
             out=st_[:, 0, :], in0=ps[S1][:], scalar1=1.0 / D)
        S.op("dve", "tensor_tensor", reads=[stt], writes=[stt],
             out=st_[:, 2, :], in0=st_[:, 0, :], in1=st_[:, 0, :], op=ALU.mult)
        S.op("dve", "scalar_tensor_tensor", reads=[pst[S2], stt], writes=[stt],
             out=st_[:, 1, :], in0=ps[S2][:], scalar=1.0 / D, in1=st_[:, 2, :], op0=ALU.mult, op1=ALU.subtract)
        S.op("dve", "tensor_scalar_add", reads=[stt], writes=[stt],
             out=st_[:, 1, :], in0=st_[:, 1, :], scalar1=EPS_S)
        S.op("act", "sqrt", reads=[stt], writes=[stt], out=st_[:, 1, :], in_=st_[:, 1, :])
        S.op("dve", "reciprocal", reads=[stt], writes=[stt], out=st_[:, 1, :], in_=st_[:, 1, :])
        def norm_step(c):
            S.op("dve", "tensor_tensor", reads=[stt], writes=[ytok[c]],
                 out=y[:, c, :], in0=y[:, c, :], in1=st_[:, 0, :], op=ALU.subtract)
            S.op("dve", "tensor_tensor", reads=[stt], writes=[ytok[c]],
                 out=y[:, c, :], in0=y[:, c, :], in1=st_[:, 1, :], op=ALU.mult)
            ob, ot, ok = self.ost.next()
            col = si * KC + c
            S.op("act", "activation", reads=[ytok[c], self.gtok, self.btok], writes=[ot],
                 out=ob[:], in_=y[:, c, :], func=AF.Identity,
                 bias=self.bcol[:, col:col + 1], scale=self.gcol[:, col:col + 1])
            S.op("sp", "dma_start", reads=[ot], writes=self.dtok(dst, t0 // TT, c), dsem=f"st{ok}",
                 out=dstv[c, :, t0:t0 + TT], in_=ob[:])

        self.pending_norm = [(lambda c=c: norm_step(c)) for c in range(KC)]

    def flush_norm(self, n=None):
        pn = getattr(self, "pending_norm", [])
        k = len(pn) if n is None else min(n, len(pn))
        for f in pn[:k]:
            f()
        self.pending_norm = pn[k:]

    def load_xT(self, xb, xtoks, xk, src, t0, ntok=TT, off=0):
        srcv = src.rearrange("(c p) t -> p c t", p=128)
        for q in range(4):
            rd = [t for c in range(4 * q, 4 * q + 4) for t in self.dtok(src, t0 // TT, c)]
            self.S.op("pool", "dma_start", reads=rd, writes=[xtoks[q]], dsem=f"x{xk}_{q}",
                      out=xb[:, 4 * q:4 * q + 4, off:off + ntok], in_=srcv[:, 4 * q:4 * q + 4, t0:t0 + ntok])

    def ffn(self, st, src, dst, widx, si):
        S, ps, pst = self.S, self.ps, self.pst
        xrot = Rot([self.sb(st, f"fx{i}", [128, KC, TT], BF16) for i in range(2)], ntok=4)
        hT = self.sb(st, "hT", [128, FCH, TT], BF16)
        htoks = [Tok() for _ in range(FCH)]
        w13 = Rot([self.sb(st, f"w13_{i}", [128, 2, KC, FB], BF16) for i in range(2)], ntok=2)
        w2r = Rot([self.sb(st, f"w2_{i}", [128, FCH, 128], BF16) for i in range(3)])
        sg = Rot([self.sb(st, f"sg{i}", [128, TT], F32) for i in range(2)])
        def issue_w13(fb):
            wb, wtok, wk = w13.next()
            S.op("pool", "dma_start", writes=[wtok[0]], dsem=f"w13_{wk}_0", out=wb[:, 0], in_=self.w1[widx * NFB + fb])
            S.op("pool", "dma_start", writes=[wtok[1]], dsem=f"w13_{wk}_1", out=wb[:, 1], in_=self.w3[widx * NFB + fb])
            return wb, wtok

        pre = {}
        for ti in range(self.NTILE):
            t0 = ti * TT
            if ti in pre:
                xb, xtok, wpre = pre.pop(ti)
            else:
                wpre = [issue_w13(0)]
                xb, xtok, xk = xrot.next()
                self.load_xT(xb, xtok, xk, src, t0)
                wpre.append(issue_w13(1))
            for fb in range(NFB):
                wb, wtok = wpre[fb] if fb < len(wpre) else issue_w13(fb)
                for fi in range(FB // 128):
                    f = fb * (FB // 128) + fi
                    ba = (f % 2) * 2
                    for wi in range(2):
                        for k in range(KC):
                            S.op("pe", "matmul", reads=[wtok[wi], xtok[k // 4]], writes=[pst[ba + wi]],
                                 out=ps[ba + wi][:], lhsT=wb[:, wi, k, fi * 128:(fi + 1) * 128], rhs=xb[:, k, :],
                                 start=(k == 0), stop=(k == KC - 1))
                    sgb, sgt, _ = sg.next()
                    S.op("act", "activation", reads=[pst[ba]], writes=[sgt], out=sgb[:], in_=ps[ba][:], func=AF.Silu)
                    S.op("dve", "tensor_tensor", reads=[sgt, pst[ba + 1]], writes=[htoks[f]],
                         out=hT[:, f, :], in0=sgb[:], in1=ps[ba + 1][:], op=ALU.mult)
                    if f >= 1:
                        self.flush_norm(1)
            self.flush_norm()

            def lhs_of(c, ti=ti):
                wb, wtok, wk = w2r.next()
                S.op("pool", "dma_start", writes=[wtok], dsem=f"w2_{wk}", out=wb[:], in_=self.w2[widx * KC + c])
                if ti + 1 < self.NTILE:
                    if c == 1:
                        nxb, nxtok, nxk = xrot.next()
                        self.load_xT(nxb, nxtok, nxk, src, (ti + 1) * TT)
                        pre[ti + 1] = (nxb, nxtok, [])
                    elif c in (3, 5):
                        pre[ti + 1][2].append(issue_w13(len(pre[ti + 1][2])))
                return (lambda fc: wb[:, fc, :]), wtok

            self.tail(t0, src, dst, si, 0.5 / ALPHA, FCH, lhs_of, lambda fc: hT[:, fc, :], htoks)

    def setup_consts(self, top):
        S = self.S
        if hasattr(self, "identd"):
            self.ident = self.sb(top, "ident", [128, 128], BF16)
            self.itok = Tok()
            S.op("pool", "dma_start", writes=[self.itok], dsem="ci", out=self.ident[:], in_=self.identd)

    @staticmethod
    def bc(ap2, n):
        g = ap2.shape[1]
        return ap2.rearrange("p (g o) -> p g o", o=1).broadcast_to([128, g, n])

    def att(self, st, src, dst, j, si):
        S, ps, pst = self.S, self.ps, self.pst
        xrot = Rot([self.sb(st, f"ax{i}", [128, KC, TT], BF16) for i in range(1)], ntok=4)
        qT = self.sb(st, "qT", [128, KC, TT], BF16)
        qtok = [Tok() for _ in range(KC)]
        kT2 = self.sb(st, "kT2", [128, 8, 128 + TT], BF16)
        ktok = [Tok() for _ in range(8)]
        vpad = self.sb(st, "vpad", [128, 5, 4, 2, 128], BF16)
        vtok = [Tok() for _ in range(5)]
        wsl = Rot([self.sb(st, f"awq{i}", [128, KC, 128], BF16) for i in range(3)])
        wv = self.sb(st, "awv", [128, KC, 256], BF16)
        bias = self.sb(st, "abias", [128, 32 * 256], F32)
        sinks = self.sb(st, "asink", [128, 32], F32)
        atok = Tok()
        astok = Tok()
        avtok = Tok()
        sbr = Rot([self.sb(st, f"asb{i}", [128, 8 * 256], F32) for i in range(2)])
        pnr = Rot([self.sb(st, f"apn{i}", [128, 8, 256], BF16) for i in range(2)])
        ptr = Rot([self.sb(st, f"apt{i}", [128, 16, 128], BF16) for i in range(2)])
        smr = Rot([self.sb(st, f"asm{i}", [128, 6, 8], F32) for i in range(2)])
        oT = self.sb(st, "oT", [128, KC, TT], BF16)
        otoks = [Tok() for _ in range(KC)]
        psb = [ps[4].bitcast(BF16), ps[5].bitcast(BF16)]

        S.op("sp", "dma_start", writes=[atok], dsem="ab", out=bias[:], in_=self.alibi)
        S.op("sp", "dma_start", writes=[astok], dsem="as", out=sinks[:], in_=self.sinks[j])
        S.op("pool", "dma_start", writes=[avtok], dsem="av", out=wv[:], in_=self.wv[j])
        S.op("dve", "memset", writes=vtok, ap=vpad[:], constant=0.0)
        S.op("dve", "memset", writes=ktok, ap=kT2[:], constant=0.0)

        for ti in range(self.NTILE):
            t0 = ti * TT
            if ti == 0:
                nx = xrot.next()
                self.load_xT(nx[0], nx[1], nx[2], src, t0)
            xb, xtok, xk = nx
            if ti > 0:
                for kv in range(8):
                    S.op("dve", "tensor_copy", reads=[], writes=[ktok[kv]],
                         out=kT2[:, kv, 0:128], in_=kT2[:, kv, TT:TT + 128])
                S.op("dve", "tensor_copy", reads=[vtok[4]], writes=[vtok[0]], out=vpad[:, 0], in_=vpad[:, 4])
            pbc = [0]

            def issue_qk(c):
                wb, wtok, wk = wsl.next()
                srcw = self.wq[j * KC + c] if c < KC else self.wk[j * 8 + (c - KC)]
                S.op("pool", "dma_start", writes=[wtok], dsem=f"aw{wk}", out=wb[:], in_=srcw)
                return wb, wtok

            def proj_qk(c, wbt, bank):
                wb, wtok = wbt
                for k in range(KC):
                    S.op("pe", "matmul", reads=[wtok, xtok[k // 4]], writes=[pst[bank]],
                         out=ps[bank][:], lhsT=wb[:, k, :], rhs=xb[:, k, :], start=(k == 0), stop=(k == KC - 1))
                if c < KC:
                    S.op("act", "copy", reads=[pst[bank]], writes=[qtok[c]], out=qT[:, c, :], in_=ps[bank][:])
                else:
                    kv = c - KC
                    S.op("act", "copy", reads=[pst[bank]], writes=[ktok[kv]],
                         out=kT2[:, kv, 128:128 + TT], in_=ps[bank][:])

            for c in list(range(KC, KC + 8)) + [0, 1, 2, 3]:
                proj_qk(c, issue_qk(c), pbc[0] % 2)
                pbc[0] += 1
                self.flush_norm(1)
            for blk in range(4):
                bank = 2 + blk % 2
                for k in range(KC):
                    S.op("pe", "matmul", reads=[avtok, xtok[k // 4]], writes=[pst[bank]],
                         out=ps[bank][:, 0:256], lhsT=xb[:, k, blk * 128:(blk + 1) * 128], rhs=wv[:, k, :],
                         start=(k == 0), stop=(k == KC - 1))
                pv = ps[bank][:, 0:256].rearrange("p (a b) -> p a b", b=64)
                S.op("dve", "tensor_copy", reads=[pst[bank]], writes=[vtok[1 + blk]],
                     out=vpad[:, 1 + blk, :, 0, 0:64], in_=pv)
                S.op("dve", "tensor_copy", reads=[pst[bank]], writes=[vtok[1 + blk]],
                     out=vpad[:, 1 + blk, :, 1, 64:128], in_=pv)
            self.flush_norm()
            groups = [(blk, kv) for kv in range(4) for blk in range(4)]
            gst = {}

            def stA(i):
                blk, kv = groups[i]
                for gp in range(4):
                    c = kv * 4 + gp
                    S.op("pe", "matmul", reads=[qtok[c], ktok[kv * 2], ktok[kv * 2 + 1]], writes=[pst[gp]],
                         out=ps[gp][:].rearrange("p (a b) -> p a b", a=2),
                         lhsT=qT[:, c, blk * 128:(blk + 1) * 128],
                         rhs=kT2[:, kv * 2:kv * 2 + 2, blk * 128:blk * 128 + 256],
                         start=True, stop=True)

            def stB(i):
                blk, kv = groups[i]
                first = (ti == 0 and blk == 0)
                sbt, sbtok, _ = sbr.next()
                sb3 = sbt[:].rearrange("p (g k) -> p g k", k=256)
                for gp in range(4):
                    h0 = kv * 8 + 2 * gp
                    S.op("dve", "scalar_tensor_tensor", reads=[pst[gp], atok], writes=[sbtok],
                         out=sbt[:, gp * 512:(gp + 1) * 512], in0=ps[gp][:], scalar=0.125,
                         in1=bias[:, h0 * 256:(h0 + 2) * 256], op0=ALU.mult, op1=ALU.add)
                if first:
                    S.op("dve", "memset", writes=[sbtok], ap=sb3[:, :, 0:128], constant=NEG)
                gst[i] = (sbt, sbtok, sb3)

            def stB2(i):
                blk, kv = groups[i]
                sbt, sbtok, sb3 = gst[i]
                sm, smtok, _ = smr.next()
                snk = sinks[:, kv * 8:(kv + 1) * 8]
                S.op("dve", "tensor_reduce", reads=[sbtok], writes=[smtok],
                     out=sm[:, 0, :], in_=sb3, axis=AX.X, op=ALU.max)
                S.op("dve", "tensor_tensor", reads=[smtok, astok], writes=[smtok],
                     out=sm[:, 1, :], in0=sm[:, 0, :], in1=snk, op=ALU.max)
                S.op("dve", "tensor_tensor", reads=[smtok, astok], writes=[smtok],
                     out=sm[:, 3, :], in0=snk, in1=sm[:, 1, :], op=ALU.subtract)
                S.op("dve", "tensor_scalar_mul", reads=[smtok], writes=[smtok],
                     out=sm[:, 1, :], in0=sm[:, 1, :], scalar1=-1.0)
                for g in range(8):
                    S.op("act", "activation", reads=[sbtok, smtok], writes=([sbtok, smtok] if g == 7 else []),
                         out=sb3[:, g, :], in_=sb3[:, g, :], func=AF.Exp, bias=sm[:, 1, g:g + 1],
                         accum_out=sm[:, 2, g:g + 1])
                S.op("act", "activation", reads=[smtok], writes=[smtok], out=sm[:, 3, :], in_=sm[:, 3, :],
                     func=AF.Exp)
                gst[i] = (sbt, sbtok, sb3, sm, smtok)

            def stB3(i):
                sbt, sbtok, sb3, sm, smtok = gst[i]
                S.op("dve", "tensor_tensor", reads=[smtok], writes=[smtok],
                     out=sm[:, 4, :], in0=sm[:, 2, :], in1=sm[:, 3, :], op=ALU.add)
                S.op("dve", "reciprocal", reads=[smtok], writes=[smtok], out=sm[:, 5, :], in_=sm[:, 4, :])
                pn, pntok, _ = pnr.next()
                S.op("dve", "tensor_tensor", reads=[sbtok, smtok], writes=[pntok],
                     out=pn[:], in0=sb3, in1=self.bc(sm[:, 5, :], 256), op=ALU.mult)
                gst[i] = (pn, pntok)

            def stC(i):
                pn, pntok = gst[i]
                for g in range(8):
                    for kb in range(2):
                        ii = g * 2 + kb
                        S.op("pe", "transpose", reads=[pntok, self.itok], writes=[pst[4 + ii // 8]],
                             out=psb[ii // 8][:, (ii % 8) * 128:(ii % 8 + 1) * 128],
                             in_=pn[:, g, kb * 128:(kb + 1) * 128], identity=self.ident[:])
                pt, pttok, _ = ptr.next()
                S.op("act", "copy", reads=[pst[4]], writes=[pttok],
                     out=pt[:, 0:8, :], in_=psb[0][:].rearrange("p (a b) -> p a b", b=128))
                S.op("dve", "tensor_copy", reads=[pst[5]], writes=[pttok],
                     out=pt[:, 8:16, :], in_=psb[1][:].rearrange("p (a b) -> p a b", b=128))
                gst[i] = (pt, pttok)

            def stD(i):
                blk, kv = groups[i]
                pt, pttok = gst.pop(i)
                for jp in range(4):
                    c = kv * 4 + jp
                    bank = 6
                    n = 0
                    for par in range(2):
                        for kb in range(2):
                            S.op("pe", "matmul", reads=[pttok, vtok[blk + kb]], writes=[pst[bank]],
                                 out=ps[bank][:, 0:128], lhsT=vpad[:, blk + kb, kv, par, :],
                                 rhs=pt[:, (2 * jp + par) * 2 + kb, :], start=(n == 0), stop=(n == 3))
                            n += 1
                    S.op("act", "copy", reads=[pst[bank]], writes=[otoks[c]],
                         out=oT[:, c, blk * 128:(blk + 1) * 128], in_=ps[bank][:, 0:128])

            ng = len(groups)
            stA(0)
            nextq = issue_qk(4)
            for i in range(ng + 2):
                if i < 12:
                    curq = nextq
                    if i + 1 < 12:
                        nextq = issue_qk(4 + i + 1)
                    proj_qk(4 + i, curq, 7)
                if i < ng:
                    stB(i)
                if i + 1 < ng:
                    stA(i + 1)
                if i < ng:
                    stB2(i)
                if 1 <= i <= ng:
                    stB3(i - 1)
                    stC(i - 1)
                if 2 <= i <= ng + 1:
                    stD(i - 2)
            if ti + 1 < self.NTILE:
                nx = xrot.next()
                self.load_xT(nx[0], nx[1], nx[2], src, (ti + 1) * TT)

            def lhs_of(c):
                wb, wtok, wk = wsl.next()
                S.op("pool", "dma_start", writes=[wtok], dsem=f"aw{wk}", out=wb[:], in_=self.wo_a[j * KC + c])
                return (lambda fc: wb[:, fc, :]), wtok

            self.tail(t0, src, dst, si, 1.0 / ALPHA, KC, lhs_of, lambda fc: oT[:, fc, :], otoks)

    def mlstm(self, st, src, dst, j, si):
        S, ps, pst = self.S, self.ps, self.pst
        bc = self.bc
        xrot = Rot([self.sb(st, "mx0", [128, KC, TT], BF16)], ntok=4)
        wsl = Rot([self.sb(st, f"mwc{i}", [128, KC, 128], BF16) for i in range(3)])
        wtm = Rot([self.sb(st, f"mwt{i}", [128, KC, 512], BF16) for i in range(2)])
        wg = self.sb(st, "mwg", [128, KC, 8], BF16)
        bg = self.sb(st, "mbg", [128, 8], F32)
        cst = self.sb(st, "mcst", [128, 4, 128], F32)
        onesb = self.sb(st, "monesb", [128, 1], BF16)
        wgtok, bgtok, csttok = Tok(), Tok(), Tok()
        qT = self.sb(st, "mqT", [128, 8, TT], BF16)
        kT = self.sb(st, "mkT", [128, 8, TT], BF16)
        qtok = [Tok() for _ in range(8)]
        ktok = [Tok() for _ in range(8)]
        sog = self.sb(st, "msog", [128, KC, TT], BF16)
        sogtok = [Tok() for _ in range(KC)]
        ktm = self.sb(st, "mktm", [128, 4, 1024], BF16)
        ktmtok = [Tok() for _ in range(4)]
        vtm = self.sb(st, "mvtm", [128, 4, 2048], BF16)
        vtmtok = [Tok() for _ in range(4)]
        C = self.sb(st, "mC", [128, 8, 512], F32)
        Cb = self.sb(st, "mCb", [128, 8, 512], BF16)
        ctok = [Tok() for _ in range(8)]
        cbtok = [Tok() for _ in range(8)]
        nst = self.sb(st, "mn", [128, 8], F32)
        nb = self.sb(st, "mnb", [128, 8], BF16)
        mprev = self.sb(st, "mm", [128, 4], F32)
        sttok = Tok()
        smr = Rot([self.sb(st, f"msm{i}", [128, 20, 4], F32) for i in range(2)])
        Dg = self.sb(st, "mDg", [128, 4, 128], F32)
        dgtok = Tok()
        dmr = Rot([self.sb(st, f"mdm{i}", [128, 4, 128], F32) for i in range(1)])
        wbf = self.sb(st, "mwbf", [128, 4, 128], BF16)
        wbftok = Tok()
        wT = self.sb(st, "mwT", [128, 4, 128], BF16)
        wTtok = Tok()
        Bs = Rot([self.sb(st, f"mBs{i}", [128, 512], F32) for i in range(2)])
        hb = self.sb(st, "mhb", [128, 4, 512], BF16)
        hbtok = Tok()
        kw = self.sb(st, "mkw", [128, 4, 256], BF16)
        kwtok = Tok()
        ident, tri, sel, maskneg = (cst[:, i, :] for i in range(4))
        ps3b = ps[3].bitcast(BF16)
        ps6b, ps7b = ps[6].bitcast(BF16), ps[7].bitcast(BF16)

        S.op("pool", "dma_start", writes=[wgtok], dsem="mg", out=wg[:], in_=self.m_wg[j])
        S.op("sp", "dma_start", writes=[bgtok], dsem="mb", out=bg[:], in_=self.m_bg[j])
        S.op("sp", "dma_start", writes=[csttok], dsem="mc", out=cst[:],
             in_=self.m_const.rearrange("p (a b) -> p a b", b=128))
        S.op("dve", "memset", writes=ctok, ap=C[:], constant=0.0)
        S.op("dve", "memset", writes=cbtok, ap=Cb[:], constant=0.0)
        S.op("dve", "memset", writes=[sttok], ap=nst[:], constant=0.0)
        S.op("dve", "memset", writes=[sttok], ap=nb[:], constant=0.0)
        S.op("dve", "memset", writes=[sttok], ap=mprev[:], constant=0.0)
        S.op("dve", "memset", writes=[csttok], ap=onesb[:], constant=1.0)

        for ti in range(self.NTILE):
            t0 = ti * TT
            if ti == 0:
                nx = xrot.next()
                self.load_xT(nx[0], nx[1], nx[2], src, t0)
            xb, xtok, xk = nx
            pb = 0
            for c in range(32):
                wb, wtok, wk_ = wsl.next()
                if c < 8:
                    srcw = self.m_wq[j * 8 + c]
                elif c < 16:
                    srcw = self.m_wk[j * 8 + (c - 8)]
                else:
                    srcw = self.m_wog[j * KC + (c - 16)]
                S.op("pool", "dma_start", writes=[wtok], dsem=f"mw{wk_}", out=wb[:], in_=srcw)
                bank = pb % 2
                pb += 1
                for k in range(KC):
                    S.op("pe", "matmul", reads=[wtok, xtok[k // 4]], writes=[pst[bank]],
                         out=ps[bank][:], lhsT=wb[:, k, :], rhs=xb[:, k, :], start=(k == 0), stop=(k == KC - 1))
                if c < 8:
                    S.op("act", "copy", reads=[pst[bank]], writes=[qtok[c]], out=qT[:, c, :], in_=ps[bank][:])
                elif c < 16:
                    S.op("act", "copy", reads=[pst[bank]], writes=[ktok[c - 8]], out=kT[:, c - 8, :], in_=ps[bank][:])
                else:
                    S.op("act", "activation", reads=[pst[bank]], writes=[sogtok[c - 16]],
                         out=sog[:, c - 16, :], in_=ps[bank][:], func=AF.Sigmoid)
                self.flush_norm(1)
            self.flush_norm()
            for ch in range(4):
                bank = 2 + ch % 2
                pkb = ps[bank].bitcast(BF16)
                for c8 in range(8):
                    S.op("pe", "transpose", reads=[ktok[c8], self.itok], writes=[pst[bank]],
                         out=pkb[:, c8 * 128:(c8 + 1) * 128], in_=kT[:, c8, ch * 128:(ch + 1) * 128],
                         identity=self.ident[:])
                S.op("dve", "tensor_copy", reads=[pst[bank]], writes=[ktmtok[ch]], out=ktm[:, ch, :], in_=pkb[:, 0:1024])
            for blk in range(2, 6):
                wb, wtok, wk_ = wtm.next()
                S.op("pool", "dma_start", writes=[wtok], dsem=f"mt{wk_}", out=wb[:], in_=self.m_wtm[j * 6 + blk])
                for ch in range(4):
                    bank = 2 + (blk * 4 + ch) % 2
                    for k in range(KC):
                        S.op("pe", "matmul", reads=[wtok, xtok[k // 4]], writes=[pst[bank]],
                             out=ps[bank][:], lhsT=xb[:, k, ch * 128:(ch + 1) * 128], rhs=wb[:, k, :],
                             start=(k == 0), stop=(k == KC - 1))
                    S.op("act", "copy", reads=[pst[bank]], writes=[vtmtok[ch]],
                         out=vtm[:, ch, (blk - 2) * 512:(blk - 1) * 512], in_=ps[bank][:])
            for ch in range(4):
                tk = slice(ch * 128, (ch + 1) * 128)
                sm, smtok, _ = smr.next()
                G, ig, fp = sm[:, 0:2, :], sm[:, 0, :], sm[:, 1, :]
                lf, b_, btot, a_, r_ = sm[:, 2, :], sm[:, 3, :], sm[:, 4, :], sm[:, 5, :], sm[:, 6, :]
                mx, mrow, inter, rsw, qn = sm[:, 7, :], sm[:, 8, :], sm[:, 9, :], sm[:, 10, :], sm[:, 11, :]
                den, emm, rdd, mnew, dec = sm[:, 12, :], sm[:, 13, :], sm[:, 14, :], sm[:, 15, :], sm[:, 16, :]
                wk16, tmp = sm[:, 17, :], sm[:, 18, :]
                for k in range(KC):
                    S.op("pe", "matmul", reads=[wgtok, xtok[k // 4]], writes=[pst[0]],
                         out=ps[0][:, 0:8], lhsT=xb[:, k, tk], rhs=wg[:, k, :], start=(k == 0), stop=(k == KC - 1))
                S.op("dve", "tensor_tensor", reads=[pst[0], bgtok], writes=[smtok],
                     out=sm[:, 0:2, :].rearrange("p a b -> p (a b)"), in0=ps[0][:, 0:8], in1=bg[:], op=ALU.add)
                S.op("act", "activation", reads=[smtok], writes=[smtok], out=lf, in_=fp, func=AF.Exp, scale=-1.0)
                S.op("dve", "tensor_scalar_add", reads=[smtok], writes=[smtok], out=lf, in0=lf, scalar1=1.0)
                S.op("act", "activation", reads=[smtok], writes=[smtok], out=lf, in_=lf, func=AF.Ln)
                S.op("dve", "tensor_scalar_mul", reads=[smtok], writes=[smtok], out=lf, in0=lf, scalar1=-1.0)
                S.op("pe", "matmul", reads=[smtok, csttok], writes=[pst[0]],
                     out=ps[0][:, 8:12], lhsT=tri, rhs=lf, start=True, stop=True)
                S.op("pe", "matmul", reads=[smtok, self.ctok], writes=[pst[0]],
                     out=ps[0][:, 12:16], lhsT=self.ones[:], rhs=lf, start=True, stop=True)
                S.op("dve", "tensor_copy", reads=[pst[0]], writes=[smtok],
                     out=sm[:, 3:5, :].rearrange("p a b -> p (a b)"), in_=ps[0][:, 8:16])
                S.op("dve", "tensor_tensor", reads=[smtok, sttok], writes=[smtok], out=a_, in0=b_, in1=mprev[:], op=ALU.add)
                S.op("dve", "tensor_tensor", reads=[smtok], writes=[smtok], out=r_, in0=ig, in1=b_, op=ALU.subtract)
                S.op("dve", "tensor_tensor", reads=[smtok, csttok], writes=[dgtok], out=Dg[:],
                     in0=cst[:, 0:1, :].broadcast_to([128, 4, 128]), in1=bc(r_, 128), op=ALU.mult)
                S.op("pe", "matmul", reads=[dgtok, self.ctok], writes=[pst[1]],
                     out=ps[1][:], lhsT=self.ones[:], rhs=Dg[:].rearrange("p a b -> p (a b)"), start=True, stop=True)
                dm, dmtok, _ = dmr.next()
                S.op("dve", "tensor_tensor", reads=[pst[1], smtok], writes=[dmtok], out=dm[:],
                     in0=ps[1][:].rearrange("p (a b) -> p a b", b=128), in1=bc(b_, 128), op=ALU.add)
                S.op("dve", "tensor_tensor", reads=[csttok], writes=[dmtok], out=dm[:], in0=dm[:],
                     in1=cst[:, 3:4, :].broadcast_to([128, 4, 128]), op=ALU.add)
                S.op("dve", "tensor_reduce", reads=[dmtok], writes=[smtok], out=mx, in_=dm[:], axis=AX.X, op=ALU.max)
                S.op("dve", "tensor_tensor", reads=[smtok], writes=[smtok], out=mrow, in0=mx, in1=a_, op=ALU.max)
                S.op("dve", "tensor_tensor", reads=[smtok], writes=[dmtok], out=dm[:], in0=dm[:], in1=bc(mrow, 128),
                     op=ALU.subtract)
                S.op("act", "activation", reads=[dmtok], writes=[dmtok], out=dm[:], in_=dm[:], func=AF.Exp)
                S.op("dve", "tensor_tensor", reads=[smtok], writes=[smtok], out=inter, in0=a_, in1=mrow, op=ALU.subtract)
                S.op("act", "activation", reads=[smtok], writes=[smtok], out=inter, in_=inter, func=AF.Exp)
                for h in range(4):
                    for dc in range(2):
                        S.op("pe", "matmul", reads=[qtok[2 * h + dc], ktok[2 * h + dc]], writes=[pst[2]],
                             out=ps[2][:, h * 128:(h + 1) * 128], lhsT=qT[:, 2 * h + dc, tk], rhs=kT[:, 2 * h + dc, tk],
                             start=(dc == 0), stop=(dc == 1))
                S.op("dve", "scalar_tensor_tensor", reads=[pst[2]], writes=[dmtok], out=dm[:],
                     in0=ps[2][:].rearrange("p (a b) -> p a b", b=128), scalar=1.0 / 16.0, in1=dm[:],
                     op0=ALU.mult, op1=ALU.mult)
                S.op("dve", "tensor_reduce", reads=[dmtok], writes=[smtok], out=rsw, in_=dm[:], axis=AX.X, op=ALU.add)
                S.op("act", "copy", reads=[dmtok], writes=[wbftok], out=wbf[:], in_=dm[:])
                for h in range(4):
                    S.op("pe", "transpose", reads=[wbftok, self.itok], writes=[pst[3]],
                         out=ps3b[:, h * 128:(h + 1) * 128], in_=wbf[:, h, :], identity=self.ident[:])
                S.op("act", "copy", reads=[pst[3]], writes=[wTtok], out=wT[:].rearrange("p a b -> p (a b)"),
                     in_=ps3b[:, 0:512])
                for h in range(4):
                    for dc in range(2):
                        S.op("pe", "matmul", reads=[qtok[2 * h + dc], sttok], writes=[pst[0]],
                             out=ps[0][:, 16 + h:17 + h], lhsT=qT[:, 2 * h + dc, tk], rhs=nb[:, 2 * h + dc:2 * h + dc + 1],
                             start=(dc == 0), stop=(dc == 1))
                S.op("dve", "tensor_copy", reads=[pst[0]], writes=[smtok], out=qn, in_=ps[0][:, 16:20])
                S.op("dve", "tensor_tensor", reads=[smtok], writes=[smtok], out=den, in0=inter, in1=qn, op=ALU.mult)
                S.op("dve", "tensor_tensor", reads=[smtok], writes=[smtok], out=den, in0=den, in1=rsw, op=ALU.add)
                S.op("dve", "tensor_scalar_mul", reads=[smtok], writes=[smtok], out=tmp, in0=den, scalar1=-1.0)
                S.op("dve", "tensor_tensor", reads=[smtok], writes=[smtok], out=den, in0=den, in1=tmp, op=ALU.max)
                S.op("act", "activation", reads=[smtok], writes=[smtok], out=emm, in_=mrow, func=AF.Exp, scale=-1.0)
                S.op("dve", "tensor_tensor", reads=[smtok], writes=[smtok], out=den, in0=den, in1=emm, op=ALU.max)
                S.op("dve", "reciprocal", reads=[smtok], writes=[smtok], out=rdd, in_=den)
                S.op("dve", "tensor_tensor", reads=[smtok], writes=[smtok], out=sm[:, 19, :], in0=inter, in1=rdd,
                     op=ALU.mult)
                for h in range(4):
                    for dc in range(2):
                        S.op("pe", "matmul", reads=[qtok[2 * h + dc], cbtok[2 * h + dc]], writes=[pst[4]],
                             out=ps[4][:], lhsT=qT[:, 2 * h + dc, tk], rhs=Cb[:, 2 * h + dc, :],
                             start=(dc == 0), stop=(dc == 1))
                    S.op("pe", "matmul", reads=[wTtok, vtmtok[ch]], writes=[pst[5]],
                         out=ps[5][:], lhsT=wT[:, h, :], rhs=vtm[:, ch, h * 512:(h + 1) * 512], start=True, stop=True)
                    bsb, bstok, _ = Bs.next()
                    S.op("act", "activation", reads=[pst[5], smtok], writes=[bstok], out=bsb[:], in_=ps[5][:],
                         func=AF.Identity, scale=sm[:, 14, h:h + 1])
                    S.op("dve", "scalar_tensor_tensor", reads=[pst[4], bstok, smtok], writes=[hbtok],
                         out=hb[:, h, :], in0=ps[4][:], scalar=sm[:, 19, h:h + 1], in1=bsb[:],
                         op0=ALU.mult, op1=ALU.add)
                for half in range(2):
                    pbv = ps6b if half == 0 else ps7b
                    for i in range(8):
                        c = half * 8 + i
                        S.op("pe", "transpose", reads=[hbtok, self.itok], writes=[pst[6 + half]],
                             out=pbv[:, i * 128:(i + 1) * 128], in_=hb[:, c // 4, (c % 4) * 128:(c % 4 + 1) * 128],
                             identity=self.ident[:])
                    S.op("dve", "tensor_tensor", reads=[pst[6 + half]], writes=sogtok[half * 8:half * 8 + 8],
                         out=sog[:, half * 8:half * 8 + 8, tk], in0=pbv[:].rearrange("p (a b) -> p a b", b=128),
                         in1=sog[:, half * 8:half * 8 + 8, tk], op=ALU.mult)
                S.op("pe", "matmul", reads=[smtok, csttok], writes=[pst[0]],
                     out=ps[0][:, 20:24], lhsT=sel, rhs=mrow, start=True, stop=True)
                S.op("dve", "tensor_copy", reads=[pst[0]], writes=[smtok], out=mnew, in_=ps[0][:, 20:24])
                S.op("dve", "tensor_tensor", reads=[smtok], writes=[smtok], out=tmp, in0=btot, in1=mnew, op=ALU.subtract)
                S.op("dve", "tensor_tensor", reads=[smtok, sttok], writes=[smtok], out=dec, in0=tmp, in1=mprev[:], op=ALU.add)
                S.op("act", "activation", reads=[smtok], writes=[smtok], out=dec, in_=dec, func=AF.Exp)
                S.op("dve", "tensor_tensor", reads=[smtok], writes=[smtok], out=wk16, in0=tmp, in1=r_, op=ALU.add)
                S.op("act", "activation", reads=[smtok], writes=[smtok], out=wk16, in_=wk16, func=AF.Exp)
                S.op("dve", "tensor_scalar_mul", reads=[smtok], writes=[smtok], out=wk16, in0=wk16, scalar1=1.0 / 16.0)
                S.op("dve", "tensor_tensor", reads=[ktmtok[ch], smtok], writes=[kwtok], out=kw[:],
                     in0=ktm[:, ch, :].rearrange("p (a b) -> p a b", b=256), in1=bc(wk16, 256), op=ALU.mult)
                for h in range(4):
                    for dc in range(2):
                        jj = 2 * h + dc
                        bank = 1 + jj % 2
                        S.op("pe", "matmul", reads=[kwtok, vtmtok[ch]], writes=[pst[bank]],
                             out=ps[bank][:], lhsT=kw[:, h, dc * 128:(dc + 1) * 128],
                             rhs=vtm[:, ch, h * 512:(h + 1) * 512], start=True, stop=True)
                        S.op("dve", "scalar_tensor_tensor", reads=[pst[bank], smtok], writes=[ctok[jj]],
                             out=C[:, jj, :], in0=C[:, jj, :], scalar=sm[:, 16, h:h + 1], in1=ps[bank][:],
                             op0=ALU.mult, op1=ALU.add)
                        S.op("act", "copy", reads=[ctok[jj]], writes=[cbtok[jj]], out=Cb[:, jj, :], in_=C[:, jj, :])
                for h in range(4):
                    for dc in range(2):
                        jj = 2 * h + dc
                        S.op("pe", "matmul", reads=[kwtok, csttok], writes=[pst[0]],
                             out=ps[0][:, 24 + jj:25 + jj], lhsT=kw[:, h, dc * 128:(dc + 1) * 128], rhs=onesb[:],
                             start=True, stop=True)
                S.op("dve", "tensor_tensor", reads=[smtok], writes=[sttok],
                     out=nst[:].rearrange("p (a b) -> p a b", b=2), in0=nst[:].rearrange("p (a b) -> p a b", b=2),
                     in1=bc(dec, 2), op=ALU.mult)
                S.op("dve", "tensor_tensor", reads=[pst[0]], writes=[sttok], out=nst[:], in0=nst[:],
                     in1=ps[0][:, 24:32], op=ALU.add)
                S.op("act", "copy", reads=[sttok], writes=[sttok], out=nb[:], in_=nst[:])
                S.op("dve", "tensor_copy", reads=[smtok], writes=[sttok], out=mprev[:], in_=mnew)

            if ti + 1 < self.NTILE:
                nx = xrot.next()
                self.load_xT(nx[0], nx[1], nx[2], src, (ti + 1) * TT)

            def lhs_of(c):
                wb, wtok, wk_ = wsl.next()
                S.op("pool", "dma_start", writes=[wtok], dsem=f"mw{wk_}", out=wb[:], in_=self.wo_m[j * KC + c])
                return (lambda fc: wb[:, fc, :]), wtok

            self.tail(t0, src, dst, si, 1.0 / ALPHA, KC, lhs_of, lambda fc: sog[:, fc, :], sogtok)


def full_plan():
    plan = []
    for l in range(DEPTH):
        plan.append(("ffn", 2 * l))
        plan.append(("att", l // 2) if l % 2 == 0 else ("mlstm", l // 2))
        plan.append(("ffn", 2 * l + 1))
    return plan


def lay_w13(w):
    n = w.shape[0]
    return np.ascontiguousarray(
        w.reshape(n, KC, 128, NFB, FB).transpose(0, 3, 2, 1, 4)).reshape(n * NFB, 128, KC, FB)


def lay_w2(w):
    n = w.shape[0]
    return np.ascontiguousarray(
        w.reshape(n, FCH, 128, KC, 128).transpose(0, 3, 2, 1, 4)).reshape(n * KC, 128, FCH, 128)


def lay_ln(v):
    n = v.shape[0]
    return np.ascontiguousarray(v.reshape(n, KC, 128).transpose(2, 0, 1)).reshape(128, n * KC)


def lay_kchunks(w, width):
    n = w.shape[1] // width
    return np.ascontiguousarray(w.reshape(KC, 128, n, width).transpose(2, 1, 0, 3))


def att_layouts(w_qkv, sinks, w_o):
    n = w_qkv.shape[0]
    wq = np.concatenate([lay_kchunks(w_qkv[i][:, :2048], 128) for i in range(n)], axis=0)
    wk = []
    for i in range(n):
        kk = w_qkv[i][:, 2048:2304].reshape(D, 4, 1, 64)
        z = np.zeros_like(kk)
        kk2 = np.concatenate([np.concatenate([kk, z], axis=3), np.concatenate([z, kk], axis=3)], axis=2)
        wk.append(lay_kchunks(kk2.reshape(D, 8 * 128), 128))
    wk = np.concatenate(wk, axis=0)
    wv = np.concatenate([lay_kchunks(w_qkv[i][:, 2304:2560], 256) for i in range(n)], axis=0)
    wo = np.concatenate([lay_kchunks(w_o[i], 128) for i in range(n)], axis=0)
    sk = np.ascontiguousarray(np.broadcast_to(sinks[:, None, :], (n, 128, 32))).astype(np.float32)
    return {"wq": wq, "wk": wk, "wv": wv, "wo_a": wo, "sinks": sk}


def ml_layouts(w_in, b_gates, w_o):
    n = w_in.shape[0]
    cat = lambda f: np.concatenate([f(i) for i in range(n)], axis=0)
    return {
        "m_wq": cat(lambda i: lay_kchunks(w_in[i][:, 0:1024], 128)),
        "m_wk": cat(lambda i: lay_kchunks(w_in[i][:, 1024:2048], 128)),
        "m_wtm": cat(lambda i: lay_kchunks(w_in[i][:, 1024:4096], 512)),
        "m_wog": cat(lambda i: lay_kchunks(w_in[i][:, 4096:6144], 128)),
        "m_wg": cat(lambda i: lay_kchunks(w_in[i][:, 6144:6152], 8)),
        "m_bg": np.ascontiguousarray(np.broadcast_to(b_gates[:, None, :], (n, 128, 8))).astype(np.float32),
        "wo_m": cat(lambda i: lay_kchunks(w_o[i], 128)),
    }


def ml_consts():
    i = np.arange(128)
    ident = np.eye(128, dtype=np.float32)
    tri = (i[:, None] <= i[None, :]).astype(np.float32)
    sel = np.zeros((128, 128), np.float32)
    sel[127, :] = 1.0
    mask = np.where(i[None, :] <= i[:, None], 0.0, NEG).astype(np.float32)
    return np.ascontiguousarray(np.concatenate([ident, tri, sel, mask], axis=1))


def alibi_table():
    q = np.arange(128)[:, None]
    jj = np.arange(256)[None, :]
    dist = (128 + q - jj).astype(np.float32)
    valid = (dist >= 0) & (dist < 128)
    slopes = (2.0 ** (-8.0 * np.arange(1, 33, dtype=np.float32) / 32)).astype(np.float32)
    tab = np.where(valid[:, None, :], -slopes[None, :, None] * dist[:, None, :], np.float32(NEG))
    return np.ascontiguousarray(tab.astype(np.float32).reshape(128, 32 * 256))


def prepare_weights(ffn_w1, ffn_w3, ffn_w2, ln_g, ln_b, att_w_qkv, att_sinks, att_w_o,
                    mlstm_w_in, mlstm_b_gates, mlstm_w_o):
    f32 = lambda a: np.asarray(a, dtype=np.float32)
    w = {
        "w1": lay_w13(f32(ffn_w1).reshape(2 * DEPTH, D, FF)),
        "w3": lay_w13(f32(ffn_w3).reshape(2 * DEPTH, D, FF)),
        "w2": lay_w2(f32(ffn_w2).reshape(2 * DEPTH, FF, D)),
        "ln_g": lay_ln(f32(ln_g).reshape(3 * DEPTH, D)),
        "ln_b": lay_ln(f32(ln_b).reshape(3 * DEPTH, D)),
        "alibi": alibi_table(),
        "identd": np.eye(128, dtype=np.float32),
        "m_const": ml_consts(),
    }
    w.update(att_layouts(f32(att_w_qkv), f32(att_sinks), f32(att_w_o)))
    w.update(ml_layouts(f32(mlstm_w_in), f32(mlstm_b_gates), f32(mlstm_w_o)))
    return w


def run_module(xs, weights, s_tok):
    n = len(xs)
    b = Builder(s_tok, full_plan(), 2 * DEPTH, DEPTH // 2, DEPTH // 2)
    nc = b.build()
    in_maps = []
    for i in range(n):
        m = dict(weights)
        m["xT"] = np.ascontiguousarray(np.asarray(xs[i], dtype=np.float32).T)
        in_maps.append(m)
    res = run_bass_kernel_spmd(nc, in_maps, core_ids=list(range(n)))
    return [np.ascontiguousarray(r["outT"].T) for r in res.results]


def kernel(x, ffn_w1, ffn_w3, ffn_w2, ln_g, ln_b, att_w_qkv, att_sinks, att_w_o,
           mlstm_w_in, mlstm_b_gates, mlstm_w_o):
    x = np.asarray(x, dtype=np.float32)
    weights = prepare_weights(ffn_w1, ffn_w3, ffn_w2, ln_g, ln_b, att_w_qkv, att_sinks, att_w_o,
                              mlstm_w_in, mlstm_b_gates, mlstm_w_o)
    outs = run_module([x[i] for i in range(x.shape[0])], weights, x.shape[1])
    return np.stack(outs, axis=0).astype(np.float32)
```
